# Optimizing a Trainium2 kernel written in Bass

```python
import math
import jax, jax.numpy as jnp
from jax import lax
import numpy as np

D_MODEL = 1024
BATCH = 8
SEQ = 2048
DEPTH = 2

CHUNK = 64
RET_HEADS = 4
RET_DK = 128
RET_DV = 256
RET_QK = RET_HEADS * RET_DK
RET_V = RET_HEADS * RET_DV
ROPE_BASE = 10000.0
HG_HEADS = 8
HG_DK = 128
HG_DV = 128
HG_K = HG_HEADS * HG_DK
HG_V = HG_HEADS * HG_DV
HG_BLOCK = 16
SWA_HQ = 16
SWA_HKV = 4
SWA_HD = 64
SWA_Q = SWA_HQ * SWA_HD
SWA_KV = SWA_HKV * SWA_HD
SWA_WINDOW = 128
SWA_WIN_CHUNKS = SWA_WINDOW // CHUNK
D_FF = 2816
N_EXPERTS = 8
TOP_K = 2
D_EXP = 3584
N_DENSE = (DEPTH + 1) // 2
N_MOE = DEPTH // 2
LN_EPS = 1e-5
RMS_EPS = 1e-6
DN_ALPHA = (2.0 * DEPTH) ** 0.25
DN_BETA = (8.0 * DEPTH) ** -0.25
IN_SPLITS = (RET_QK, RET_QK, RET_V, RET_V,
             HG_K, HG_K, HG_V, HG_V,
             SWA_Q, SWA_KV, SWA_KV,
             D_MODEL, D_MODEL, D_MODEL)
N_IN = sum(IN_SPLITS)

kernel_name = "hybrid_retention_hgrn2_swa_moe_deepnorm"


def layer_norm(x, g, b):
    xf = x.astype(jnp.float32)
    mu = jnp.mean(xf, -1, keepdims=True)
    var = jnp.mean(jnp.square(xf - mu), -1, keepdims=True)
    return ((xf - mu) * lax.rsqrt(var + LN_EPS) * g + b).astype(x.dtype)


def rms_norm(x):
    xf = x.astype(jnp.float32)
    return xf * lax.rsqrt(jnp.mean(xf * xf, -1, keepdims=True) + RMS_EPS)


def rotary(x, pos):
    half = x.shape[-1] // 2
    inv = 1.0 / (ROPE_BASE ** jnp.linspace(0.0, 1.0, half, dtype=jnp.float32))
    ang = pos[:, None] * inv[None, :]
    cos = jnp.cos(ang)[None, :, None, :]
    sin = jnp.sin(ang)[None, :, None, :]
    x1, x2 = x[..., :half], x[..., half:]
    return jnp.concatenate([x1 * cos - x2 * sin, x1 * sin + x2 * cos], axis=-1)


def retention(q, k, v):
    B, S, H, DK = q.shape
    DV = v.shape[-1]
    n = S // CHUNK
    log_gamma = jnp.log(1.0 - 2.0 ** (-5.0 - jnp.arange(H, dtype=jnp.float32)))
    k = k * DK ** -0.5
    qc = q.reshape(B, n, CHUNK, H, DK)
    kc = k.reshape(B, n, CHUNK, H, DK)
    vc = v.reshape(B, n, CHUNK, H, DV)
    idx = jnp.arange(CHUNK, dtype=jnp.float32)
    diff = idx[:, None] - idx[None, :]
    decay = jnp.where(diff[None] >= 0,
                      jnp.exp(jnp.maximum(diff, 0.0)[None] * log_gamma[:, None, None]), 0.0)
    scores = jnp.einsum('bnthd,bnshd->bnhts', qc, kc) * decay
    o_intra = jnp.einsum('bnhts,bnshv->bnthv', scores, vc)
    k_decay = jnp.exp((CHUNK - 1.0 - idx)[:, None] * log_gamma[None, :])
    q_decay = jnp.exp((idx + 1.0)[:, None] * log_gamma[None, :])
    chunk_decay = jnp.exp(CHUNK * log_gamma)[None, :, None, None]

    def step(state, inp):
        q_n, k_n, v_n = inp
        o = jnp.einsum('bthd,th,bhdv->bthv', q_n, q_decay, state)
        state = chunk_decay * state + jnp.einsum('bshd,sh,bshv->bhdv', k_n, k_decay, v_n)
        return state, o

    init = jnp.zeros((B, H, DK, DV), jnp.float32)
    _, o_inter = lax.scan(step, init, (jnp.moveaxis(qc, 1, 0), jnp.moveaxis(kc, 1, 0),
                                       jnp.moveaxis(vc, 1, 0)))
    return (o_intra + jnp.moveaxis(o_inter, 0, 1)).reshape(B, S, H, DV)


def hgrn2_scan(q, log_f, k, v):
    B, S, H, DK = q.shape
    DV = v.shape[-1]
    n = S // HG_BLOCK
    L = HG_BLOCK
    qc = q.reshape(B, n, L, H, DK)
    kc = k.reshape(B, n, L, H, DK)
    vc = v.reshape(B, n, L, H, DV)
    bcum = jnp.cumsum(log_f.reshape(B, n, L, H, DK), axis=2)
    q_t = qc * jnp.exp(bcum)
    k_t = kc * jnp.exp(-bcum)
    causal = jnp.tril(jnp.ones((L, L), dtype=bool))
    a = jnp.where(causal, jnp.einsum('bnthd,bnshd->bnhts', q_t, k_t), 0.0)
    o_intra = jnp.einsum('bnhts,bnshv->bnthv', a, vc)
    b_last = bcum[:, :, -1]
    k_state = kc * jnp.exp(b_last[:, :, None] - bcum)
    blk_decay = jnp.exp(b_last)

    def step(state, inp):
        q_n, ks_n, v_n, d_n = inp
        o = jnp.einsum('bthd,bhdv->bthv', q_n, state)
        state = d_n[..., None] * state + jnp.einsum('bshd,bshv->bhdv', ks_n, v_n)
        return state, o

    init = jnp.zeros((B, H, DK, DV), jnp.float32)
    _, o_inter = lax.scan(step, init, (jnp.moveaxis(q_t, 1, 0), jnp.moveaxis(k_state, 1, 0),
                                       jnp.moveaxis(vc, 1, 0), jnp.moveaxis(blk_decay, 1, 0)))
    return (o_intra + jnp.moveaxis(o_inter, 0, 1)).reshape(B, S, H, DV)


def swa_with_sinks(q, k, v, sinks):
    B, S, HQ, HD = q.shape
    HKV = k.shape[2]
    G = HQ // HKV
    W = SWA_WIN_CHUNKS
    n = S // CHUNK
    qc = q.astype(jnp.float32).reshape(B, n, CHUNK, HKV, G, HD)
    kc = k.astype(jnp.float32).reshape(B, n, CHUNK, HKV, HD)
    vc = v.astype(jnp.float32).reshape(B, n, CHUNK, HKV, HD)
    padw = ((0, 0), (W, 0), (0, 0), (0, 0), (0, 0))
    kp = jnp.pad(kc, padw)
    vp = jnp.pad(vc, padw)
    kb = jnp.concatenate([kp[:, j:j + n] for j in range(W + 1)], axis=2)
    vb = jnp.concatenate([vp[:, j:j + n] for j in range(W + 1)], axis=2)
    chunk_ids = jnp.arange(n)[:, None] - W + jnp.arange(W + 1)[None, :]
    valid = jnp.repeat(chunk_ids >= 0, CHUNK, axis=1)
    s = jnp.einsum('bntkgd,bnskd->bnkgts', qc, kb) * HD ** -0.5
    s = jnp.where(valid[None, :, None, None, None, :], s, -jnp.inf)
    sink = sinks.astype(jnp.float32).reshape(HKV, G)[None, None, :, :, None, None]
    m = jnp.maximum(jnp.max(s, -1, keepdims=True), sink)
    p = jnp.exp(s - m)
    p = p / (jnp.sum(p, -1, keepdims=True) + jnp.exp(sink - m))
    o = jnp.einsum('bnkgts,bnskd->bntkgd', p, vb)
    return o.reshape(B, S, HQ * HD)


def swiglu(h, w_gate, w_up, w_down):
    return (jax.nn.silu(h @ w_gate) * (h @ w_up)) @ w_down


def moe_swiglu(h2, w_router, w_gate, w_up, w_down):
    logits = (h2 @ w_router).astype(jnp.float32)
    top_logits, top_idx = lax.top_k(logits, TOP_K)
    top_w = jax.nn.softmax(top_logits, axis=-1)
    flat_e = top_idx.reshape(-1)
    order = jnp.argsort(flat_e)
    tok = order // TOP_K
    xs = h2[tok]
    sizes = jnp.bincount(flat_e, length=N_EXPERTS).astype(jnp.int32)
    hid = jax.nn.silu(lax.ragged_dot(xs, w_gate, sizes)) * lax.ragged_dot(xs, w_up, sizes)
    y = lax.ragged_dot(hid, w_down, sizes)
    y = y * top_w.reshape(-1)[order][:, None].astype(y.dtype)
    return jnp.zeros_like(h2).at[tok].add(y.astype(h2.dtype))


def setup_inputs(seed: int = 0) -> dict:
    key = jax.random.key(seed)
    ks = jax.random.split(key, 24)
    f32 = jnp.float32
    D = D_MODEL

    def nrm(k, shape, scale):
        return jax.random.normal(k, shape, f32) * scale

    return {
        'x': nrm(ks[0], (BATCH, SEQ, D), 1.0),
        'ln_in_g': 1.0 + nrm(ks[1], (D,), 0.02),
        'ln_in_b': nrm(ks[2], (D,), 0.02),
        'w_in': nrm(ks[3], (DEPTH, D, N_IN), D ** -0.5),
        'ret_w_out': nrm(ks[4], (DEPTH, RET_V, D), DN_BETA * RET_V ** -0.5),
        'hgrn_lower_bounds': 1.0 + nrm(ks[5], (DEPTH, HG_K), 0.5),
        'hgrn_norm_g': 1.0 + nrm(ks[6], (DEPTH, HG_DV), 0.02),
        'hgrn_w_out': nrm(ks[7], (DEPTH, HG_V, D), DN_BETA * HG_V ** -0.5),
        'swa_sinks': nrm(ks[8], (DEPTH, SWA_HQ), 0.5),
        'swa_w_out': nrm(ks[9], (DEPTH, SWA_Q, D), DN_BETA * SWA_Q ** -0.5),
        'w_o': nrm(ks[10], (DEPTH, D, D), DN_BETA * D ** -0.5),
        'ln_mix_g': 1.0 + nrm(ks[11], (DEPTH, D), 0.02),
        'ln_mix_b': nrm(ks[12], (DEPTH, D), 0.02),
        'ffn_w_gate': nrm(ks[13], (N_DENSE, D, D_FF), D ** -0.5),
        'ffn_w_up': nrm(ks[14], (N_DENSE, D, D_FF), D ** -0.5),
        'ffn_w_down': nrm(ks[15], (N_DENSE, D_FF, D), DN_BETA * D_FF ** -0.5),
        'moe_router': nrm(ks[16], (N_MOE, D, N_EXPERTS), D ** -0.5),
        'moe_w_gate': nrm(ks[17], (N_MOE, N_EXPERTS, D, D_EXP), D ** -0.5),
        'moe_w_up': nrm(ks[18], (N_MOE, N_EXPERTS, D, D_EXP), D ** -0.5),
        'moe_w_down': nrm(ks[19], (N_MOE, N_EXPERTS, D_EXP, D), DN_BETA * D_EXP ** -0.5),
        'ln_ffn_g': 1.0 + nrm(ks[20], (DEPTH, D), 0.02),
        'ln_ffn_b': nrm(ks[21], (DEPTH, D), 0.02),
    }


def reference(x, ln_in_g, ln_in_b, w_in, ret_w_out, hgrn_lower_bounds, hgrn_norm_g, hgrn_w_out,
              swa_sinks, swa_w_out, w_o, ln_mix_g, ln_mix_b, ffn_w_gate, ffn_w_up, ffn_w_down,
              moe_router, moe_w_gate, moe_w_up, moe_w_down, ln_ffn_g, ln_ffn_b):
    B, S, D = x.shape
    f32 = jnp.float32
    pos = jnp.arange(S, dtype=f32)
    split_points = np.cumsum(np.array(IN_SPLITS))[:-1].tolist()
    lb_all = jnp.cumsum(jax.nn.softmax(hgrn_lower_bounds.astype(f32), axis=0), axis=0)
    lb_all = lb_all - lb_all[0]

    h = layer_norm(x, ln_in_g, ln_in_b)
    for layer in range(DEPTH):
        proj = jnp.einsum('bsd,dc->bsc', h, w_in[layer])
        (rq, rk, rv, rg, hq, hf, hi, hg, sq, sk, sv, ga, gb, gc) = jnp.split(proj, split_points, axis=-1)

        r_q = rotary(rq.astype(f32).reshape(B, S, RET_HEADS, RET_DK), pos)
        r_k = rotary(rk.astype(f32).reshape(B, S, RET_HEADS, RET_DK), pos)
        r_v = rv.astype(f32).reshape(B, S, RET_HEADS, RET_DV)
        r_o = rms_norm(retention(r_q, r_k, r_v)).reshape(B, S, RET_V)
        y_a = (jax.nn.silu(rg.astype(f32)) * r_o) @ ret_w_out[layer]

        lb = lb_all[layer].reshape(HG_HEADS, HG_DK)
        z = hf.astype(f32).reshape(B, S, HG_HEADS, HG_DK)
        log_f = jnp.logaddexp(jnp.log(lb), jnp.log1p(-lb) + jax.nn.log_sigmoid(z))
        h_k = -jnp.expm1(log_f)
        h_q = hq.astype(f32).reshape(B, S, HG_HEADS, HG_DK)
        h_v = hi.astype(f32).reshape(B, S, HG_HEADS, HG_DV)
        h_o = rms_norm(hgrn2_scan(h_q, log_f, h_k, h_v)) * hgrn_norm_g[layer]
        y_b = (jax.nn.silu(hg.astype(f32)) * h_o.reshape(B, S, HG_V)) @ hgrn_w_out[layer]

        s_o = swa_with_sinks(sq.reshape(B, S, SWA_HQ, SWA_HD), sk.reshape(B, S, SWA_HKV, SWA_HD),
                             sv.reshape(B, S, SWA_HKV, SWA_HD), swa_sinks[layer])
        y_c = s_o @ swa_w_out[layer]

        merged = (jax.nn.sigmoid(ga.astype(f32)) * y_a + jax.nn.sigmoid(gb.astype(f32)) * y_b
                  + jax.nn.sigmoid(gc.astype(f32)) * y_c)
        mix = merged @ w_o[layer]
        h = layer_norm((DN_ALPHA * h + mix).astype(h.dtype), ln_mix_g[layer], ln_mix_b[layer])

        j = layer // 2
        if layer % 2 == 0:
            ff = swiglu(h, ffn_w_gate[j], ffn_w_up[j], ffn_w_down[j])
        else:
            ff = moe_swiglu(h.reshape(B * S, D), moe_router[j], moe_w_gate[j], moe_w_up[j],
                            moe_w_down[j]).reshape(B, S, D)
        h = layer_norm((DN_ALPHA * h + ff).astype(h.dtype), ln_ffn_g[layer], ln_ffn_b[layer])
    return h
```

```python
import contextlib
import math
import numpy as np
import concourse.bass as bass
import concourse.mybir as mybir
from concourse.bass_utils import run_bass_kernel_spmd

F32 = mybir.dt.float32
BF16 = mybir.dt.bfloat16
ALU = mybir.AluOpType
AF = mybir.ActivationFunctionType
AX = mybir.AxisListType

S = 2048
D = 1024
NT = 16
KC = 8
DEPTH = 2
N_IN = 11776
D_FF = 2816
D_EXP = 3584
NEXP = 8
LN_EPS = 1e-5
RMS_EPS = 1e-6
DN_ALPHA = (2.0 * DEPTH) ** 0.25
OFF = dict(rq=0, rk=512, rv=1024, rg=2048, hq=3072, hf=4096, hi=5120, hg=6144,
           sq=7168, sk=8192, sv=8448, ga=8704, gb=9728, gc=10752)
HL = 32

COMPUTE = ('pe', 'act', 'dve', 'pool')
ALL_ENG = ('pe', 'act', 'dve', 'pool', 'sp')
NDMASEM = 6


class Op:
    __slots__ = ('eng', 'fn', 'deps', 'dma', 'sem', 'ticket', 'idx', 'prev_ticket', 'semkey')


class Prog:
    def __init__(self, nc, sems, dma_sems, same_engine_sync=True):
        self.nc = nc
        self.sems = sems
        self.dma_sems = dma_sems
        self.count = {e: 0 for e in COMPUTE}
        self.dma_count = {(q, i): 0 for q in dma_sems for i in range(len(dma_sems[q]))}
        self.dma_rr = {q: 0 for q in dma_sems}
        self.waited = {e: {} for e in ALL_ENG}
        self.same_engine_sync = same_engine_sync
        self.ops = []
        self.lw = {}
        self.rd = {}
        self.prev_final = []

    def op(self, eng, fn, r=(), w=(), dma=False):
        deps = set()
        for b in r:
            if b in self.lw:
                deps.add(self.lw[b])
        for b in w:
            if b in self.lw:
                deps.add(self.lw[b])
            for x in self.rd.get(b, ()):
                deps.add(x)
        o = Op()
        o.eng = eng; o.fn = fn; o.dma = dma; o.sem = None; o.ticket = None
        o.idx = len(self.ops)
        o.deps = deps
        self.ops.append(o)
        for b in r:
            self.rd.setdefault(b, []).append(o.idx)
        for b in w:
            self.lw[b] = o.idx
            self.rd[b] = []
        return o.idx

    def dma(self, q, out, in_, r=(), w=(), **kw):
        def fn(e):
            return e.dma_start(out=out, in_=in_, **kw)
        return self.op(q, fn, r, w, dma=True)

    def emit(self, final_wait=False):
        nc = self.nc
        ops = self.ops
        needed = set()
        for o in ops:
            for d in o.deps:
                od = ops[d]
                if od.eng == o.eng and not od.dma:
                    if od.eng == 'pe' or not self.same_engine_sync:
                        continue
                needed.add(d)
        last = {}
        for o in ops:
            if not o.dma:
                last[o.eng] = o.idx
        for e, i in last.items():
            needed.add(i)
        for o in ops:
            if o.dma:
                q = o.eng
                i = self.dma_rr[q]
                self.dma_rr[q] = (i + 1) % len(self.dma_sems[q])
                o.prev_ticket = self.dma_count[(q, i)]
                self.dma_count[(q, i)] += 16
                o.sem = self.dma_sems[q][i]
                o.semkey = ('d', q, i)
                o.ticket = self.dma_count[(q, i)]
            elif o.idx in needed:
                self.count[o.eng] += 1
                o.sem = self.sems[o.eng]
                o.semkey = ('c', o.eng)
                o.ticket = self.count[o.eng]
        by_eng = {e: [o for o in ops if o.eng == e] for e in ALL_ENG}
        prev_final = self.prev_final
        waited = self.waited
        same_sync = self.same_engine_sync

        def run(engname, eh):
            wd = waited[engname]

            def wait(semkey, sem, ticket):
                if wd.get(semkey, 0) >= ticket:
                    return
                eh.wait_ge(sem, ticket)
                wd[semkey] = ticket
            for (semkey, sem, ticket) in prev_final:
                if semkey == ('c', engname) and (engname == 'pe' or not same_sync):
                    continue
                wait(semkey, sem, ticket)
            for o in by_eng[engname]:
                for d in sorted(o.deps):
                    od = ops[d]
                    if od.ticket is None:
                        continue
                    if od.eng == o.eng and not od.dma and (engname == 'pe' or not same_sync):
                        continue
                    wait(od.semkey, od.sem, od.ticket)
                if o.dma and o.prev_ticket > 0:
                    wait(o.semkey, o.sem, o.prev_ticket)
                inst = o.fn(eh)
                if o.ticket is not None:
                    inst.then_inc(o.sem, 16 if o.dma else 1)
            if engname in self.dma_sems:
                for i, sem in enumerate(self.dma_sems[engname]):
                    t = self.dma_count[(engname, i)]
                    if t > 0:
                        wait(('d', engname, i), sem, t)
            if final_wait and engname == 'sp':
                for q in self.dma_sems:
                    for i, sem in enumerate(self.dma_sems[q]):
                        t = self.dma_count[(q, i)]
                        if t > 0:
                            wait(('d', q, i), sem, t)

        with nc.Block() as block:
            @block.tensor
            def _(e):
                run('pe', e)

            @block.scalar
            def _(e):
                run('act', e)

            @block.vector
            def _(e):
                run('dve', e)

            @block.gpsimd
            def _(e):
                run('pool', e)

            @block.sync
            def _(e):
                run('sp', e)
        fin = []
        for e in COMPUTE:
            if self.count[e] > 0:
                fin.append((('c', e), self.sems[e], self.count[e]))
        for (q, i), t in self.dma_count.items():
            if t > 0:
                fin.append((('d', q, i), self.dma_sems[q][i], t))
        self.prev_final = fin
        self.ops = []
        self.lw = {}
        self.rd = {}


class Ctx:
    pass


G_SKIP = set()
import os
HG_LEVEL = int(os.environ.get('HG_LEVEL', '4'))
SW_LEVEL = int(os.environ.get('SW_LEVEL', '3'))
SW_SUB = int(os.environ.get("SW_SUB", "3"))
SW_W = int(os.environ.get("SW_W", "4"))
LN_W = int(os.environ.get("LN_W", "3"))
HG_MERGE = bool(int(os.environ.get("HG_MERGE", "0")))
SAME_SYNC = bool(int(os.environ.get('SAME_SYNC', '1')))


def _consts():
    c = {}
    half = 64
    inv = (1.0 / (10000.0 ** np.linspace(0.0, 1.0, half, dtype=np.float32))).astype(np.float32)
    pos = np.arange(S, dtype=np.float32)
    ang = pos[:, None] * inv[None, :]
    cos = np.cos(ang).astype(np.float32).T
    sin = np.sin(ang).astype(np.float32).T
    c['cosT'] = np.ascontiguousarray(np.concatenate([cos, cos], 0))
    c['sinT'] = np.ascontiguousarray(np.concatenate([sin, sin], 0))
    pm = np.zeros((128, 128), np.float32)
    for d in range(64):
        pm[d + 64, d] = -1.0
        pm[d, d + 64] = 1.0
    c['pm'] = pm
    c['ident'] = np.eye(128, dtype=np.float32)
    lg = np.log(1.0 - 2.0 ** (-5.0 - np.arange(4, dtype=np.float64)))
    idx = np.arange(128, dtype=np.float64)
    dt = np.zeros((128, 4, 128), np.float32)
    qd = np.zeros((128, 4, 128), np.float32)
    kd = np.zeros((128, 4), np.float32)
    for h in range(4):
        diff = idx[None, :] - idx[:, None]
        dt[:, h, :] = np.where(diff >= 0, np.exp(np.maximum(diff, 0) * lg[h]), 0.0) * 128 ** -0.5
        qd[:, h, :] = np.exp((idx + 1.0) * lg[h])[None, :]
        kd[:, h] = np.exp((127.0 - idx) * lg[h]) * 128 ** -0.5
    c['ret_dt'] = dt
    c['ret_qd'] = qd
    c['ret_kd'] = kd
    c['ret_cd'] = [float(np.exp(128.0 * lg[h])) for h in range(4)]
    s_ = np.arange(128)
    bm = ((s_[:, None] // HL) == (s_[None, :] // HL)) & (s_[:, None] <= s_[None, :])
    c['hg_bm'] = bm.astype(np.float32)
    sm = np.ones((128, 512), np.float32)
    sm[:, ::HL] = 0.0
    c['hg_scanm'] = sm
    c['hg_blk'] = (s_[:, None] // HL == np.arange(128 // HL)[None, :]).astype(np.float32)
    NEG = -30000.0
    m1 = np.zeros((128, 256), np.float32)
    m1[:64, 192:256] = NEG
    m1[64:, 0:64] = NEG
    m0 = m1.copy()
    m0[:, 0:128] = NEG
    c['swa_mask'] = np.stack([m0, m1], 1)
    return c


CONST_NAMES = ['cosT', 'sinT', 'pm', 'ident', 'ret_dt', 'ret_qd', 'ret_kd', 'hg_bm', 'hg_scanm', 'hg_blk', 'swa_mask']
W_SPECS = [
    ('ln_in_g', [D]), ('ln_in_b', [D]), ('w_in', [DEPTH, D, N_IN]), ('ret_w_out', [DEPTH, D, D]),
    ('hgrn_lower_bounds', [DEPTH, D]), ('hgrn_norm_g', [DEPTH, 128]), ('hgrn_w_out', [DEPTH, D, D]),
    ('swa_sinks', [DEPTH, 16]), ('swa_w_out', [DEPTH, D, D]), ('w_o', [DEPTH, D, D]),
    ('ln_mix_g', [DEPTH, D]), ('ln_mix_b', [DEPTH, D]),
    ('ffn_w_gate', [1, D, D_FF]), ('ffn_w_up', [1, D, D_FF]), ('ffn_w_down', [1, D_FF, D]),
    ('moe_router', [1, D, NEXP]), ('moe_w_gate', [1, NEXP, D, D_EXP]), ('moe_w_up', [1, NEXP, D, D_EXP]),
    ('moe_w_down', [1, NEXP, D_EXP, D]), ('ln_ffn_g', [DEPTH, D]), ('ln_ffn_b', [DEPTH, D]),
]


def build(stop_after=None, debug=False):
    C = _consts()
    nc = bass.Bass("TRN2", target_bir_lowering=False)
    G = Ctx()
    G.nc = nc
    G.x = nc.dram_tensor("x", [S, D], F32, kind="ExternalInput").ap()
    G.W = {}
    for name, shp in W_SPECS:
        G.W[name] = nc.dram_tensor(name, shp, F32, kind="ExternalInput").ap()
    G.K = {}
    for name in CONST_NAMES:
        G.K[name] = nc.dram_tensor("c_" + name, list(C[name].shape), F32, kind="ExternalInput").ap()
    G.out = nc.dram_tensor("out", [S, D], F32, kind="ExternalOutput").ap()
    skind = "ExternalOutput" if debug else "Internal"
    G.h_res = nc.dram_tensor("h_res", [S, D], F32, kind=skind).ap()
    G.xT = {b: nc.dram_tensor("x%sT" % b, [D, S], BF16, kind=skind).ap() for b in 'abc'}
    G.mgT = nc.dram_tensor("mgT", [D, S], BF16, kind=skind).ap()
    G.cd = C['ret_cd']

    with contextlib.ExitStack() as es:
        uid = [0]

        def sb(name, shape, dt, st=es):
            uid[0] += 1
            return st.enter_context(nc.sbuf_tensor("%s_u%d" % (name, uid[0]), shape, dt))
        sems = {e: es.enter_context(nc.semaphore("s_" + e)) for e in COMPUTE}
        dsems = {q: [es.enter_context(nc.semaphore("d_%s%d" % (q, i))) for i in range(NDMASEM)]
                 for q in ('sp', 'pool')}
        P = Prog(nc, sems, dsems, same_engine_sync=SAME_SYNC)
        G.P = P
        G.sb = sb
        G.hT = sb("hT", [128, KC, S], BF16)
        G.ident_f = sb("ident_f", [128, 128], F32)
        G.ident_b = sb("ident_b", [128, 128], BF16)
        G.ws = [sb("ws%d" % i, [128, KC, 512], BF16) for i in range(4)]
        G.ws_rr = 0
        G.xt_rr = 0
        G.small = sb("small", [128, 64], F32)
        G.ps = [es.enter_context(nc.psum_tensor("ps%d" % i, [128, 512], F32)) for i in range(8)]
        G.ps_rr = 0
        G.gate_all = sb("gate_all", [128, NT, NEXP], F32)

        phases = []
        phases.append(('ln_in', lambda: phase_ln_in(G)))
        for l in range(DEPTH):
            phases.append(('ret%d' % l, lambda l=l: phase_ret(G, l)))
            phases.append(('hgrn%d' % l, lambda l=l: phase_hgrn(G, l)))
            phases.append(('swa%d' % l, lambda l=l: phase_swa(G, l)))
            phases.append(('merge%d' % l, lambda l=l: phase_merge(G, l)))
            phases.append(('wo%d' % l, lambda l=l: phase_wo(G, l)))
            phases.append(('ffn%d' % l, lambda l=l: phase_ffn(G, l)))
        for i, (name, fn) in enumerate(phases):
            if G_SKIP and name.rstrip('0123456789') in G_SKIP and name != stop_after:
                continue
            fn()
            lastp = (i == len(phases) - 1) or (name == stop_after)
            if not lastp and not G_SKIP:
                nxt = phases[i + 1][0]
                ln_ = int(nxt[-1]) if nxt[-1].isdigit() else 0
                kind = nxt.rstrip('0123456789')
                w_in_n = G.W['w_in'][ln_]
                if kind == 'ret':
                    prefetch_w(G, ('rv', ln_, 0), w_in_n, OFF['rv'], 512)
                elif kind == 'hgrn':
                    prefetch_w(G, ('hi', ln_, 0), w_in_n, OFF['hi'], 512)
                elif kind == 'swa':
                    prefetch_w(G, ('sq', ln_, 0), w_in_n, OFF['sq'], 512)
                elif kind == 'wo':
                    prefetch_w(G, ('wo', ln_), G.W['w_o'][ln_], 0, 512)
                elif kind == 'ffn':
                    if ln_ % 2 == 1:
                        prefetch_w(G, ('ffn', ln_, 0, 0), G.W['moe_w_gate'][ln_ // 2][0], 0, 512)
                    else:
                        prefetch_w(G, ('ffn', ln_, None, 0), G.W['ffn_w_gate'][ln_ // 2], 0, 512)
            P.emit(final_wait=lastp)
            if lastp:
                break
    return nc


def next_ps(G):
    i = G.ps_rr % len(G.ps_sel)
    G.ps_rr = (i + 1) % len(G.ps_sel)
    j = G.ps_sel[i]
    return G.ps[j], ('ps', j)


def next_ws(G):
    i = G.ws_rr
    G.ws_rr = (i + 1) % len(G.ws)
    return G.ws[i], ('ws', i)


def load_w(G, wdram, c0, ncols, q='pool', tag=None):
    pf = getattr(G, 'prefetched', None)
    if tag is not None and pf is not None and pf[0] == tag:
        G.prefetched = None
        return pf[1], pf[2]
    wt, key = next_ws(G)
    G.P.dma(q, wt[:, :, 0:ncols], wdram[:, c0:c0 + ncols].rearrange("(k p) c -> p k c", p=128), w=[key])
    return wt, key


def prefetch_w(G, tag, wdram, c0, ncols):
    if len(G.ws) != 4:
        return
    wt, key = load_w(G, wdram, c0, ncols)
    G.prefetched = (tag, wt, key)


def fm_group(G, wt, wkey, cc, tg, rkeys=('hT',), src=None, ncols_tok=512, t0=None):
    src = G.hT if src is None else src
    ps, pk = next_ps(G)
    t0 = tg * 512 if t0 is None else t0

    def fn(e, ps=ps, wt=wt, cc=cc, t0=t0, src=src):
        ins = None
        for k in range(KC):
            ins = e.matmul(ps[:, 0:ncols_tok], wt[:, k, cc * 128:(cc + 1) * 128], src[:, k, t0:t0 + ncols_tok],
                           start=(k == 0), stop=(k == KC - 1))
        return ins
    G.P.op('pe', fn, r=[wkey] + list(rkeys), w=[pk])
    return ps, pk


def tm_group(G, wt, wkey, tile, ncols=512, rkeys=('hT',), src=None):
    src = G.hT if src is None else src
    ps, pk = next_ps(G)

    def fn(e, ps=ps, wt=wt, tile=tile, src=src):
        ins = None
        for k in range(KC):
            ins = e.matmul(ps[:, 0:ncols], src[:, k, tile * 128:(tile + 1) * 128], wt[:, k, 0:ncols],
                           start=(k == 0), stop=(k == KC - 1))
        return ins
    G.P.op('pe', fn, r=[wkey] + list(rkeys), w=[pk])
    return ps, pk


def load_consts_common(G):
    P = G.P
    P.dma('sp', G.ident_f[:, :], G.K['ident'], w=['ident_f'])
    P.dma('pool', G.ident_b[:, :], G.K['ident'], w=['ident_b'])


def rr(gens, width):
    active = []
    it = iter(gens)
    done = False
    while True:
        while len(active) < width and not done:
            try:
                active.append(next(it))
            except StopIteration:
                done = True
        if not active:
            break
        for g in list(active):
            try:
                next(g)
            except StopIteration:
                active.remove(g)


def ln_tile(G, xt, xkey, tile, slot=0, out_final=None, pss=None):
    P = G.P
    sm = G.small
    o = slot * 16
    st = sm[:, o:o + 12]
    mv = sm[:, o + 12:o + 14]
    rs = sm[:, o + 14:o + 15]
    k = lambda n: (n, slot)
    P.op('dve', lambda e: e.bn_stats(st[:, 0:6], xt[:, 0:512]), r=[xkey], w=[k('ln_st0')])
    P.op('dve', lambda e: e.bn_stats(st[:, 6:12], xt[:, 512:1024]), r=[xkey], w=[k('ln_st1')])
    yield
    P.op('dve', lambda e: e.bn_aggr(mv, st), r=[k('ln_st0'), k('ln_st1')], w=[k('ln_mv')])
    yield
    P.op('act', lambda e: e.activation(rs, mv[:, 1:2], AF.Sqrt, bias=LN_EPS, scale=1.0), r=[k('ln_mv')], w=[k('ln_rs')])
    yield
    P.op('dve', lambda e: e.reciprocal(rs, rs), r=[k('ln_rs')], w=[k('ln_rs')])
    yield
    P.op('dve', lambda e: e.scalar_tensor_tensor(xt[:, :], xt[:, :], mv[:, 0:1], G.gbc[:, :], ALU.subtract, ALU.mult),
         r=[xkey, k('ln_mv'), 'gbc'], w=[xkey])
    yield
    P.op('dve', lambda e: e.scalar_tensor_tensor(xt[:, :], xt[:, :], rs, G.bbc[:, :], ALU.mult, ALU.add),
         r=[xkey, k('ln_rs'), 'bbc'], w=[xkey])
    yield
    rows = slice(tile * 128, (tile + 1) * 128)
    if out_final is not None:
        P.dma('sp', out_final[rows, :], xt[:, :], r=[xkey], w=[('out', tile)])
    else:
        P.dma('sp', G.h_res[rows, :], xt[:, :], r=[xkey], w=[('h_res', tile)])
        for hb in range(2):
            ps, pk = next_ps(G)

            def fn(e, ps=ps, hb=hb):
                ins = None
                for i in range(4):
                    kk = hb * 4 + i
                    ins = e.matmul(ps[:, i * 128:(i + 1) * 128], xt[:, kk * 128:(kk + 1) * 128], G.ident_f[:, :],
                                   start=True, stop=True)
                return ins
            P.op('pe', fn, r=[xkey, 'ident_f'], w=[pk])
            yield
            P.op('act', lambda e, ps=ps, hb=hb: e.activation(
                G.hT[:, hb * 4:(hb + 1) * 4, tile * 128:(tile + 1) * 128],
                ps[:, :].rearrange("p (a t) -> p a t", t=128), AF.Copy), r=[pk], w=['hT'])
            if pss is not None:
                pss.append((ps, pk))
            yield


def load_ln_params(G, g_ap, b_ap):
    G.P.dma('sp', G.gbc[:, :], g_ap.partition_broadcast(128), w=['gbc'])
    G.P.dma('sp', G.bbc[:, :], b_ap.partition_broadcast(128), w=['bbc'])


def next_xt(G):
    i = G.xt_rr
    G.xt_rr = (i + 1) % len(G.xt)
    return G.xt[i], ('xt', i)


def phase_ln_in(G):
    P = G.P
    G.ps_sel = list(range(8))
    with contextlib.ExitStack() as st:
        sb = lambda n, s, d: G.sb(n, s, d, st)
        G.xt = [sb("i_xt%d" % i, [128, D], F32) for i in range(LN_W + 1)]
        G.gbc = sb("i_gbc", [128, D], F32)
        G.bbc = sb("i_bbc", [128, D], F32)
        load_consts_common(G)
        load_ln_params(G, G.W['ln_in_g'], G.W['ln_in_b'])
        def unit(t):
            xt, xk = next_xt(G)
            P.dma('sp', xt[:, :], G.x[t * 128:(t + 1) * 128, :], w=[xk])
            yield
            yield from ln_tile(G, xt, xk, t, slot=t % LN_W)
        rr((unit(t) for t in range(NT)), LN_W)


def phase_ret(G, l):
    P = G.P
    nc = G.nc
    w_in = G.W['w_in'][l]
    G.ps_sel = [0, 1]
    with contextlib.ExitStack() as st:
        sb = lambda n, s, d: G.sb(n, s, d, st)
        v_tok = sb("r_v", [128, NT, 512], BF16)
        g_tok = sb("r_g", [128, NT, 512], BF16)
        qT = sb("r_q", [128, 2, S], BF16)
        kT = sb("r_k", [128, 2, S], BF16)
        cs = [sb("r_cs%d" % i, [128, 2, 512], F32) for i in range(2)]
        xf = [sb("r_xf%d" % i, [128, 512], F32) for i in range(2)]
        t1 = [sb("r_t1%d" % i, [128, 512], F32) for i in range(2)]
        pm = sb("r_pm", [128, 128], F32)
        dtm = sb("r_dt", [128, 4, 128], F32)
        qdm = sb("r_qd", [128, 4, 128], F32)
        kdm = sb("r_kd", [128, 4], F32)
        scm = [sb("r_scm%d" % i, [128, 128], BF16) for i in range(2)]
        qd = [sb("r_qdd%d" % i, [128, 128], BF16) for i in range(2)]
        kd = [sb("r_kdd%d" % i, [128, 128], BF16) for i in range(2)]
        stf = [sb("r_stf%d" % i, [128, 256], F32) for i in range(2)]
        stb = [sb("r_stb%d" % i, [128, 256], BF16) for i in range(2)]
        xg = [sb("r_xg%d" % i, [128, 512], F32) for i in range(2)]
        xo = [sb("r_xo%d" % i, [128, 4, 128], BF16) for i in range(2)]
        ss = sb("r_ss", [128, 4], F32)
        junk = sb("r_junk", [128, 256], F32)
        ps_o = [G.ps[2], G.ps[3]]
        ps_s = [G.ps[4], G.ps[5]]
        ps_m = [G.ps[6], G.ps[7]]
        P.dma('sp', pm[:, :], G.K['pm'], w=['pm'])
        P.dma('sp', dtm[:, :, :], G.K['ret_dt'], w=['dtm'])
        P.dma('sp', qdm[:, :, :], G.K['ret_qd'], w=['qdm'])
        P.dma('sp', kdm[:, :], G.K['ret_kd'], w=['kdm'])
        for hp in range(2):
            G.ps_sel = list(range(8))
            wt, wk = load_w(G, w_in, OFF['rv'] + hp * 512, 512, tag=('rv', l, hp))
            for t in range(NT):
                ps, pk = tm_group(G, wt, wk, t)
                P.op('act', lambda e, ps=ps, t=t: e.activation(v_tok[:, t, :], ps[:, :], AF.Copy), r=[pk], w=[('v', t)])
            wt, wk = load_w(G, w_in, OFF['rg'] + hp * 512, 512)
            for t in range(NT):
                ps, pk = tm_group(G, wt, wk, t)
                P.op('act', lambda e, ps=ps, t=t: e.activation(g_tok[:, t, :], ps[:, :], AF.Silu), r=[pk], w=[('g', t)])
            for (nm, dst) in (('rq', qT), ('rk', kT)):
                wt, wk = load_w(G, w_in, OFF[nm] + hp * 256, 256)
                for tg in range(4):
                    ci = tg % 2
                    P.dma('sp', cs[ci][:, 0, :], G.K['cosT'][:, tg * 512:(tg + 1) * 512], w=[('cs', ci, 0)])
                    P.dma('sp', cs[ci][:, 1, :], G.K['sinT'][:, tg * 512:(tg + 1) * 512], w=[('cs', ci, 1)])
                    for j in range(2):
                        bi = (tg * 2 + j) % 2
                        ps, pk = fm_group(G, wt, wk, j, tg)
                        P.op('act', lambda e, ps=ps, bi=bi: e.activation(xf[bi][:, :], ps[:, :], AF.Copy), r=[pk], w=[('xf', bi)])
                        ps2, pk2 = next_ps(G)
                        P.op('pe', lambda e, ps2=ps2, bi=bi: e.matmul(ps2[:, :], pm[:, :], xf[bi][:, :], start=True, stop=True),
                             r=[('xf', bi), 'pm'], w=[pk2])
                        P.op('dve', lambda e, bi=bi, ci=ci: e.tensor_tensor(t1[bi][:, :], xf[bi][:, :], cs[ci][:, 0, :], ALU.mult),
                             r=[('xf', bi), ('cs', ci, 0)], w=[('t1', bi)])
                        P.op('dve', lambda e, ps2=ps2, bi=bi, ci=ci: e.tensor_tensor(xf[bi][:, :], ps2[:, :], cs[ci][:, 1, :], ALU.mult),
                             r=[pk2, ('cs', ci, 1)], w=[('xf', bi)])
                        P.op('dve', lambda e, bi=bi, dst=dst, j=j, tg=tg: e.tensor_tensor(
                            dst[:, j, tg * 512:(tg + 1) * 512], t1[bi][:, :], xf[bi][:, :], ALU.add),
                            r=[('t1', bi), ('xf', bi)], w=[(nm, j, tg)])
            G.ps_sel = [0, 1]
            for n in range(NT):
                pso = ps_o[n % 2]
                pok = ('ps', 2 + n % 2)
                tgk = n // 4
                cols = slice(n * 128, (n + 1) * 128)
                for j in range(2):
                    h = hp * 2 + j
                    psm = ps_m[j]
                    pmk = ('ps', 6 + j)
                    pss_ = ps_s[j]
                    psk = ('ps', 4 + j)
                    P.op('pe', lambda e, pss_=pss_, j=j, cols=cols: e.matmul(pss_[:, 0:128], kT[:, j, cols], qT[:, j, cols], start=True, stop=True),
                         r=[('rk', j, tgk), ('rq', j, tgk)], w=[psk])
                    P.op('dve', lambda e, pss_=pss_, j=j, h=h: e.tensor_tensor(scm[j][:, :], pss_[:, 0:128], dtm[:, h, :], ALU.mult),
                         r=[psk, 'dtm'], w=[('scm', j)])
                    if n > 0:
                        P.op('dve', lambda e, j=j, h=h, cols=cols: e.tensor_tensor(qd[j][:, :], qT[:, j, cols], qdm[:, h, :], ALU.mult),
                             r=[('rq', j, tgk), 'qdm'], w=[('qd', j)])

                    def fo(e, pso=pso, j=j, n=n):
                        ins = e.matmul(pso[:, j * 256:(j + 1) * 256], scm[j][:, :], v_tok[:, n, j * 256:(j + 1) * 256],
                                       start=True, stop=(n == 0))
                        if n > 0:
                            ins = e.matmul(pso[:, j * 256:(j + 1) * 256], qd[j][:, :], stb[j][:, :], start=False, stop=True)
                        return ins
                    P.op('pe', fo, r=[('scm', j), ('v', n), ('qd', j), ('stb', j)], w=[pok])
                    if n < NT - 1:
                        P.op('pe', lambda e, psm=psm, j=j, cols=cols: e.matmul(psm[:, 0:128], kT[:, j, cols], G.ident_b[:, :], start=True, stop=True),
                             r=[('rk', j, tgk), 'ident_b'], w=[pmk])
                        P.op('act', lambda e, psm=psm, j=j, h=h: e.activation(kd[j][:, :], psm[:, 0:128], AF.Copy, scale=kdm[:, h:h + 1]),
                             r=[pmk, 'kdm'], w=[('kd', j)])
                        P.op('pe', lambda e, psm=psm, j=j, n=n: e.matmul(psm[:, 128:384], kd[j][:, :], v_tok[:, n, j * 256:(j + 1) * 256], start=True, stop=True),
                             r=[('kd', j), ('v', n)], w=[pmk])
                        if n == 0:
                            P.op('dve', lambda e, psm=psm, j=j: e.tensor_copy(stf[j][:, :], psm[:, 128:384]), r=[pmk], w=[('stf', j)])
                        else:
                            P.op('dve', lambda e, psm=psm, j=j, h=h: e.scalar_tensor_tensor(
                                stf[j][:, :], stf[j][:, :], G.cd[h], psm[:, 128:384], ALU.mult, ALU.add),
                                r=[pmk, ('stf', j)], w=[('stf', j)])
                        P.op('act', lambda e, j=j: e.activation(stb[j][:, :], stf[j][:, :], AF.Copy), r=[('stf', j)], w=[('stb', j)])
                xi = n % 2
                for j in range(2):
                    P.op('act', lambda e, pso=pso, j=j: e.activation(junk[:, :], pso[:, j * 256:(j + 1) * 256], AF.Square, accum_out=ss[:, j:j + 1]),
                         r=[pok], w=['junk', ('ss', j)])
                P.op('act', lambda e: e.activation(ss[:, 2:4], ss[:, 0:2], AF.Sqrt, bias=RMS_EPS, scale=1.0 / 256.0),
                     r=[('ss', 0), ('ss', 1)], w=['rstd'])
                P.op('dve', lambda e: e.reciprocal(ss[:, 2:4], ss[:, 2:4]), r=['rstd'], w=['rstd'])
                for j in range(2):
                    P.op('dve', lambda e, pso=pso, j=j, n=n, xi=xi: e.scalar_tensor_tensor(
                        xg[xi][:, j * 256:(j + 1) * 256], pso[:, j * 256:(j + 1) * 256], ss[:, 2 + j:3 + j],
                        g_tok[:, n, j * 256:(j + 1) * 256], ALU.mult, ALU.mult),
                        r=[pok, 'rstd', ('g', n)], w=[('xg', xi, j)])
                ps, pk = next_ps(G)

                def ft(e, ps=ps, xi=xi):
                    ins = None
                    for i in range(4):
                        ins = e.matmul(ps[:, i * 128:(i + 1) * 128], xg[xi][:, i * 128:(i + 1) * 128], G.ident_f[:, :], start=True, stop=True)
                    return ins
                P.op('pe', ft, r=[('xg', xi, 0), ('xg', xi, 1), 'ident_f'], w=[pk])
                P.op('act', lambda e, ps=ps, xi=xi: e.activation(xo[xi][:, :, :], ps[:, :].rearrange("p (a t) -> p a t", t=128), AF.Copy),
                     r=[pk], w=[('xo', xi)])
                P.dma('sp', G.xT['a'][hp * 512:(hp + 1) * 512, cols].rearrange("(i p) t -> p i t", p=128), xo[xi][:, :, :],
                      r=[('xo', xi)], w=[('xaT', hp, n)])


def phase_hgrn(G, l):
    P = G.P
    w_in = G.W['w_in'][l]
    G.ps_sel = [0, 1]
    with contextlib.ExitStack() as st:
        sb = lambda n, s, d: G.sb(n, s, d, st)
        v_tok = sb("h_v", [128, NT, 512], BF16)
        qtT = sb("h_qt", [128, 4, S], BF16)
        ktT = sb("h_kt", [128, 4, S], BF16)
        ksT = sb("h_ks", [128, 4, S], BF16)
        sgT = sb("h_sg", [128, 4, S], BF16)
        bdT = sb("h_bd", [128, 4, 64], F32)
        T = [[sb("h_t%d_%d" % (a, i), [128, 512], F32) for i in range(5)] for a in range(2)]
        lbr = sb("h_lbr", [128, 2, 8], F32)
        lbt = sb("h_lb", [128, 8], F32)
        oml = sb("h_oml", [128, 8], F32)
        gn = sb("h_gn", [128, 1], F32)
        bm = sb("h_bm", [128, 128], F32)
        scanm = sb("h_scanm", [128, 512], F32)
        ones_f = sb("h_ones", [128, 128], BF16)
        am = [[sb("h_am%d_%d" % (a, j), [128, 128], BF16) for j in range(4)] for a in range(2)]
        kst = [[sb("h_kst%d_%d" % (a, j), [128, 128], BF16) for j in range(4)] for a in range(2)]
        stf = [sb("h_stf%d" % j, [128, 128], F32) for j in range(4)]
        stb = [[sb("h_stb%d_%d" % (j, a), [128, 128], BF16) for a in range(2)] for j in range(4)]
        osb = [sb("h_osb%d" % a, [128, 512], F32) for a in range(2)]
        sq = [sb("h_sq%d" % a, [128, 512], BF16) for a in range(2)]
        rt = [sb("h_rt%d" % a, [128, 512], F32) for a in range(2)]
        xo = [sb("h_xo%d" % i, [128, 4, 128], BF16) for i in range(2)]
        blkm = sb("h_blkm", [128, 128 // HL], F32)
        vblk = [[sb("h_vblk%d_%d" % (a, j), [128, 128 // HL, 128], BF16) for j in range(4)] for a in range(2)]
        psU = [G.ps[2], G.ps[3], G.ps[6], G.ps[7]]
        P.dma('sp', blkm[:, :], G.K['hg_blk'], w=['blkm'])
        P.dma('sp', bm[:, :], G.K['hg_bm'], w=['bm'])
        P.dma('sp', scanm[:, :], G.K['hg_scanm'], w=['scanm'])
        P.dma('sp', gn[:, :], G.W['hgrn_norm_g'][l].rearrange("(p o) -> p o", o=1), w=['gn'])
        P.op('pool', lambda e: e.memset(ones_f[:, :], 1.0), w=['ones_f'])
        if l == 0:
            P.op('pool', lambda e: e.memset(lbt[:, :], 0.0), w=['lbt'])
        else:
            for a in range(2):
                P.dma('sp', lbr[:, a, :], G.W['hgrn_lower_bounds'][a].rearrange("(h d) -> d h", d=128), w=[('lbr', a)],
                      allow_slow_non_contiguous=True)
            P.op('dve', lambda e: e.tensor_tensor(lbt[:, :], lbr[:, 1, :], lbr[:, 0, :], ALU.subtract), r=[('lbr', 0), ('lbr', 1)], w=['lbt'])
            P.op('act', lambda e: e.activation(lbt[:, :], lbt[:, :], AF.Sigmoid), r=['lbt'], w=['lbt'])
        P.op('dve', lambda e: e.tensor_scalar(oml[:, :], lbt[:, :], -1.0, 1.0, ALU.mult, ALU.add), r=['lbt'], w=['oml'])
        NB = 128 // HL
        for hg in range(2):
            G.ps_sel = [0, 1, 2, 3, 6, 7]
            wt, wk = load_w(G, w_in, OFF['hi'] + hg * 512, 512, tag=('hi', l, hg))
            for t in range(NT):
                ps, pk = tm_group(G, wt, wk, t)
                P.op('act', lambda e, ps=ps, t=t: e.activation(v_tok[:, t, :], ps[:, :], AF.Copy), r=[pk], w=[('v', t)])
            wq, wqk = load_w(G, w_in, OFF['hq'] + hg * 512, 512)
            wf, wfk = load_w(G, w_in, OFF['hf'] + hg * 512, 512)
            wg, wgk = load_w(G, w_in, OFF['hg'] + hg * 512, 512)

            def h2unit(j, tg, a):
                h = hg * 4 + j
                t1, t2, t3, t4, t5 = T[a]
                tk = lambda i, a=a: ('T', a, i)
                cols = slice(tg * 512, (tg + 1) * 512)
                psz, pkz = fm_group(G, wf, wfk, j, tg)
                yield
                P.op('act', lambda e: e.activation(t2[:, :], psz[:, :], AF.Exp, scale=-1.0), r=[pkz], w=[tk(2)])
                yield
                P.op('act', lambda e: e.activation(t1[:, :], t2[:, :], AF.Ln, bias=1.0, scale=1.0), r=[tk(2)], w=[tk(1)])
                yield
                P.op('act', lambda e: e.activation(t1[:, :], t1[:, :], AF.Exp, scale=-1.0), r=[tk(1)], w=[tk(1)])
                yield
                P.op('dve', lambda e: e.tensor_tensor(t2[:, :], t2[:, :], t1[:, :], ALU.mult), r=[tk(1), tk(2)], w=[tk(2)])
                psq, pkq = fm_group(G, wq, wqk, j, tg)
                yield
                P.op('dve', lambda e: e.tensor_scalar(t1[:, :], t1[:, :], oml[:, h:h + 1], lbt[:, h:h + 1], ALU.mult, ALU.add),
                     r=[tk(1), 'oml', 'lbt'], w=[tk(1)])
                yield
                P.op('act', lambda e: e.activation(t3[:, :], t1[:, :], AF.Ln), r=[tk(1)], w=[tk(3)])
                yield
                P.op('dve', lambda e: e.tensor_tensor_scan(t4[:, :], scanm[:, :], t3[:, :], 0.0, ALU.mult, ALU.add),
                     r=[tk(3), 'scanm'], w=[tk(4)])
                yield
                P.op('act', lambda e: e.activation(t1[:, :], t4[:, :], AF.Exp), r=[tk(4)], w=[tk(1)])
                P.op('act', lambda e: e.activation(t3[:, :], t4[:, :], AF.Exp, scale=-1.0), r=[tk(4)], w=[tk(3)])
                yield
                P.op('dve', lambda e: e.tensor_tensor(qtT[:, j, cols], psq[:, :], t1[:, :], ALU.mult),
                     r=[pkq, tk(1)], w=[('qt', j, tg)])
                yield
                P.op('dve', lambda e: e.scalar_tensor_tensor(t2[:, :], t2[:, :], oml[:, h:h + 1], t3[:, :], ALU.mult, ALU.mult),
                     r=[tk(2), tk(3), 'oml'], w=[tk(2)])
                yield
                P.op('act', lambda e: e.activation(ktT[:, j, cols], t2[:, :], AF.Copy), r=[tk(2)], w=[('kt', j, tg)])
                yield
                P.op('dve', lambda e: e.tensor_tensor(
                    ksT[:, j, cols].rearrange("p (b l) -> p b l", l=HL),
                    t2[:, :].rearrange("p (b l) -> p b l", l=HL),
                    t1[:, :].rearrange("p (b l) -> p b l", l=HL)[:, :, HL - 1:HL].broadcast_to([128, 512 // HL, HL]), ALU.mult),
                    r=[tk(1), tk(2)], w=[('ks', j, tg)])
                yield
                nb = 512 // HL
                P.op('dve', lambda e: e.tensor_copy(
                    bdT[:, j, tg * nb:(tg + 1) * nb],
                    t1[:, :].rearrange("p (b l) -> p b l", l=HL)[:, :, HL - 1:HL].rearrange("p b o -> p (b o)")),
                    r=[tk(1)], w=[('bd', j, tg)])
                yield
            for j in range(4):
                for tg in range(4):
                    psg, pkg = fm_group(G, wg, wgk, j, tg)
                    P.op('act', lambda e, psg=psg, j=j, tg=tg: e.activation(sgT[:, j, tg * 512:(tg + 1) * 512], psg[:, :], AF.Silu), r=[pkg], w=[('sg', j, tg)])
            units = [(j, tg) for j in range(4) for tg in range(4)]
            rr((h2unit(j, tg, n % 2) for n, (j, tg) in enumerate(units)), 2)
            if HG_LEVEL < 2:
                continue
            G.ps_sel = [0, 1]
            psok = [('ps', 4), ('ps', 5)]
            psUk = [('ps', 2), ('ps', 3), ('ps', 6), ('ps', 7)]

            def front(i):
                tg = i // 4
                cols = slice(i * 128, (i + 1) * 128)
                a = i % 2
                for j in range(4):
                    ps, pk = next_ps(G)
                    P.op('pe', lambda e, ps=ps, j=j: e.matmul(ps[:, 0:128], ktT[:, j, cols], qtT[:, j, cols], start=True, stop=True),
                         r=[('kt', j, tg), ('qt', j, tg)], w=[pk])
                    P.op('dve', lambda e, ps=ps, j=j: e.tensor_tensor(am[a][j][:, :], ps[:, 0:128], bm[:, :], ALU.mult), r=[pk, 'bm'], w=[('am', a, j)])
                    ps, pk = next_ps(G)
                    P.op('pe', lambda e, ps=ps, j=j: e.matmul(ps[:, 128:256], ksT[:, j, cols], G.ident_b[:, :], start=True, stop=True),
                         r=[('ks', j, tg), 'ident_b'], w=[pk])
                    P.op('act', lambda e, ps=ps, j=j: e.activation(kst[a][j][:, :], ps[:, 128:256], AF.Copy), r=[pk], w=[('kst', a, j)])
                    P.op('dve', lambda e, j=j: e.tensor_tensor(
                        vblk[a][j][:, :, :], v_tok[:, i, j * 128:(j + 1) * 128].rearrange("p (o v) -> p o v", o=1).broadcast_to([128, NB, 128]),
                        blkm[:, :].rearrange("p (b o) -> p b o", o=1).broadcast_to([128, NB, 128]), ALU.mult),
                        r=[('v', i), 'blkm'], w=[('vblk', a, j)])

            def umat(i):
                a = i % 2
                for j in range(4):
                    P.op('pe', lambda e, j=j: e.matmul(psU[j][:, :], kst[a][j][:, :], vblk[a][j][:, :, :].rearrange("p b v -> p (b v)"), start=True, stop=True),
                         r=[('kst', a, j), ('vblk', a, j)], w=[psUk[j]])

            def mid(i):
                tg = i // 4
                a = i % 2
                pso = G.ps[4 + a]
                pok = psok[a]
                if HG_MERGE:
                    for j in range(4):
                        P.op('pe', lambda e, j=j: e.matmul(pso[:, j * 128:(j + 1) * 128], v_tok[:, i, j * 128:(j + 1) * 128], am[a][j][:, :],
                                                           start=True, stop=False, skip_group_check=True),
                             r=[('v', i), ('am', a, j)], w=[pok])
                for b in range(NB):
                    gb = i * NB + b
                    for j in range(4):
                        def fo(e, j=j, b=b, gb=gb):
                            oc = pso[:, j * 128 + b * HL: j * 128 + (b + 1) * HL]
                            ins = None
                            if not HG_MERGE:
                                ins = e.matmul(oc, v_tok[:, i, j * 128:(j + 1) * 128], am[a][j][:, b * HL:(b + 1) * HL], start=True, stop=(gb == 0))
                            if gb > 0:
                                ins = e.matmul(oc, stb[j][gb % 2][:, :], qtT[:, j, i * 128 + b * HL: i * 128 + (b + 1) * HL], start=False, stop=True,
                                               skip_group_check=HG_MERGE)
                            return ins
                        if HG_MERGE and gb == 0:
                            continue
                        P.op('pe', fo, r=[('v', i), ('am', a, j), ('stb', j, gb % 2), ('qt', j, tg)], w=[pok])
                    if gb < S // HL - 1:
                        for j in range(4):
                            if gb == 0:
                                P.op('dve', lambda e, j=j, b=b: e.tensor_copy(stf[j][:, :], psU[j][:, b * 128:(b + 1) * 128]), r=[psUk[j]], w=[('stf', j)])
                            else:
                                P.op('dve', lambda e, j=j, gb=gb, b=b: e.scalar_tensor_tensor(
                                    stf[j][:, :], stf[j][:, :], bdT[:, j, gb:gb + 1], psU[j][:, b * 128:(b + 1) * 128], ALU.mult, ALU.add),
                                    r=[psUk[j], ('stf', j), ('bd', j, gb // (512 // HL))], w=[('stf', j)])
                            P.op('act', lambda e, j=j, gb=gb: e.activation(stb[j][(gb + 1) % 2][:, :], stf[j][:, :], AF.Copy),
                                 r=[('stf', j)], w=[('stb', j, (gb + 1) % 2)])

            def back(i):
                tg = i // 4
                cols = slice(i * 128, (i + 1) * 128)
                a = i % 2
                pso = G.ps[4 + a]
                pok = psok[a]
                osb_, sq_, rt_ = osb[a], sq[a], rt[a]
                P.op('act', lambda e: e.activation(osb_[:, :], pso[:, :], AF.Copy), r=[pok], w=[('osb', a)])
                P.op('act', lambda e: e.activation(sq_[:, :], pso[:, :], AF.Square), r=[pok], w=[('sq', a)])
                ps, pk = next_ps(G)
                P.op('pe', lambda e, ps=ps: e.matmul(ps[:, :], ones_f[:, :], sq_[:, :], start=True, stop=True), r=[('sq', a), 'ones_f'], w=[pk])
                P.op('act', lambda e, ps=ps: e.activation(rt_[:, :], ps[:, :], AF.Ln, bias=RMS_EPS, scale=1.0 / 128.0), r=[pk], w=[('rt', a)])
                P.op('act', lambda e: e.activation(rt_[:, :], rt_[:, :], AF.Exp, scale=-0.5), r=[('rt', a)], w=[('rt', a)])
                P.op('dve', lambda e: e.scalar_tensor_tensor(osb_[:, :], osb_[:, :], gn[:, 0:1], rt_[:, :], ALU.mult, ALU.mult), r=[('osb', a), ('rt', a), 'gn'], w=[('osb', a)])
                P.op('dve', lambda e: e.tensor_tensor(
                    xo[a][:, :, :], osb_[:, :].rearrange("p (a t) -> p a t", t=128), sgT[:, :, cols], ALU.mult),
                    r=[('osb', a)] + [('sg', j, tg) for j in range(4)], w=[('xo', a)])
                P.dma('sp', G.xT['b'][hg * 512:(hg + 1) * 512, cols].rearrange("(i p) t -> p i t", p=128), xo[a][:, :, :],
                      r=[('xo', a)], w=[('xbT', hg, i)])

            front(0)
            umat(0)
            for sstep in range(NT):
                if sstep + 1 < NT:
                    front(sstep + 1)
                mid(sstep)
                if sstep + 1 < NT:
                    umat(sstep + 1)
                back(sstep)


def phase_swa(G, l):
    P = G.P
    w_in = G.W['w_in'][l]
    G.ps_sel = list(range(8))
    with contextlib.ExitStack() as st:
        sb = lambda n, s, d: G.sb(n, s, d, st)
        qT = sb("s_q", [128, 8, S], BF16)
        kT = sb("s_k", [128, 4, 2, 128 + S], BF16)
        vt = sb("s_v", [128, NT + 1, 4, 2, 128], BF16)
        mask = sb("s_mask", [128, 2, 256], F32)
        sink = sb("s_sink", [128, 16], F32)
        with contextlib.ExitStack() as st1:
            wkd = G.sb("s_wkd", [128, KC, 4, 2, 128], BF16, st1)
            P.dma('sp', mask[:, :, :], G.K['swa_mask'], w=['mask'])
            P.dma('sp', sink[:, :], G.W['swa_sinks'][l].partition_broadcast(128), w=['sink'])
            P.op('pool', lambda e: e.memset(kT[:, :, :, 0:128], 0.0), w=['kpad'])
            P.op('pool', lambda e: e.memset(vt[:, :, :, :, :].rearrange("p a b c d -> p (a b c d)"), 0.0), w=['vt0'])
            P.op('pool', lambda e: e.memset(wkd[:, :, :, :, :].rearrange("p a b c d -> p (a b c d)"), 0.0), w=['wkd0'])
            for g in range(4):
                for var in range(2):
                    P.dma('pool', wkd[:, :, g, var, var * 64:(var + 1) * 64],
                          w_in[:, OFF['sk'] + g * 64: OFF['sk'] + (g + 1) * 64].rearrange("(k p) c -> p k c", p=128),
                          r=['wkd0'], w=[('wkd', g, var)])
            for blk in range(2):
                wt, wk = load_w(G, w_in, OFF['sq'] + blk * 512, 512, tag=('sq', l, blk))
                for cc in range(4):
                    for tg in range(4):
                        ps, pk = fm_group(G, wt, wk, cc, tg)
                        P.op('act', lambda e, ps=ps, c8=blk * 4 + cc, tg=tg: e.activation(qT[:, c8, tg * 512:(tg + 1) * 512], ps[:, :], AF.Copy, scale=0.125),
                             r=[pk], w=[('q', blk * 4 + cc, tg)])
            for g in range(4):
                for var in range(2):
                    for tg in range(4):
                        ps, pk = next_ps(G)

                        def fk(e, ps=ps, g=g, var=var, tg=tg):
                            ins = None
                            for k in range(KC):
                                ins = e.matmul(ps[:, :], wkd[:, k, g, var, :], G.hT[:, k, tg * 512:(tg + 1) * 512], start=(k == 0), stop=(k == KC - 1))
                            return ins
                        P.op('pe', fk, r=[('wkd', g, var), 'hT'], w=[pk])
                        P.op('act', lambda e, ps=ps, g=g, var=var, tg=tg: e.activation(kT[:, g, var, 128 + tg * 512:128 + (tg + 1) * 512], ps[:, :], AF.Copy),
                             r=[pk, 'kpad'], w=[('k', g, var, tg)])
            wt, wk = load_w(G, w_in, OFF['sv'], 256)
            for t in range(NT):
                ps, pk = tm_group(G, wt, wk, t, ncols=256)
                P.op('act', lambda e, ps=ps, t=t: e.activation(vt[:, t + 1, :, 0, 0:64], ps[:, 0:256].rearrange("p (g d) -> p g d", d=64), AF.Copy),
                     r=[pk, 'vt0'], w=[('vt', t + 1, 0)])
                P.op('act', lambda e, ps=ps, t=t: e.activation(vt[:, t + 1, :, 1, 64:128], ps[:, 0:256].rearrange("p (g d) -> p g d", d=64), AF.Copy),
                     r=[pk, 'vt0'], w=[('vt', t + 1, 1)])
            P.emit()
        NSL = SW_W
        sms = [sb("s_sm%d" % i, [128, 4, 256], F32) for i in range(NSL)]
        ps_ = [sb("s_p%d" % i, [128, 4, 256], BF16) for i in range(NSL)]
        mxs = [sb("s_mx%d" % i, [128, 16], F32) for i in range(NSL)]
        pTs = [sb("s_pT%d" % i, [128, 8, 128], BF16) for i in range(NSL)]
        xos = [sb("s_xo%d" % i, [128, 2, 128], BF16) for i in range(NSL)]

        def s2unit(i, g, slot):
            cols = slice(i * 128, (i + 1) * 128)
            mi = 0 if i == 0 else 1
            sm, p, mx, pT, xo = sms[slot], ps_[slot], mxs[slot], pTs[slot], xos[slot]
            bk = [slot * 2, slot * 2 + 1]
            kk = lambda n: (n, slot)
            for a in range(4):
                hq = 4 * g + a
                c8, var = hq // 2, hq % 2
                bank = bk[a // 2]
                P.op('pe', lambda e, a=a, c8=c8, var=var, bank=bank: e.matmul(
                    G.ps[bank][:, (a % 2) * 256:(a % 2 + 1) * 256], qT[:, c8, cols], kT[:, g, var, i * 128:i * 128 + 256], start=True, stop=True),
                    w=[('ps', bank)])
            yield
            for hh in range(2):
                bank = bk[hh]
                P.op('dve', lambda e, hh=hh, bank=bank: e.tensor_tensor(
                    sm[:, hh * 2:(hh + 1) * 2, :], G.ps[bank][:, :].rearrange("p (a k) -> p a k", k=256),
                    mask[:, mi:mi + 1, :].broadcast_to([128, 2, 256]), ALU.add),
                    r=[('ps', bank)], w=[kk(('sm', hh))])
            yield
            P.op('dve', lambda e: e.tensor_reduce(mx[:, 0:4], sm[:, :, :], AX.X, ALU.max), r=[kk(('sm', 0)), kk(('sm', 1))], w=[kk('mx_m')])
            yield
            P.op('dve', lambda e: e.tensor_tensor(mx[:, 0:4], mx[:, 0:4], sink[:, 4 * g:4 * g + 4], ALU.max), r=[kk('mx_m')], w=[kk('mx_m')])
            yield
            P.op('dve', lambda e: e.tensor_scalar(mx[:, 4:8], mx[:, 0:4], -1.0, None, ALU.mult), r=[kk('mx_m')], w=[kk('mx_n')])
            yield
            for a in range(4):
                P.op('act', lambda e, a=a: e.activation(p[:, a, :], sm[:, a, :], AF.Exp, bias=mx[:, 4 + a:5 + a], scale=1.0, accum_out=mx[:, 8 + a:9 + a]),
                     r=[kk(('sm', a // 2)), kk('mx_n')], w=[kk('p'), kk(('mx_s', a))])
            P.op('dve', lambda e: e.tensor_tensor(mx[:, 12:16], sink[:, 4 * g:4 * g + 4], mx[:, 4:8], ALU.add), r=[kk('mx_n')], w=[kk('mx_d')])
            yield
            P.op('act', lambda e: e.activation(mx[:, 12:16], mx[:, 12:16], AF.Exp), r=[kk('mx_d')], w=[kk('mx_d')])
            yield
            P.op('dve', lambda e: e.tensor_tensor(mx[:, 12:16], mx[:, 12:16], mx[:, 8:12], ALU.add), r=[kk('mx_d')] + [kk(('mx_s', a)) for a in range(4)], w=[kk('mx_d')])
            yield
            P.op('dve', lambda e: e.reciprocal(mx[:, 12:16], mx[:, 12:16]), r=[kk('mx_d')], w=[kk('mx_d')])
            yield
            P.op('dve', lambda e: e.tensor_tensor(p[:, :, :], p[:, :, :], mx[:, 12:16].rearrange("p (a o) -> p a o", o=1).broadcast_to([128, 4, 256]), ALU.mult),
                 r=[kk('p'), kk('mx_d')], w=[kk('p')])
            yield
            for hh in range(2):
                def ftr(e, hh=hh):
                    ins = None
                    for q in range(4):
                        idx = hh * 4 + q
                        a, kt = idx // 2, idx % 2
                        ins = e.matmul(G.ps[bk[hh]][:, q * 128:(q + 1) * 128], p[:, a, kt * 128:(kt + 1) * 128], G.ident_b[:, :], start=True, stop=True)
                    return ins
                P.op('pe', ftr, r=[kk('p'), 'ident_b', kk(('sm', hh))], w=[('ps', bk[hh])])
            yield
            P.op('act', lambda e: e.activation(pT[:, 0:4, :], G.ps[bk[0]][:, :].rearrange("p (a t) -> p a t", t=128), AF.Copy), r=[('ps', bk[0])], w=[kk(('pT', 0))])
            P.op('dve', lambda e: e.tensor_copy(pT[:, 4:8, :], G.ps[bk[1]][:, :].rearrange("p (a t) -> p a t", t=128)), r=[('ps', bk[1])], w=[kk(('pT', 1))])
            yield
            for pr in range(2):
                pso = G.ps[bk[pr]]

                def fpv(e, pso=pso, pr=pr):
                    ins = None
                    n = 0
                    for (a, var) in ((2 * pr, 0), (2 * pr + 1, 1)):
                        for kt in range(2):
                            ins = e.matmul(pso[:, 0:128], vt[:, i + kt, g, var, :], pT[:, a * 2 + kt, :], start=(n == 0), stop=(n == 3))
                            n += 1
                    return ins
                P.op('pe', fpv, r=[kk(('pT', pr))], w=[('ps', bk[pr])])
                yield
                P.op('act', lambda e, pso=pso, pr=pr: e.activation(xo[:, pr, :], pso[:, 0:128], AF.Copy), r=[('ps', bk[pr])], w=[kk(('xo', pr))])
                yield
            P.dma('sp', G.xT['c'][g * 256:(g + 1) * 256, cols].rearrange("(i p) t -> p i t", p=128), xo[:, :, :],
                  r=[kk(('xo', 0)), kk(('xo', 1))], w=[('xcT', g, i)])
            yield
        units = [(i, g) for i in range(NT) for g in range(4)]
        rr((s2unit(i, g, n % NSL) for n, (i, g) in enumerate(units)), NSL)


def phase_merge(G, l):
    P = G.P
    G.ps_sel = list(range(8))
    w_in = G.W['w_in'][l]
    with contextlib.ExitStack() as st:
        sb = lambda n, s, d: G.sb(n, s, d, st)
        xs = {b: sb("m_x" + b, [128, KC, S], BF16) for b in 'abc'}
        ws_save = G.ws
        G.ws = list(G.ws) + [sb("m_ws%d" % i, [128, KC, 512], BF16) for i in range(2)]
        G.ws_rr = 0
        sg = [sb("m_sg%d" % i, [128, 512], F32) for i in range(3)]
        macc = sb("m_acc", [128, 512], F32)
        mo = [sb("m_o%d" % i, [128, 512], BF16) for i in range(2)]
        for b in 'abc':
            for k in range(KC):
                P.dma('sp', xs[b][:, k, :], G.xT[b][k * 128:(k + 1) * 128, :], w=[('x', b)] if k == 0 else [('x', b, k)])
        xkeys = {b: [('x', b)] + [('x', b, k) for k in range(1, KC)] for b in 'abc'}
        wouts = {'a': G.W['ret_w_out'][l], 'b': G.W['hgrn_w_out'][l], 'c': G.W['swa_w_out'][l]}
        goff = {'a': OFF['ga'], 'b': OFF['gb'], 'c': OFF['gc']}
        n = 0
        for cb in range(2):
            wy = {b: load_w(G, wouts[b], cb * 512, 512) for b in 'abc'}
            wg = {b: load_w(G, w_in, goff[b] + cb * 512, 512) for b in 'abc'}
            for cc in range(4):
                for tg in range(4):
                    oi = n % 2
                    n += 1
                    for bi, b in enumerate('abc'):
                        psy, pky = fm_group(G, wy[b][0], wy[b][1], cc, tg, rkeys=xkeys[b], src=xs[b])
                        psg, pkg = fm_group(G, wg[b][0], wg[b][1], cc, tg)
                        P.op('act', lambda e, psg=psg, bi=bi: e.activation(sg[bi][:, :], psg[:, :], AF.Sigmoid), r=[pkg], w=[('sg', bi)])
                        if bi == 0:
                            P.op('dve', lambda e, psy=psy, bi=bi: e.tensor_tensor(macc[:, :], sg[bi][:, :], psy[:, :], ALU.mult), r=[('sg', bi), pky], w=['macc'])
                        else:
                            P.op('dve', lambda e, psy=psy, bi=bi: e.tensor_tensor(sg[bi][:, :], sg[bi][:, :], psy[:, :], ALU.mult), r=[('sg', bi), pky], w=[('sg', bi)])
                            if bi == 1:
                                P.op('dve', lambda e, bi=bi: e.tensor_tensor(macc[:, :], macc[:, :], sg[bi][:, :], ALU.add), r=['macc', ('sg', bi)], w=['macc'])
                            else:
                                P.op('dve', lambda e, bi=bi, oi=oi: e.tensor_tensor(mo[oi][:, :], macc[:, :], sg[bi][:, :], ALU.add), r=['macc', ('sg', bi)], w=[('mo', oi)])
                    c8 = cb * 4 + cc
                    P.dma('sp', G.mgT[c8 * 128:(c8 + 1) * 128, tg * 512:(tg + 1) * 512], mo[oi][:, :], r=[('mo', oi)], w=[('mgT', c8, tg)])
        G.ws = ws_save
        G.ws_rr = 0


def router_tile(G, R, tile, pss, slot=0):
    P = G.P
    wr, hTfs, lgs, rrs = R
    hTf, lg, rr_ = hTfs[slot], lgs[slot], rrs[slot]
    k = lambda n: (n, slot)
    for hb, (ps, pk) in enumerate(pss):
        P.op('act', lambda e, ps=ps, hb=hb: e.activation(hTf[:, hb * 4:(hb + 1) * 4, :], ps[:, :].rearrange("p (a t) -> p a t", t=128), AF.Copy),
             r=[pk], w=[k(('hTf', hb))])
        yield
    ps, pk = next_ps(G)

    def fr(e, ps=ps):
        ins = None
        for kk in range(KC):
            ins = e.matmul(ps[:, 0:NEXP], hTf[:, kk, :], wr[:, kk, :], start=(kk == 0), stop=(kk == KC - 1))
        return ins
    P.op('pe', fr, r=[k(('hTf', 0)), k(('hTf', 1)), 'wr'], w=[pk])
    yield
    P.op('dve', lambda e, ps=ps: e.tensor_copy(lg[:, 0:8], ps[:, 0:NEXP]), r=[pk], w=[k('lg')])
    yield
    P.op('dve', lambda e: e.tensor_reduce(rr_[:, 0:1], lg[:, 0:8], AX.X, ALU.max), r=[k('lg')], w=[k('rr0')])
    yield
    P.op('dve', lambda e: e.tensor_scalar(lg[:, 8:16], lg[:, 0:8], rr_[:, 0:1], None, ALU.is_equal), r=[k('lg'), k('rr0')], w=[k('lg1')])
    yield
    P.op('dve', lambda e: e.scalar_tensor_tensor(lg[:, 8:16], lg[:, 8:16], -1e30, lg[:, 0:8], ALU.mult, ALU.add), r=[k('lg'), k('lg1')], w=[k('lg1')])
    yield
    P.op('dve', lambda e: e.tensor_reduce(rr_[:, 1:2], lg[:, 8:16], AX.X, ALU.max), r=[k('lg1')], w=[k('rr1')])
    yield
    P.op('dve', lambda e: e.tensor_scalar(lg[:, 8:16], lg[:, 0:8], rr_[:, 1:2], None, ALU.is_ge), r=[k('lg'), k('rr1'), k('lg1')], w=[k('lg1')])
    P.op('dve', lambda e: e.tensor_scalar(rr_[:, 2:3], rr_[:, 0:1], -1.0, None, ALU.mult), r=[k('rr0')], w=[k('rr2')])
    yield
    P.op('act', lambda e: e.activation(lg[:, 16:24], lg[:, 0:8], AF.Exp, bias=rr_[:, 2:3], scale=1.0), r=[k('lg'), k('rr2')], w=[k('lg2')])
    yield
    P.op('dve', lambda e: e.tensor_tensor(lg[:, 16:24], lg[:, 16:24], lg[:, 8:16], ALU.mult), r=[k('lg2'), k('lg1')], w=[k('lg2')])
    yield
    P.op('dve', lambda e: e.tensor_reduce(rr_[:, 3:4], lg[:, 16:24], AX.X, ALU.add), r=[k('lg2')], w=[k('rr3')])
    yield
    P.op('dve', lambda e: e.reciprocal(rr_[:, 3:4], rr_[:, 3:4]), r=[k('rr3')], w=[k('rr3')])
    yield
    P.op('dve', lambda e, tile=tile: e.tensor_scalar(G.gate_all[:, tile, :], lg[:, 16:24], rr_[:, 3:4], None, ALU.mult), r=[k('lg2'), k('rr3')], w=[('gate', tile)])
    yield


def phase_wo(G, l):
    P = G.P
    G.ps_sel = list(range(8))
    with contextlib.ExitStack() as st:
        sb = lambda n, s, d: G.sb(n, s, d, st)
        mg = sb("o_mg", [128, KC, S], BF16)
        G.xt = [sb("o_xt%d" % i, [128, D], F32) for i in range(LN_W + 1)]
        G.gbc = sb("o_gbc", [128, D], F32)
        G.bbc = sb("o_bbc", [128, D], F32)
        R = None
        if l % 2 == 1:
            wr = sb("o_wr", [128, KC, NEXP], F32)
            hTfs = [sb("o_hTf%d" % i, [128, KC, 128], F32) for i in range(LN_W)]
            lgs = [sb("o_lg%d" % i, [128, 24], F32) for i in range(LN_W)]
            rrs = [sb("o_rr%d" % i, [128, 4], F32) for i in range(LN_W)]
            R = (wr, hTfs, lgs, rrs)
            P.dma('sp', wr[:, :, :], G.W['moe_router'][l // 2].rearrange("(k p) e -> p k e", p=128), w=['wr'])
        for k in range(KC):
            P.dma('sp', mg[:, k, :], G.mgT[k * 128:(k + 1) * 128, :], w=[('mg', k)])
        mgk = [('mg', k) for k in range(KC)]
        load_ln_params(G, G.W['ln_mix_g'][l], G.W['ln_mix_b'][l])
        w0 = load_w(G, G.W['w_o'][l], 0, 512, tag=('wo', l))
        w1 = load_w(G, G.W['w_o'][l], 512, 512)

        def unit(t):
            xt, xk = next_xt(G)
            P.dma('sp', xt[:, :], G.h_res[t * 128:(t + 1) * 128, :], r=[('h_res', t)], w=[xk])
            yield
            for hf, (wt, wk) in enumerate((w0, w1)):
                ps, pk = tm_group(G, wt, wk, t, rkeys=mgk, src=mg)
                yield
                P.op('dve', lambda e, ps=ps, xt=xt, hf=hf: e.scalar_tensor_tensor(
                    xt[:, hf * 512:(hf + 1) * 512], xt[:, hf * 512:(hf + 1) * 512], DN_ALPHA, ps[:, :], ALU.mult, ALU.add), r=[pk, xk], w=[xk])
                yield
            pss = []
            yield from ln_tile(G, xt, xk, t, slot=t % LN_W, pss=pss)
            if R is not None:
                yield from router_tile(G, R, t, pss, slot=t % LN_W)
        rr((unit(t) for t in range(NT)), LN_W)


def phase_ffn(G, l):
    P = G.P
    G.ps_sel = list(range(8))
    moe = (l % 2 == 1)
    jx = l // 2
    last = (l == DEPTH - 1)
    with contextlib.ExitStack() as st:
        sb = lambda n, s, d: G.sb(n, s, d, st)
        yacc = sb("f_y", [128, NT, D], F32)
        hid = sb("f_hid", [128, 4, S], BF16)
        wds = [sb("f_wd%d" % i, [128, 4, D], BF16) for i in range(2)]
        sl = [sb("f_sl%d" % i, [128, 512], F32) for i in range(2)]
        G.xt = [sb("f_xt%d" % i, [128, D], F32) for i in range(4)]
        G.xt_rr = 0
        G.gbc = sb("f_gbc", [128, D], F32)
        G.bbc = sb("f_bbc", [128, D], F32)
        if moe:
            experts = [(G.W['moe_w_gate'][jx][e], G.W['moe_w_up'][jx][e], G.W['moe_w_down'][jx][e], e) for e in range(NEXP)]
            F = D_EXP
        else:
            experts = [(G.W['ffn_w_gate'][jx], G.W['ffn_w_up'][jx], G.W['ffn_w_down'][jx], None)]
            F = D_FF
        nblk = (F + 511) // 512
        load_ln_params(G, G.W['ln_ffn_g'][l], G.W['ln_ffn_b'][l])
        ln_active = []

        def unit(t):
            xt, xk = next_xt(G)
            P.dma('sp', xt[:, :], G.h_res[t * 128:(t + 1) * 128, :], r=[('h_res', t)], w=[xk])
            yield
            P.op('dve', lambda e, xt=xt, t=t: e.scalar_tensor_tensor(xt[:, :], xt[:, :], DN_ALPHA, yacc[:, t, :], ALU.mult, ALU.add),
                 r=[xk, ('y', t, 0), ('y', t, 1)], w=[xk])
            yield
            yield from ln_tile(G, xt, xk, t, slot=t % LN_W, out_final=(G.out if last else None))

        first = True
        n = 0
        wdi = 0
        for (wg_d, wu_d, wd_d, e_idx) in experts:
            for blk in range(nblk):
                ncols = min(512, F - blk * 512)
                nj = ncols // 128
                wg, wgk = load_w(G, wg_d, blk * 512, ncols, tag=('ffn', l, e_idx, blk))
                wu, wuk = load_w(G, wu_d, blk * 512, ncols)
                wd = wds[wdi % 2]
                wdk = ('wd', wdi % 2)
                wdi += 1
                P.dma('pool', wd[:, 0:nj, :], wd_d[blk * 512:blk * 512 + ncols, :].rearrange("(j p) c -> p j c", p=128), w=[wdk])
                for j in range(nj):
                    for tg in range(4):
                        si = n % 2
                        n += 1
                        psg, pkg = fm_group(G, wg, wgk, j, tg)
                        psu, pku = fm_group(G, wu, wuk, j, tg)
                        P.op('act', lambda e, psg=psg, si=si: e.activation(sl[si][:, :], psg[:, :], AF.Silu), r=[pkg], w=[('sl', si)])
                        P.op('dve', lambda e, psu=psu, si=si, j=j, tg=tg: e.tensor_tensor(hid[:, j, tg * 512:(tg + 1) * 512], sl[si][:, :], psu[:, :], ALU.mult),
                             r=[('sl', si), pku], w=[('hid', j, tg)])
                is_last = (e_idx is None or e_idx == NEXP - 1) and blk == nblk - 1
                for t in range(NT):
                    for hf in range(2):
                        ps, pk = next_ps(G)

                        def fd(e, ps=ps, t=t, hf=hf, nj=nj, wd=wd):
                            ins = None
                            for j in range(nj):
                                ins = e.matmul(ps[:, :], hid[:, j, t * 128:(t + 1) * 128], wd[:, j, hf * 512:(hf + 1) * 512], start=(j == 0), stop=(j == nj - 1))
                            return ins
                        P.op('pe', fd, r=[('hid', j, t // 4) for j in range(nj)] + [wdk], w=[pk])
                        ya = yacc[:, t, hf * 512:(hf + 1) * 512]
                        yk = ('y', t, hf)
                        if e_idx is None:
                            if first:
                                P.op('act', lambda e, ps=ps, ya=ya: e.activation(ya, ps[:, :], AF.Copy), r=[pk], w=[yk])
                            else:
                                P.op('dve', lambda e, ps=ps, ya=ya: e.tensor_tensor(ya, ya, ps[:, :], ALU.add), r=[pk, yk], w=[yk])
                        else:
                            gsc = G.gate_all[:, t, e_idx:e_idx + 1]
                            if first:
                                P.op('act', lambda e, ps=ps, ya=ya, gsc=gsc: e.activation(ya, ps[:, :], AF.Copy, scale=gsc), r=[pk, ('gate', t)], w=[yk])
                            else:
                                P.op('dve', lambda e, ps=ps, ya=ya, gsc=gsc: e.scalar_tensor_tensor(ya, ps[:, :], gsc, ya, ALU.mult, ALU.add),
                                     r=[pk, yk, ('gate', t)], w=[yk])
                    if is_last:
                        while len(ln_active) >= LN_W:
                            for g_ in list(ln_active):
                                try:
                                    next(g_)
                                except StopIteration:
                                    ln_active.remove(g_)
                        ln_active.append(unit(t))
                        for _ in range(4):
                            for g_ in list(ln_active):
                                try:
                                    next(g_)
                                except StopIteration:
                                    ln_active.remove(g_)
                first = False
        while ln_active:
            for g_ in list(ln_active):
                try:
                    next(g_)
                except StopIteration:
                    ln_active.remove(g_)


_CACHE = {}


def kernel(**inputs):
    if 'nc' not in _CACHE:
        _CACHE['nc'] = build()
    nc = _CACHE['nc']
    C = _consts()
    x = np.asarray(inputs['x'], np.float32)
    B = x.shape[0]
    base = {name: np.ascontiguousarray(np.asarray(inputs[name], np.float32)) for name, _ in W_SPECS}
    for name in CONST_NAMES:
        base["c_" + name] = np.ascontiguousarray(C[name])
    in_maps = []
    for b in range(B):
        m = dict(base)
        m['x'] = np.ascontiguousarray(x[b])
        in_maps.append(m)
    res = run_bass_kernel_spmd(nc, in_maps, core_ids=list(range(B)))
    return np.stack([np.asarray(r['out'], np.float32) for r in res.results], 0)
```

```python
import contextlib
import math
import numpy as np
import concourse.bass as bass
import concourse.mybir as mybir
from concourse.bass_utils import run_bass_kernel_spmd

F32 = mybir.dt.float32
BF16 = mybir.dt.bfloat16
ALU = mybir.AluOpType
AF = mybir.ActivationFunctionType
AX = mybir.AxisListType

S = 2048
D = 1024
NT = 16
KC = 8
DEPTH = 2
N_IN = 11776
D_FF = 2816
D_EXP = 3584
NEXP = 8
LN_EPS = 1e-5
RMS_EPS = 1e-6
DN_ALPHA = (2.0 * DEPTH) ** 0.25
OFF = dict(rq=0, rk=512, rv=1024, rg=2048, hq=3072, hf=4096, hi=5120, hg=6144,
           sq=7168, sk=8192, sv=8448, ga=8704, gb=9728, gc=10752)
HL = 32

COMPUTE = ('pe', 'act', 'dve', 'pool')
ALL_ENG = ('pe', 'act', 'dve', 'pool', 'sp')
NDMASEM = 6


class Op:
    __slots__ = ('eng', 'fn', 'deps', 'dma', 'sem', 'ticket', 'idx', 'prev_ticket', 'semkey')


class Prog:
    def __init__(self, nc, sems, dma_sems, same_engine_sync=True):
        self.nc = nc
        self.sems = sems
        self.dma_sems = dma_sems
        self.count = {e: 0 for e in COMPUTE}
        self.dma_count = {(q, i): 0 for q in dma_sems for i in range(len(dma_sems[q]))}
        self.dma_rr = {q: 0 for q in dma_sems}
        self.waited = {e: {} for e in ALL_ENG}
        self.same_engine_sync = same_engine_sync
        self.ops = []
        self.lw = {}
        self.rd = {}
        self.prev_final = []

    def op(self, eng, fn, r=(), w=(), dma=False):
        deps = set()
        for b in r:
            if b in self.lw:
                deps.add(self.lw[b])
        for b in w:
            if b in self.lw:
                deps.add(self.lw[b])
            for x in self.rd.get(b, ()):
                deps.add(x)
        o = Op()
        o.eng = eng; o.fn = fn; o.dma = dma; o.sem = None; o.ticket = None
        o.idx = len(self.ops)
        o.deps = deps
        self.ops.append(o)
        for b in r:
            self.rd.setdefault(b, []).append(o.idx)
        for b in w:
            self.lw[b] = o.idx
            self.rd[b] = []
        return o.idx

    def dma(self, q, out, in_, r=(), w=(), **kw):
        def fn(e):
            return e.dma_start(out=out, in_=in_, **kw)
        return self.op(q, fn, r, w, dma=True)

    def emit(self, final_wait=False):
        nc = self.nc
        ops = self.ops
        needed = set()
        for o in ops:
            for d in o.deps:
                od = ops[d]
                if od.eng == o.eng and not od.dma:
                    if od.eng == 'pe' or not self.same_engine_sync:
                        continue
                needed.add(d)
        last = {}
        for o in ops:
            if not o.dma:
                last[o.eng] = o.idx
        for e, i in last.items():
            needed.add(i)
        for o in ops:
            if o.dma:
                q = o.eng
                i = self.dma_rr[q]
                self.dma_rr[q] = (i + 1) % len(self.dma_sems[q])
                o.prev_ticket = self.dma_count[(q, i)]
                self.dma_count[(q, i)] += 16
                o.sem = self.dma_sems[q][i]
                o.semkey = ('d', q, i)
                o.ticket = self.dma_count[(q, i)]
            elif o.idx in needed:
                self.count[o.eng] += 1
                o.sem = self.sems[o.eng]
                o.semkey = ('c', o.eng)
                o.ticket = self.count[o.eng]
        by_eng = {e: [o for o in ops if o.eng == e] for e in ALL_ENG}
        prev_final = self.prev_final
        waited = self.waited
        same_sync = self.same_engine_sync

        def run(engname, eh):
            wd = waited[engname]

            def wait(semkey, sem, ticket):
                if wd.get(semkey, 0) >= ticket:
                    return
                eh.wait_ge(sem, ticket)
                wd[semkey] = ticket
            for (semkey, sem, ticket) in prev_final:
                if semkey == ('c', engname) and (engname == 'pe' or not same_sync):
                    continue
                wait(semkey, sem, ticket)
            for o in by_eng[engname]:
                for d in sorted(o.deps):
                    od = ops[d]
                    if od.ticket is None:
                        continue
                    if od.eng == o.eng and not od.dma and (engname == 'pe' or not same_sync):
                        continue
                    wait(od.semkey, od.sem, od.ticket)
                if o.dma and o.prev_ticket > 0:
                    wait(o.semkey, o.sem, o.prev_ticket)
                inst = o.fn(eh)
                if o.ticket is not None:
                    inst.then_inc(o.sem, 16 if o.dma else 1)
            if engname in self.dma_sems:
                for i, sem in enumerate(self.dma_sems[engname]):
                    t = self.dma_count[(engname, i)]
                    if t > 0:
                        wait(('d', engname, i), sem, t)
            if final_wait and engname == 'sp':
                for q in self.dma_sems:
                    for i, sem in enumerate(self.dma_sems[q]):
                        t = self.dma_count[(q, i)]
                        if t > 0:
                            wait(('d', q, i), sem, t)

        with nc.Block() as block:
            @block.tensor
            def _(e):
                run('pe', e)

            @block.scalar
            def _(e):
                run('act', e)

            @block.vector
            def _(e):
                run('dve', e)

            @block.gpsimd
            def _(e):
                run('pool', e)

            @block.sync
            def _(e):
                run('sp', e)
        fin = []
        for e in COMPUTE:
            if self.count[e] > 0:
                fin.append((('c', e), self.sems[e], self.count[e]))
        for (q, i), t in self.dma_count.items():
            if t > 0:
                fin.append((('d', q, i), self.dma_sems[q][i], t))
        self.prev_final = fin
        self.ops = []
        self.lw = {}
        self.rd = {}


class Ctx:
    pass


G_SKIP = set()
import os
HG_LEVEL = int(os.environ.get('HG_LEVEL', '4'))
SW_LEVEL = int(os.environ.get('SW_LEVEL', '3'))
SW_SUB = int(os.environ.get("SW_SUB", "3"))
SW_W = int(os.environ.get("SW_W", "4"))
LN_W = int(os.environ.get("LN_W", "4"))
HG_MERGE = bool(int(os.environ.get("HG_MERGE", "0")))
SAME_SYNC = bool(int(os.environ.get('SAME_SYNC', '1')))


def _consts():
    c = {}
    half = 64
    inv = (1.0 / (10000.0 ** np.linspace(0.0, 1.0, half, dtype=np.float32))).astype(np.float32)
    pos = np.arange(S, dtype=np.float32)
    ang = pos[:, None] * inv[None, :]
    cos = np.cos(ang).astype(np.float32).T
    sin = np.sin(ang).astype(np.float32).T
    c['cosT'] = np.ascontiguousarray(np.concatenate([cos, cos], 0))
    c['sinT'] = np.ascontiguousarray(np.concatenate([sin, sin], 0))
    pm = np.zeros((128, 128), np.float32)
    for d in range(64):
        pm[d + 64, d] = -1.0
        pm[d, d + 64] = 1.0
    c['pm'] = pm
    c['ident'] = np.eye(128, dtype=np.float32)
    lg = np.log(1.0 - 2.0 ** (-5.0 - np.arange(4, dtype=np.float64)))
    idx = np.arange(128, dtype=np.float64)
    dt = np.zeros((128, 4, 128), np.float32)
    qd = np.zeros((128, 4, 128), np.float32)
    kd = np.zeros((128, 4), np.float32)
    for h in range(4):
        diff = idx[None, :] - idx[:, None]
        dt[:, h, :] = np.where(diff >= 0, np.exp(np.maximum(diff, 0) * lg[h]), 0.0) * 128 ** -0.5
        qd[:, h, :] = np.exp((idx + 1.0) * lg[h])[None, :]
        kd[:, h] = np.exp((127.0 - idx) * lg[h]) * 128 ** -0.5
    c['ret_dt'] = dt
    c['ret_qd'] = qd
    c['ret_kd'] = kd
    c['ret_cd'] = [float(np.exp(128.0 * lg[h])) for h in range(4)]
    s_ = np.arange(128)
    bm = ((s_[:, None] // HL) == (s_[None, :] // HL)) & (s_[:, None] <= s_[None, :])
    c['hg_bm'] = bm.astype(np.float32)
    sm = np.ones((128, 512), np.float32)
    sm[:, ::HL] = 0.0
    c['hg_scanm'] = sm
    c['hg_blk'] = (s_[:, None] // HL == np.arange(128 // HL)[None, :]).astype(np.float32)
    NEG = -30000.0
    m1 = np.zeros((128, 256), np.float32)
    m1[:64, 192:256] = NEG
    m1[64:, 0:64] = NEG
    m0 = m1.copy()
    m0[:, 0:128] = NEG
    c['swa_mask'] = np.stack([m0, m1], 1)
    return c


CONST_NAMES = ['cosT', 'sinT', 'pm', 'ident', 'ret_dt', 'ret_qd', 'ret_kd', 'hg_bm', 'hg_scanm', 'hg_blk', 'swa_mask']
W_SPECS = [
    ('ln_in_g', [D]), ('ln_in_b', [D]), ('w_in', [DEPTH, D, N_IN]), ('ret_w_out', [DEPTH, D, D]),
    ('hgrn_lower_bounds', [DEPTH, D]), ('hgrn_norm_g', [DEPTH, 128]), ('hgrn_w_out', [DEPTH, D, D]),
    ('swa_sinks', [DEPTH, 16]), ('swa_w_out', [DEPTH, D, D]), ('w_o', [DEPTH, D, D]),
    ('ln_mix_g', [DEPTH, D]), ('ln_mix_b', [DEPTH, D]),
    ('ffn_w_gate', [1, D, D_FF]), ('ffn_w_up', [1, D, D_FF]), ('ffn_w_down', [1, D_FF, D]),
    ('moe_router', [1, D, NEXP]), ('moe_w_gate', [1, NEXP, D, D_EXP]), ('moe_w_up', [1, NEXP, D, D_EXP]),
    ('moe_w_down', [1, NEXP, D_EXP, D]), ('ln_ffn_g', [DEPTH, D]), ('ln_ffn_b', [DEPTH, D]),
]


def build(stop_after=None, debug=False):
    C = _consts()
    nc = bass.Bass("TRN2", target_bir_lowering=False)
    G = Ctx()
    G.nc = nc
    G.x = nc.dram_tensor("x", [S, D], F32, kind="ExternalInput").ap()
    G.W = {}
    for name, shp in W_SPECS:
        G.W[name] = nc.dram_tensor(name, shp, F32, kind="ExternalInput").ap()
    G.K = {}
    for name in CONST_NAMES:
        G.K[name] = nc.dram_tensor("c_" + name, list(C[name].shape), F32, kind="ExternalInput").ap()
    G.out = nc.dram_tensor("out", [S, D], F32, kind="ExternalOutput").ap()
    skind = "ExternalOutput" if debug else "Internal"
    G.h_res = nc.dram_tensor("h_res", [S, D], F32, kind=skind).ap()
    G.xT = {b: nc.dram_tensor("x%sT" % b, [D, S], BF16, kind=skind).ap() for b in 'abc'}
    G.mgT = nc.dram_tensor("mgT", [D, S], BF16, kind=skind).ap()
    G.cd = C['ret_cd']

    with contextlib.ExitStack() as es:
        uid = [0]

        def sb(name, shape, dt, st=es):
            uid[0] += 1
            return st.enter_context(nc.sbuf_tensor("%s_u%d" % (name, uid[0]), shape, dt))
        sems = {e: es.enter_context(nc.semaphore("s_" + e)) for e in COMPUTE}
        dsems = {q: [es.enter_context(nc.semaphore("d_%s%d" % (q, i))) for i in range(NDMASEM)]
                 for q in ('sp', 'pool')}
        P = Prog(nc, sems, dsems, same_engine_sync=SAME_SYNC)
        G.P = P
        G.sb = sb
        G.hT = sb("hT", [128, KC, S], BF16)
        G.ident_f = sb("ident_f", [128, 128], F32)
        G.ident_b = sb("ident_b", [128, 128], BF16)
        G.ws = [sb("ws%d" % i, [128, KC, 512], BF16) for i in range(4)]
        G.ws_rr = 0
        G.xt_rr = 0
        G.small = sb("small", [128, 64], F32)
        G.ps = [es.enter_context(nc.psum_tensor("ps%d" % i, [128, 512], F32)) for i in range(8)]
        G.ps_rr = 0
        G.gate_all = sb("gate_all", [128, NT, NEXP], F32)

        phases = []
        phases.append(('ln_in', lambda: phase_ln_in(G)))
        for l in range(DEPTH):
            phases.append(('ret%d' % l, lambda l=l: phase_ret(G, l)))
            phases.append(('hgrn%d' % l, lambda l=l: phase_hgrn(G, l)))
            phases.append(('swa%d' % l, lambda l=l: phase_swa(G, l)))
            phases.append(('merge%d' % l, lambda l=l: phase_merge(G, l)))
            phases.append(('wo%d' % l, lambda l=l: phase_wo(G, l)))
            phases.append(('ffn%d' % l, lambda l=l: phase_ffn(G, l)))
        for i, (name, fn) in enumerate(phases):
            if G_SKIP and name.rstrip('0123456789') in G_SKIP and name != stop_after:
                continue
            fn()
            lastp = (i == len(phases) - 1) or (name == stop_after)
            if not lastp and not G_SKIP:
                nxt = phases[i + 1][0]
                ln_ = int(nxt[-1]) if nxt[-1].isdigit() else 0
                kind = nxt.rstrip('0123456789')
                w_in_n = G.W['w_in'][ln_]
                if kind == 'ret':
                    prefetch_w(G, ('rv', ln_, 0), w_in_n, OFF['rv'], 512)
                elif kind == 'hgrn':
                    prefetch_w(G, ('hi', ln_, 0), w_in_n, OFF['hi'], 512)
                elif kind == 'swa':
                    prefetch_w(G, ('sq', ln_, 0), w_in_n, OFF['sq'], 512)
                elif kind == 'merge':
                    G.ws_rr = 0
                    prefetch_w(G, ('my', ln_, 'a', 0), G.W['ret_w_out'][ln_], 0, 512)
                    prefetch_w(G, ('my', ln_, 'b', 0), G.W['hgrn_w_out'][ln_], 0, 512)
                    prefetch_w(G, ('my', ln_, 'c', 0), G.W['swa_w_out'][ln_], 0, 512)
                    prefetch_w(G, ('mg', ln_, 'a', 0), w_in_n, OFF['ga'], 512)
                elif kind == 'wo':
                    prefetch_w(G, ('wo', ln_), G.W['w_o'][ln_], 0, 512)
                elif kind == 'ffn':
                    if ln_ % 2 == 1:
                        prefetch_w(G, ('ffn', ln_, 0, 0), G.W['moe_w_gate'][ln_ // 2][0], 0, 512)
                    else:
                        prefetch_w(G, ('ffn', ln_, None, 0), G.W['ffn_w_gate'][ln_ // 2], 0, 512)
            P.emit(final_wait=lastp)
            if lastp:
                break
    return nc


def next_ps(G):
    i = G.ps_rr % len(G.ps_sel)
    G.ps_rr = (i + 1) % len(G.ps_sel)
    j = G.ps_sel[i]
    return G.ps[j], ('ps', j)


def next_ws(G):
    i = G.ws_rr
    G.ws_rr = (i + 1) % len(G.ws)
    return G.ws[i], ('ws', i)


def load_w(G, wdram, c0, ncols, q='pool', tag=None):
    pf = getattr(G, 'prefetched', None)
    if tag is not None and pf and tag in pf:
        wt, key = pf.pop(tag)
        return wt, key
    wt, key = next_ws(G)
    G.P.dma(q, wt[:, :, 0:ncols], wdram[:, c0:c0 + ncols].rearrange("(k p) c -> p k c", p=128), w=[key])
    return wt, key


def prefetch_w(G, tag, wdram, c0, ncols):
    if len(G.ws) != 4:
        return
    wt, key = load_w(G, wdram, c0, ncols)
    if getattr(G, 'prefetched', None) is None:
        G.prefetched = {}
    G.prefetched[tag] = (wt, key)


def fm_group(G, wt, wkey, cc, tg, rkeys=('hT',), src=None, ncols_tok=512, t0=None):
    src = G.hT if src is None else src
    ps, pk = next_ps(G)
    t0 = tg * 512 if t0 is None else t0

    def fn(e, ps=ps, wt=wt, cc=cc, t0=t0, src=src):
        ins = None
        for k in range(KC):
            ins = e.matmul(ps[:, 0:ncols_tok], wt[:, k, cc * 128:(cc + 1) * 128], src[:, k, t0:t0 + ncols_tok],
                           start=(k == 0), stop=(k == KC - 1))
        return ins
    G.P.op('pe', fn, r=[wkey] + list(rkeys), w=[pk])
    return ps, pk


def tm_group(G, wt, wkey, tile, ncols=512, rkeys=('hT',), src=None):
    src = G.hT if src is None else src
    ps, pk = next_ps(G)

    def fn(e, ps=ps, wt=wt, tile=tile, src=src):
        ins = None
        for k in range(KC):
            ins = e.matmul(ps[:, 0:ncols], src[:, k, tile * 128:(tile + 1) * 128], wt[:, k, 0:ncols],
                           start=(k == 0), stop=(k == KC - 1))
        return ins
    G.P.op('pe', fn, r=[wkey] + list(rkeys), w=[pk])
    return ps, pk


def load_consts_common(G):
    P = G.P
    P.dma('sp', G.ident_f[:, :], G.K['ident'], w=['ident_f'])
    P.dma('pool', G.ident_b[:, :], G.K['ident'], w=['ident_b'])


def rr(gens, width):
    active = []
    it = iter(gens)
    done = False
    while True:
        while len(active) < width and not done:
            try:
                active.append(next(it))
            except StopIteration:
                done = True
        if not active:
            break
        for g in list(active):
            try:
                next(g)
            except StopIteration:
                active.remove(g)


def ln_tile(G, xt, xkey, tile, slot=0, out_final=None, pss=None):
    P = G.P
    sm = G.small
    o = slot * 16
    st = sm[:, o:o + 12]
    mv = sm[:, o + 12:o + 14]
    rs = sm[:, o + 14:o + 15]
    k = lambda n: (n, slot)
    P.op('dve', lambda e: e.bn_stats(st[:, 0:6], xt[:, 0:512]), r=[xkey], w=[k('ln_st0')])
    P.op('dve', lambda e: e.bn_stats(st[:, 6:12], xt[:, 512:1024]), r=[xkey], w=[k('ln_st1')])
    yield
    P.op('dve', lambda e: e.bn_aggr(mv, st), r=[k('ln_st0'), k('ln_st1')], w=[k('ln_mv')])
    yield
    P.op('act', lambda e: e.activation(rs, mv[:, 1:2], AF.Sqrt, bias=LN_EPS, scale=1.0), r=[k('ln_mv')], w=[k('ln_rs')])
    yield
    P.op('dve', lambda e: e.reciprocal(rs, rs), r=[k('ln_rs')], w=[k('ln_rs')])
    yield
    P.op('dve', lambda e: e.scalar_tensor_tensor(xt[:, :], xt[:, :], mv[:, 0:1], G.gbc[:, :], ALU.subtract, ALU.mult),
         r=[xkey, k('ln_mv'), 'gbc'], w=[xkey])
    yield
    P.op('dve', lambda e: e.scalar_tensor_tensor(xt[:, :], xt[:, :], rs, G.bbc[:, :], ALU.mult, ALU.add),
         r=[xkey, k('ln_rs'), 'bbc'], w=[xkey])
    yield
    rows = slice(tile * 128, (tile + 1) * 128)
    if out_final is not None:
        P.dma('sp', out_final[rows, :], xt[:, :], r=[xkey], w=[('out', tile)])
    else:
        P.dma('sp', G.h_res[rows, :], xt[:, :], r=[xkey], w=[('h_res', tile)])
        for hb in range(2):
            ps, pk = next_ps(G)

            def fn(e, ps=ps, hb=hb):
                ins = None
                for i in range(4):
                    kk = hb * 4 + i
                    ins = e.matmul(ps[:, i * 128:(i + 1) * 128], xt[:, kk * 128:(kk + 1) * 128], G.ident_f[:, :],
                                   start=True, stop=True)
                return ins
            P.op('pe', fn, r=[xkey, 'ident_f'], w=[pk])
            yield
            P.op('act', lambda e, ps=ps, hb=hb: e.activation(
                G.hT[:, hb * 4:(hb + 1) * 4, tile * 128:(tile + 1) * 128],
                ps[:, :].rearrange("p (a t) -> p a t", t=128), AF.Copy), r=[pk], w=['hT'])
            if pss is not None:
                pss.append((ps, pk))
            yield


def load_ln_params(G, g_ap, b_ap):
    G.P.dma('sp', G.gbc[:, :], g_ap.partition_broadcast(128), w=['gbc'])
    G.P.dma('sp', G.bbc[:, :], b_ap.partition_broadcast(128), w=['bbc'])


def next_xt(G):
    i = G.xt_rr
    G.xt_rr = (i + 1) % len(G.xt)
    return G.xt[i], ('xt', i)


def phase_ln_in(G):
    P = G.P
    G.ps_sel = list(range(8))
    with contextlib.ExitStack() as st:
        sb = lambda n, s, d: G.sb(n, s, d, st)
        G.xt = [sb("i_xt%d" % i, [128, D], F32) for i in range(LN_W + 1)]
        G.gbc = sb("i_gbc", [128, D], F32)
        G.bbc = sb("i_bbc", [128, D], F32)
        load_consts_common(G)
        load_ln_params(G, G.W['ln_in_g'], G.W['ln_in_b'])
        def unit(t):
            xt, xk = next_xt(G)
            P.dma('sp', xt[:, :], G.x[t * 128:(t + 1) * 128, :], w=[xk])
            yield
            yield from ln_tile(G, xt, xk, t, slot=t % LN_W)
        rr((unit(t) for t in range(NT)), LN_W)


def phase_ret(G, l):
    P = G.P
    nc = G.nc
    w_in = G.W['w_in'][l]
    G.ps_sel = [0, 1]
    with contextlib.ExitStack() as st:
        sb = lambda n, s, d: G.sb(n, s, d, st)
        v_tok = sb("r_v", [128, NT, 512], BF16)
        g_tok = sb("r_g", [128, NT, 512], BF16)
        qT = sb("r_q", [128, 2, S], BF16)
        kT = sb("r_k", [128, 2, S], BF16)
        cs = [sb("r_cs%d" % i, [128, 2, 512], F32) for i in range(2)]
        xf = [sb("r_xf%d" % i, [128, 512], F32) for i in range(2)]
        t1 = [sb("r_t1%d" % i, [128, 512], F32) for i in range(2)]
        pm = sb("r_pm", [128, 128], F32)
        dtm = sb("r_dt", [128, 4, 128], F32)
        qdm = sb("r_qd", [128, 4, 128], F32)
        kdm = sb("r_kd", [128, 4], F32)
        scm = [sb("r_scm%d" % i, [128, 128], BF16) for i in range(2)]
        qd = [sb("r_qdd%d" % i, [128, 128], BF16) for i in range(2)]
        kd = [sb("r_kdd%d" % i, [128, 128], BF16) for i in range(2)]
        stf = [sb("r_stf%d" % i, [128, 256], F32) for i in range(2)]
        stb = [sb("r_stb%d" % i, [128, 256], BF16) for i in range(2)]
        xg = [sb("r_xg%d" % i, [128, 512], F32) for i in range(2)]
        xo = [sb("r_xo%d" % i, [128, 4, 128], BF16) for i in range(2)]
        ss = sb("r_ss", [128, 4], F32)
        junk = sb("r_junk", [128, 256], F32)
        ps_o = [G.ps[2], G.ps[3]]
        ps_s = [G.ps[4], G.ps[5]]
        ps_m = [G.ps[6], G.ps[7]]
        P.dma('sp', pm[:, :], G.K['pm'], w=['pm'])
        P.dma('sp', dtm[:, :, :], G.K['ret_dt'], w=['dtm'])
        P.dma('sp', qdm[:, :, :], G.K['ret_qd'], w=['qdm'])
        P.dma('sp', kdm[:, :], G.K['ret_kd'], w=['kdm'])
        for hp in range(2):
            G.ps_sel = list(range(8))
            wt, wk = load_w(G, w_in, OFF['rv'] + hp * 512, 512, tag=('rv', l, hp))
            for t in range(NT):
                ps, pk = tm_group(G, wt, wk, t)
                P.op('act', lambda e, ps=ps, t=t: e.activation(v_tok[:, t, :], ps[:, :], AF.Copy), r=[pk], w=[('v', t)])
            wt, wk = load_w(G, w_in, OFF['rg'] + hp * 512, 512)
            for t in range(NT):
                ps, pk = tm_group(G, wt, wk, t)
                P.op('act', lambda e, ps=ps, t=t: e.activation(g_tok[:, t, :], ps[:, :], AF.Silu), r=[pk], w=[('g', t)])
            for (nm, dst) in (('rq', qT), ('rk', kT)):
                wt, wk = load_w(G, w_in, OFF[nm] + hp * 256, 256)
                for tg in range(4):
                    ci = tg % 2
                    P.dma('sp', cs[ci][:, 0, :], G.K['cosT'][:, tg * 512:(tg + 1) * 512], w=[('cs', ci, 0)])
                    P.dma('sp', cs[ci][:, 1, :], G.K['sinT'][:, tg * 512:(tg + 1) * 512], w=[('cs', ci, 1)])
                    for j in range(2):
                        bi = (tg * 2 + j) % 2
                        ps, pk = fm_group(G, wt, wk, j, tg)
                        P.op('act', lambda e, ps=ps, bi=bi: e.activation(xf[bi][:, :], ps[:, :], AF.Copy), r=[pk], w=[('xf', bi)])
                        ps2, pk2 = next_ps(G)
                        P.op('pe', lambda e, ps2=ps2, bi=bi: e.matmul(ps2[:, :], pm[:, :], xf[bi][:, :], start=True, stop=True),
                             r=[('xf', bi), 'pm'], w=[pk2])
                        P.op('dve', lambda e, bi=bi, ci=ci: e.tensor_tensor(t1[bi][:, :], xf[bi][:, :], cs[ci][:, 0, :], ALU.mult),
                             r=[('xf', bi), ('cs', ci, 0)], w=[('t1', bi)])
                        P.op('dve', lambda e, ps2=ps2, bi=bi, ci=ci: e.tensor_tensor(xf[bi][:, :], ps2[:, :], cs[ci][:, 1, :], ALU.mult),
                             r=[pk2, ('cs', ci, 1)], w=[('xf', bi)])
                        P.op('dve', lambda e, bi=bi, dst=dst, j=j, tg=tg: e.tensor_tensor(
                            dst[:, j, tg * 512:(tg + 1) * 512], t1[bi][:, :], xf[bi][:, :], ALU.add),
                            r=[('t1', bi), ('xf', bi)], w=[(nm, j, tg)])
            G.ps_sel = [0, 1]
            for n in range(NT):
                pso = ps_o[n % 2]
                pok = ('ps', 2 + n % 2)
                tgk = n // 4
                cols = slice(n * 128, (n + 1) * 128)
                for j in range(2):
                    h = hp * 2 + j
                    psm = ps_m[j]
                    pmk = ('ps', 6 + j)
                    pss_ = ps_s[j]
                    psk = ('ps', 4 + j)
                    P.op('pe', lambda e, pss_=pss_, j=j, cols=cols: e.matmul(pss_[:, 0:128], kT[:, j, cols], qT[:, j, cols], start=True, stop=True),
                         r=[('rk', j, tgk), ('rq', j, tgk)], w=[psk])
                    P.op('dve', lambda e, pss_=pss_, j=j, h=h: e.tensor_tensor(scm[j][:, :], pss_[:, 0:128], dtm[:, h, :], ALU.mult),
                         r=[psk, 'dtm'], w=[('scm', j)])
                    if n > 0:
                        P.op('dve', lambda e, j=j, h=h, cols=cols: e.tensor_tensor(qd[j][:, :], qT[:, j, cols], qdm[:, h, :], ALU.mult),
                             r=[('rq', j, tgk), 'qdm'], w=[('qd', j)])

                    def fo(e, pso=pso, j=j, n=n):
                        ins = e.matmul(pso[:, j * 256:(j + 1) * 256], scm[j][:, :], v_tok[:, n, j * 256:(j + 1) * 256],
                                       start=True, stop=(n == 0))
                        if n > 0:
                            ins = e.matmul(pso[:, j * 256:(j + 1) * 256], qd[j][:, :], stb[j][:, :], start=False, stop=True)
                        return ins
                    P.op('pe', fo, r=[('scm', j), ('v', n), ('qd', j), ('stb', j)], w=[pok])
                    if n < NT - 1:
                        P.op('pe', lambda e, psm=psm, j=j, cols=cols: e.matmul(psm[:, 0:128], kT[:, j, cols], G.ident_b[:, :], start=True, stop=True),
                             r=[('rk', j, tgk), 'ident_b'], w=[pmk])
                        P.op('act', lambda e, psm=psm, j=j, h=h: e.activation(kd[j][:, :], psm[:, 0:128], AF.Copy, scale=kdm[:, h:h + 1]),
                             r=[pmk, 'kdm'], w=[('kd', j)])
                        P.op('pe', lambda e, psm=psm, j=j, n=n: e.matmul(psm[:, 128:384], kd[j][:, :], v_tok[:, n, j * 256:(j + 1) * 256], start=True, stop=True),
                             r=[('kd', j), ('v', n)], w=[pmk])
                        if n == 0:
                            P.op('dve', lambda e, psm=psm, j=j: e.tensor_copy(stf[j][:, :], psm[:, 128:384]), r=[pmk], w=[('stf', j)])
                        else:
                            P.op('dve', lambda e, psm=psm, j=j, h=h: e.scalar_tensor_tensor(
                                stf[j][:, :], stf[j][:, :], G.cd[h], psm[:, 128:384], ALU.mult, ALU.add),
                                r=[pmk, ('stf', j)], w=[('stf', j)])
                        P.op('act', lambda e, j=j: e.activation(stb[j][:, :], stf[j][:, :], AF.Copy), r=[('stf', j)], w=[('stb', j)])
                xi = n % 2
                for j in range(2):
                    P.op('act', lambda e, pso=pso, j=j: e.activation(junk[:, :], pso[:, j * 256:(j + 1) * 256], AF.Square, accum_out=ss[:, j:j + 1]),
                         r=[pok], w=['junk', ('ss', j)])
                P.op('act', lambda e: e.activation(ss[:, 2:4], ss[:, 0:2], AF.Sqrt, bias=RMS_EPS, scale=1.0 / 256.0),
                     r=[('ss', 0), ('ss', 1)], w=['rstd'])
                P.op('dve', lambda e: e.reciprocal(ss[:, 2:4], ss[:, 2:4]), r=['rstd'], w=['rstd'])
                for j in range(2):
                    P.op('dve', lambda e, pso=pso, j=j, n=n, xi=xi: e.scalar_tensor_tensor(
                        xg[xi][:, j * 256:(j + 1) * 256], pso[:, j * 256:(j + 1) * 256], ss[:, 2 + j:3 + j],
                        g_tok[:, n, j * 256:(j + 1) * 256], ALU.mult, ALU.mult),
                        r=[pok, 'rstd', ('g', n)], w=[('xg', xi, j)])
                ps, pk = next_ps(G)

                def ft(e, ps=ps, xi=xi):
                    ins = None
                    for i in range(4):
                        ins = e.matmul(ps[:, i * 128:(i + 1) * 128], xg[xi][:, i * 128:(i + 1) * 128], G.ident_f[:, :], start=True, stop=True)
                    return ins
                P.op('pe', ft, r=[('xg', xi, 0), ('xg', xi, 1), 'ident_f'], w=[pk])
                P.op('act', lambda e, ps=ps, xi=xi: e.activation(xo[xi][:, :, :], ps[:, :].rearrange("p (a t) -> p a t", t=128), AF.Copy),
                     r=[pk], w=[('xo', xi)])
                P.dma('sp', G.xT['a'][hp * 512:(hp + 1) * 512, cols].rearrange("(i p) t -> p i t", p=128), xo[xi][:, :, :],
                      r=[('xo', xi)], w=[('xaT', hp, n)])


def phase_hgrn(G, l):
    P = G.P
    w_in = G.W['w_in'][l]
    G.ps_sel = [0, 1]
    with contextlib.ExitStack() as st:
        sb = lambda n, s, d: G.sb(n, s, d, st)
        v_tok = sb("h_v", [128, NT, 512], BF16)
        qtT = sb("h_qt", [128, 4, S], BF16)
        ktT = sb("h_kt", [128, 4, S], BF16)
        ksT = sb("h_ks", [128, 4, S], BF16)
        sgT = sb("h_sg", [128, 4, S], BF16)
        bdT = sb("h_bd", [128, 4, 64], F32)
        T = [[sb("h_t%d_%d" % (a, i), [128, 512], F32) for i in range(5)] for a in range(2)]
        lbr = sb("h_lbr", [128, 2, 8], F32)
        lbt = sb("h_lb", [128, 8], F32)
        oml = sb("h_oml", [128, 8], F32)
        gn = sb("h_gn", [128, 1], F32)
        bm = sb("h_bm", [128, 128], F32)
        scanm = sb("h_scanm", [128, 512], F32)
        ones_f = sb("h_ones", [128, 128], BF16)
        am = [[sb("h_am%d_%d" % (a, j), [128, 128], BF16) for j in range(4)] for a in range(2)]
        kst = [[sb("h_kst%d_%d" % (a, j), [128, 128], BF16) for j in range(4)] for a in range(2)]
        stf = [sb("h_stf%d" % j, [128, 128], F32) for j in range(4)]
        stb = [[sb("h_stb%d_%d" % (j, a), [128, 128], BF16) for a in range(2)] for j in range(4)]
        osb = [sb("h_osb%d" % a, [128, 512], F32) for a in range(2)]
        sq = [sb("h_sq%d" % a, [128, 512], BF16) for a in range(2)]
        rt = [sb("h_rt%d" % a, [128, 512], F32) for a in range(2)]
        xo = [sb("h_xo%d" % i, [128, 4, 128], BF16) for i in range(2)]
        blkm = sb("h_blkm", [128, 128 // HL], F32)
        vblk = [[sb("h_vblk%d_%d" % (a, j), [128, 128 // HL, 128], BF16) for j in range(4)] for a in range(2)]
        psU = [G.ps[2], G.ps[3], G.ps[6], G.ps[7]]
        P.dma('sp', blkm[:, :], G.K['hg_blk'], w=['blkm'])
        P.dma('sp', bm[:, :], G.K['hg_bm'], w=['bm'])
        P.dma('sp', scanm[:, :], G.K['hg_scanm'], w=['scanm'])
        P.dma('sp', gn[:, :], G.W['hgrn_norm_g'][l].rearrange("(p o) -> p o", o=1), w=['gn'])
        P.op('pool', lambda e: e.memset(ones_f[:, :], 1.0), w=['ones_f'])
        if l == 0:
            P.op('pool', lambda e: e.memset(lbt[:, :], 0.0), w=['lbt'])
        else:
            for a in range(2):
                P.dma('sp', lbr[:, a, :], G.W['hgrn_lower_bounds'][a].rearrange("(h d) -> d h", d=128), w=[('lbr', a)],
                      allow_slow_non_contiguous=True)
            P.op('dve', lambda e: e.tensor_tensor(lbt[:, :], lbr[:, 1, :], lbr[:, 0, :], ALU.subtract), r=[('lbr', 0), ('lbr', 1)], w=['lbt'])
            P.op('act', lambda e: e.activation(lbt[:, :], lbt[:, :], AF.Sigmoid), r=['lbt'], w=['lbt'])
        P.op('dve', lambda e: e.tensor_scalar(oml[:, :], lbt[:, :], -1.0, 1.0, ALU.mult, ALU.add), r=['lbt'], w=['oml'])
        NB = 128 // HL
        for hg in range(2):
            G.ps_sel = [0, 1, 2, 3, 6, 7]
            wt, wk = load_w(G, w_in, OFF['hi'] + hg * 512, 512, tag=('hi', l, hg))
            for t in range(NT):
                ps, pk = tm_group(G, wt, wk, t)
                P.op('act', lambda e, ps=ps, t=t: e.activation(v_tok[:, t, :], ps[:, :], AF.Copy), r=[pk], w=[('v', t)])
            wq, wqk = load_w(G, w_in, OFF['hq'] + hg * 512, 512)
            wf, wfk = load_w(G, w_in, OFF['hf'] + hg * 512, 512)
            wg, wgk = load_w(G, w_in, OFF['hg'] + hg * 512, 512)

            def h2unit(j, tg, a):
                h = hg * 4 + j
                t1, t2, t3, t4, t5 = T[a]
                tk = lambda i, a=a: ('T', a, i)
                cols = slice(tg * 512, (tg + 1) * 512)
                psz, pkz = fm_group(G, wf, wfk, j, tg)
                yield
                P.op('act', lambda e: e.activation(t2[:, :], psz[:, :], AF.Exp, scale=-1.0), r=[pkz], w=[tk(2)])
                yield
                P.op('act', lambda e: e.activation(t1[:, :], t2[:, :], AF.Ln, bias=1.0, scale=1.0), r=[tk(2)], w=[tk(1)])
                yield
                P.op('act', lambda e: e.activation(t1[:, :], t1[:, :], AF.Exp, scale=-1.0), r=[tk(1)], w=[tk(1)])
                yield
                P.op('dve', lambda e: e.tensor_tensor(t2[:, :], t2[:, :], t1[:, :], ALU.mult), r=[tk(1), tk(2)], w=[tk(2)])
                psq, pkq = fm_group(G, wq, wqk, j, tg)
                yield
                P.op('dve', lambda e: e.tensor_scalar(t1[:, :], t1[:, :], oml[:, h:h + 1], lbt[:, h:h + 1], ALU.mult, ALU.add),
                     r=[tk(1), 'oml', 'lbt'], w=[tk(1)])
                yield
                P.op('act', lambda e: e.activation(t3[:, :], t1[:, :], AF.Ln), r=[tk(1)], w=[tk(3)])
                yield
                P.op('dve', lambda e: e.tensor_tensor_scan(t4[:, :], scanm[:, :], t3[:, :], 0.0, ALU.mult, ALU.add),
                     r=[tk(3), 'scanm'], w=[tk(4)])
                yield
                P.op('act', lambda e: e.activation(t1[:, :], t4[:, :], AF.Exp), r=[tk(4)], w=[tk(1)])
                P.op('act', lambda e: e.activation(t3[:, :], t4[:, :], AF.Exp, scale=-1.0), r=[tk(4)], w=[tk(3)])
                yield
                P.op('dve', lambda e: e.tensor_tensor(qtT[:, j, cols], psq[:, :], t1[:, :], ALU.mult),
                     r=[pkq, tk(1)], w=[('qt', j, tg)])
                yield
                P.op('dve', lambda e: e.scalar_tensor_tensor(t2[:, :], t2[:, :], oml[:, h:h + 1], t3[:, :], ALU.mult, ALU.mult),
                     r=[tk(2), tk(3), 'oml'], w=[tk(2)])
                yield
                P.op('act', lambda e: e.activation(ktT[:, j, cols], t2[:, :], AF.Copy), r=[tk(2)], w=[('kt', j, tg)])
                yield
                P.op('dve', lambda e: e.tensor_tensor(
                    ksT[:, j, cols].rearrange("p (b l) -> p b l", l=HL),
                    t2[:, :].rearrange("p (b l) -> p b l", l=HL),
                    t1[:, :].rearrange("p (b l) -> p b l", l=HL)[:, :, HL - 1:HL].broadcast_to([128, 512 // HL, HL]), ALU.mult),
                    r=[tk(1), tk(2)], w=[('ks', j, tg)])
                yield
                nb = 512 // HL
                P.op('dve', lambda e: e.tensor_copy(
                    bdT[:, j, tg * nb:(tg + 1) * nb],
                    t1[:, :].rearrange("p (b l) -> p b l", l=HL)[:, :, HL - 1:HL].rearrange("p b o -> p (b o)")),
                    r=[tk(1)], w=[('bd', j, tg)])
                yield
            for j in range(4):
                for tg in range(4):
                    psg, pkg = fm_group(G, wg, wgk, j, tg)
                    P.op('act', lambda e, psg=psg, j=j, tg=tg: e.activation(sgT[:, j, tg * 512:(tg + 1) * 512], psg[:, :], AF.Silu), r=[pkg], w=[('sg', j, tg)])
            units = [(j, tg) for j in range(4) for tg in range(4)]
            rr((h2unit(j, tg, n % 2) for n, (j, tg) in enumerate(units)), 2)
            if HG_LEVEL < 2:
                continue
            G.ps_sel = [0, 1]
            psok = [('ps', 4), ('ps', 5)]
            psUk = [('ps', 2), ('ps', 3), ('ps', 6), ('ps', 7)]

            def front(i):
                tg = i // 4
                cols = slice(i * 128, (i + 1) * 128)
                a = i % 2
                for j in range(4):
                    ps, pk = next_ps(G)
                    P.op('pe', lambda e, ps=ps, j=j: e.matmul(ps[:, 0:128], ktT[:, j, cols], qtT[:, j, cols], start=True, stop=True),
                         r=[('kt', j, tg), ('qt', j, tg)], w=[pk])
                    P.op('dve', lambda e, ps=ps, j=j: e.tensor_tensor(am[a][j][:, :], ps[:, 0:128], bm[:, :], ALU.mult), r=[pk, 'bm'], w=[('am', a, j)])
                    ps, pk = next_ps(G)
                    P.op('pe', lambda e, ps=ps, j=j: e.matmul(ps[:, 128:256], ksT[:, j, cols], G.ident_b[:, :], start=True, stop=True),
                         r=[('ks', j, tg), 'ident_b'], w=[pk])
                    P.op('act', lambda e, ps=ps, j=j: e.activation(kst[a][j][:, :], ps[:, 128:256], AF.Copy), r=[pk], w=[('kst', a, j)])
                    P.op('dve', lambda e, j=j: e.tensor_tensor(
                        vblk[a][j][:, :, :], v_tok[:, i, j * 128:(j + 1) * 128].rearrange("p (o v) -> p o v", o=1).broadcast_to([128, NB, 128]),
                        blkm[:, :].rearrange("p (b o) -> p b o", o=1).broadcast_to([128, NB, 128]), ALU.mult),
                        r=[('v', i), 'blkm'], w=[('vblk', a, j)])

            def umat(i):
                a = i % 2
                for j in range(4):
                    P.op('pe', lambda e, j=j: e.matmul(psU[j][:, :], kst[a][j][:, :], vblk[a][j][:, :, :].rearrange("p b v -> p (b v)"), start=True, stop=True),
                         r=[('kst', a, j), ('vblk', a, j)], w=[psUk[j]])

            def mid(i):
                tg = i // 4
                a = i % 2
                pso = G.ps[4 + a]
                pok = psok[a]
                if HG_MERGE:
                    for j in range(4):
                        P.op('pe', lambda e, j=j: e.matmul(pso[:, j * 128:(j + 1) * 128], v_tok[:, i, j * 128:(j + 1) * 128], am[a][j][:, :],
                                                           start=True, stop=False, skip_group_check=True),
                             r=[('v', i), ('am', a, j)], w=[pok])
                for b in range(NB):
                    gb = i * NB + b
                    for j in range(4):
                        def fo(e, j=j, b=b, gb=gb):
                            oc = pso[:, j * 128 + b * HL: j * 128 + (b + 1) * HL]
                            ins = None
                            if not HG_MERGE:
                                ins = e.matmul(oc, v_tok[:, i, j * 128:(j + 1) * 128], am[a][j][:, b * HL:(b + 1) * HL], start=True, stop=(gb == 0))
                            if gb > 0:
                                ins = e.matmul(oc, stb[j][gb % 2][:, :], qtT[:, j, i * 128 + b * HL: i * 128 + (b + 1) * HL], start=False, stop=True,
                                               skip_group_check=HG_MERGE)
                            return ins
                        if HG_MERGE and gb == 0:
                            continue
                        P.op('pe', fo, r=[('v', i), ('am', a, j), ('stb', j, gb % 2), ('qt', j, tg)], w=[pok])
                    if gb < S // HL - 1:
                        for j in range(4):
                            if gb == 0:
                                P.op('dve', lambda e, j=j, b=b: e.tensor_copy(stf[j][:, :], psU[j][:, b * 128:(b + 1) * 128]), r=[psUk[j]], w=[('stf', j)])
                            else:
                                P.op('dve', lambda e, j=j, gb=gb, b=b: e.scalar_tensor_tensor(
                                    stf[j][:, :], stf[j][:, :], bdT[:, j, gb:gb + 1], psU[j][:, b * 128:(b + 1) * 128], ALU.mult, ALU.add),
                                    r=[psUk[j], ('stf', j), ('bd', j, gb // (512 // HL))], w=[('stf', j)])
                            P.op('act', lambda e, j=j, gb=gb: e.activation(stb[j][(gb + 1) % 2][:, :], stf[j][:, :], AF.Copy),
                                 r=[('stf', j)], w=[('stb', j, (gb + 1) % 2)])

            def back(i):
                tg = i // 4
                cols = slice(i * 128, (i + 1) * 128)
                a = i % 2
                pso = G.ps[4 + a]
                pok = psok[a]
                osb_, sq_, rt_ = osb[a], sq[a], rt[a]
                P.op('act', lambda e: e.activation(osb_[:, :], pso[:, :], AF.Copy), r=[pok], w=[('osb', a)])
                P.op('act', lambda e: e.activation(sq_[:, :], pso[:, :], AF.Square), r=[pok], w=[('sq', a)])
                ps, pk = next_ps(G)
                P.op('pe', lambda e, ps=ps: e.matmul(ps[:, :], ones_f[:, :], sq_[:, :], start=True, stop=True), r=[('sq', a), 'ones_f'], w=[pk])
                P.op('act', lambda e, ps=ps: e.activation(rt_[:, :], ps[:, :], AF.Ln, bias=RMS_EPS, scale=1.0 / 128.0), r=[pk], w=[('rt', a)])
                P.op('act', lambda e: e.activation(rt_[:, :], rt_[:, :], AF.Exp, scale=-0.5), r=[('rt', a)], w=[('rt', a)])
                P.op('dve', lambda e: e.scalar_tensor_tensor(osb_[:, :], osb_[:, :], gn[:, 0:1], rt_[:, :], ALU.mult, ALU.mult), r=[('osb', a), ('rt', a), 'gn'], w=[('osb', a)])
                P.op('dve', lambda e: e.tensor_tensor(
                    xo[a][:, :, :], osb_[:, :].rearrange("p (a t) -> p a t", t=128), sgT[:, :, cols], ALU.mult),
                    r=[('osb', a)] + [('sg', j, tg) for j in range(4)], w=[('xo', a)])
                P.dma('sp', G.xT['b'][hg * 512:(hg + 1) * 512, cols].rearrange("(i p) t -> p i t", p=128), xo[a][:, :, :],
                      r=[('xo', a)], w=[('xbT', hg, i)])

            front(0)
            umat(0)
            for sstep in range(NT):
                if sstep + 1 < NT:
                    front(sstep + 1)
                mid(sstep)
                if sstep + 1 < NT:
                    umat(sstep + 1)
                back(sstep)


def phase_swa(G, l):
    P = G.P
    w_in = G.W['w_in'][l]
    G.ps_sel = list(range(8))
    with contextlib.ExitStack() as st:
        sb = lambda n, s, d: G.sb(n, s, d, st)
        qT = sb("s_q", [128, 8, S], BF16)
        kT = sb("s_k", [128, 4, 2, 128 + S], BF16)
        vt = sb("s_v", [128, NT + 1, 4, 2, 128], BF16)
        mask = sb("s_mask", [128, 2, 256], F32)
        sink = sb("s_sink", [128, 16], F32)
        with contextlib.ExitStack() as st1:
            wkd = G.sb("s_wkd", [128, KC, 4, 2, 128], BF16, st1)
            P.dma('sp', mask[:, :, :], G.K['swa_mask'], w=['mask'])
            P.dma('sp', sink[:, :], G.W['swa_sinks'][l].partition_broadcast(128), w=['sink'])
            P.op('pool', lambda e: e.memset(kT[:, :, :, 0:128], 0.0), w=['kpad'])
            P.op('pool', lambda e: e.memset(vt[:, :, :, :, :].rearrange("p a b c d -> p (a b c d)"), 0.0), w=['vt0'])
            P.op('pool', lambda e: e.memset(wkd[:, :, :, :, :].rearrange("p a b c d -> p (a b c d)"), 0.0), w=['wkd0'])
            for g in range(4):
                for var in range(2):
                    P.dma('pool', wkd[:, :, g, var, var * 64:(var + 1) * 64],
                          w_in[:, OFF['sk'] + g * 64: OFF['sk'] + (g + 1) * 64].rearrange("(k p) c -> p k c", p=128),
                          r=['wkd0'], w=[('wkd', g, var)])
            for blk in range(2):
                wt, wk = load_w(G, w_in, OFF['sq'] + blk * 512, 512, tag=('sq', l, blk))
                for cc in range(4):
                    for tg in range(4):
                        ps, pk = fm_group(G, wt, wk, cc, tg)
                        P.op('act', lambda e, ps=ps, c8=blk * 4 + cc, tg=tg: e.activation(qT[:, c8, tg * 512:(tg + 1) * 512], ps[:, :], AF.Copy, scale=0.125),
                             r=[pk], w=[('q', blk * 4 + cc, tg)])
            for g in range(4):
                for var in range(2):
                    for tg in range(4):
                        ps, pk = next_ps(G)

                        def fk(e, ps=ps, g=g, var=var, tg=tg):
                            ins = None
                            for k in range(KC):
                                ins = e.matmul(ps[:, :], wkd[:, k, g, var, :], G.hT[:, k, tg * 512:(tg + 1) * 512], start=(k == 0), stop=(k == KC - 1))
                            return ins
                        P.op('pe', fk, r=[('wkd', g, var), 'hT'], w=[pk])
                        P.op('act', lambda e, ps=ps, g=g, var=var, tg=tg: e.activation(kT[:, g, var, 128 + tg * 512:128 + (tg + 1) * 512], ps[:, :], AF.Copy),
                             r=[pk, 'kpad'], w=[('k', g, var, tg)])
            wt, wk = load_w(G, w_in, OFF['sv'], 256)
            for t in range(NT):
                ps, pk = tm_group(G, wt, wk, t, ncols=256)
                P.op('act', lambda e, ps=ps, t=t: e.activation(vt[:, t + 1, :, 0, 0:64], ps[:, 0:256].rearrange("p (g d) -> p g d", d=64), AF.Copy),
                     r=[pk, 'vt0'], w=[('vt', t + 1, 0)])
                P.op('act', lambda e, ps=ps, t=t: e.activation(vt[:, t + 1, :, 1, 64:128], ps[:, 0:256].rearrange("p (g d) -> p g d", d=64), AF.Copy),
                     r=[pk, 'vt0'], w=[('vt', t + 1, 1)])
            P.emit()
        NSL = SW_W
        sms = [sb("s_sm%d" % i, [128, 4, 256], F32) for i in range(NSL)]
        ps_ = [sb("s_p%d" % i, [128, 4, 256], BF16) for i in range(NSL)]
        mxs = [sb("s_mx%d" % i, [128, 16], F32) for i in range(NSL)]
        pTs = [sb("s_pT%d" % i, [128, 8, 128], BF16) for i in range(NSL)]
        xos = [sb("s_xo%d" % i, [128, 2, 128], BF16) for i in range(NSL)]

        def s2unit(i, g, slot):
            cols = slice(i * 128, (i + 1) * 128)
            mi = 0 if i == 0 else 1
            sm, p, mx, pT, xo = sms[slot], ps_[slot], mxs[slot], pTs[slot], xos[slot]
            bk = [slot * 2, slot * 2 + 1]
            kk = lambda n: (n, slot)
            for a in range(4):
                hq = 4 * g + a
                c8, var = hq // 2, hq % 2
                bank = bk[a // 2]
                P.op('pe', lambda e, a=a, c8=c8, var=var, bank=bank: e.matmul(
                    G.ps[bank][:, (a % 2) * 256:(a % 2 + 1) * 256], qT[:, c8, cols], kT[:, g, var, i * 128:i * 128 + 256], start=True, stop=True),
                    w=[('ps', bank)])
            yield
            for hh in range(2):
                bank = bk[hh]
                P.op('dve', lambda e, hh=hh, bank=bank: e.tensor_tensor(
                    sm[:, hh * 2:(hh + 1) * 2, :], G.ps[bank][:, :].rearrange("p (a k) -> p a k", k=256),
                    mask[:, mi:mi + 1, :].broadcast_to([128, 2, 256]), ALU.add),
                    r=[('ps', bank)], w=[kk(('sm', hh))])
            yield
            P.op('dve', lambda e: e.tensor_reduce(mx[:, 0:4], sm[:, :, :], AX.X, ALU.max), r=[kk(('sm', 0)), kk(('sm', 1))], w=[kk('mx_m')])
            yield
            P.op('dve', lambda e: e.tensor_tensor(mx[:, 0:4], mx[:, 0:4], sink[:, 4 * g:4 * g + 4], ALU.max), r=[kk('mx_m')], w=[kk('mx_m')])
            yield
            P.op('dve', lambda e: e.tensor_scalar(mx[:, 4:8], mx[:, 0:4], -1.0, None, ALU.mult), r=[kk('mx_m')], w=[kk('mx_n')])
            yield
            for a in range(4):
                P.op('act', lambda e, a=a: e.activation(p[:, a, :], sm[:, a, :], AF.Exp, bias=mx[:, 4 + a:5 + a], scale=1.0, accum_out=mx[:, 8 + a:9 + a]),
                     r=[kk(('sm', a // 2)), kk('mx_n')], w=[kk('p'), kk(('mx_s', a))])
            P.op('dve', lambda e: e.tensor_tensor(mx[:, 12:16], sink[:, 4 * g:4 * g + 4], mx[:, 4:8], ALU.add), r=[kk('mx_n')], w=[kk('mx_d')])
            yield
            P.op('act', lambda e: e.activation(mx[:, 12:16], mx[:, 12:16], AF.Exp), r=[kk('mx_d')], w=[kk('mx_d')])
            yield
            P.op('dve', lambda e: e.tensor_tensor(mx[:, 12:16], mx[:, 12:16], mx[:, 8:12], ALU.add), r=[kk('mx_d')] + [kk(('mx_s', a)) for a in range(4)], w=[kk('mx_d')])
            yield
            P.op('dve', lambda e: e.reciprocal(mx[:, 12:16], mx[:, 12:16]), r=[kk('mx_d')], w=[kk('mx_d')])
            yield
            P.op('dve', lambda e: e.tensor_tensor(p[:, :, :], p[:, :, :], mx[:, 12:16].rearrange("p (a o) -> p a o", o=1).broadcast_to([128, 4, 256]), ALU.mult),
                 r=[kk('p'), kk('mx_d')], w=[kk('p')])
            yield
            for hh in range(2):
                def ftr(e, hh=hh):
                    ins = None
                    for q in range(4):
                        idx = hh * 4 + q
                        a, kt = idx // 2, idx % 2
                        ins = e.matmul(G.ps[bk[hh]][:, q * 128:(q + 1) * 128], p[:, a, kt * 128:(kt + 1) * 128], G.ident_b[:, :], start=True, stop=True)
                    return ins
                P.op('pe', ftr, r=[kk('p'), 'ident_b', kk(('sm', hh))], w=[('ps', bk[hh])])
            yield
            P.op('act', lambda e: e.activation(pT[:, 0:4, :], G.ps[bk[0]][:, :].rearrange("p (a t) -> p a t", t=128), AF.Copy), r=[('ps', bk[0])], w=[kk(('pT', 0))])
            P.op('dve', lambda e: e.tensor_copy(pT[:, 4:8, :], G.ps[bk[1]][:, :].rearrange("p (a t) -> p a t", t=128)), r=[('ps', bk[1])], w=[kk(('pT', 1))])
            yield
            for pr in range(2):
                pso = G.ps[bk[pr]]

                def fpv(e, pso=pso, pr=pr):
                    ins = None
                    n = 0
                    for (a, var) in ((2 * pr, 0), (2 * pr + 1, 1)):
                        for kt in range(2):
                            ins = e.matmul(pso[:, 0:128], vt[:, i + kt, g, var, :], pT[:, a * 2 + kt, :], start=(n == 0), stop=(n == 3))
                            n += 1
                    return ins
                P.op('pe', fpv, r=[kk(('pT', pr))], w=[('ps', bk[pr])])
                yield
                P.op('act', lambda e, pso=pso, pr=pr: e.activation(xo[:, pr, :], pso[:, 0:128], AF.Copy), r=[('ps', bk[pr])], w=[kk(('xo', pr))])
                yield
            P.dma('sp', G.xT['c'][g * 256:(g + 1) * 256, cols].rearrange("(i p) t -> p i t", p=128), xo[:, :, :],
                  r=[kk(('xo', 0)), kk(('xo', 1))], w=[('xcT', g, i)])
            yield
        units = [(i, g) for i in range(NT) for g in range(4)]
        rr((s2unit(i, g, n % NSL) for n, (i, g) in enumerate(units)), NSL)


def phase_merge(G, l):
    P = G.P
    G.ps_sel = list(range(8))
    w_in = G.W['w_in'][l]
    with contextlib.ExitStack() as st:
        sb = lambda n, s, d: G.sb(n, s, d, st)
        xs = {b: sb("m_x" + b, [128, KC, S], BF16) for b in 'abc'}
        ws_save = G.ws
        G.ws = list(G.ws) + [sb("m_ws%d" % i, [128, KC, 512], BF16) for i in range(2)]
        G.ws_rr = 4 if getattr(G, 'prefetched', None) else 0
        sg = [sb("m_sg%d" % i, [128, 512], F32) for i in range(3)]
        macc = sb("m_acc", [128, 512], F32)
        mo = [sb("m_o%d" % i, [128, 512], BF16) for i in range(2)]
        for b in 'abc':
            for k in range(KC):
                P.dma('sp', xs[b][:, k, :], G.xT[b][k * 128:(k + 1) * 128, :], w=[('x', b)] if k == 0 else [('x', b, k)])
        xkeys = {b: [('x', b)] + [('x', b, k) for k in range(1, KC)] for b in 'abc'}
        wouts = {'a': G.W['ret_w_out'][l], 'b': G.W['hgrn_w_out'][l], 'c': G.W['swa_w_out'][l]}
        goff = {'a': OFF['ga'], 'b': OFF['gb'], 'c': OFF['gc']}
        n = 0
        for cb in range(2):
            wy = {b: load_w(G, wouts[b], cb * 512, 512, tag=('my', l, b, cb)) for b in 'abc'}
            wg = {b: load_w(G, w_in, goff[b] + cb * 512, 512, tag=('mg', l, b, cb)) for b in 'abc'}
            for cc in range(4):
                for tg in range(4):
                    oi = n % 2
                    n += 1
                    for bi, b in enumerate('abc'):
                        psy, pky = fm_group(G, wy[b][0], wy[b][1], cc, tg, rkeys=xkeys[b], src=xs[b])
                        psg, pkg = fm_group(G, wg[b][0], wg[b][1], cc, tg)
                        P.op('act', lambda e, psg=psg, bi=bi: e.activation(sg[bi][:, :], psg[:, :], AF.Sigmoid), r=[pkg], w=[('sg', bi)])
                        if bi == 0:
                            P.op('dve', lambda e, psy=psy, bi=bi: e.tensor_tensor(macc[:, :], sg[bi][:, :], psy[:, :], ALU.mult), r=[('sg', bi), pky], w=['macc'])
                        else:
                            P.op('dve', lambda e, psy=psy, bi=bi: e.tensor_tensor(sg[bi][:, :], sg[bi][:, :], psy[:, :], ALU.mult), r=[('sg', bi), pky], w=[('sg', bi)])
                            if bi == 1:
                                P.op('dve', lambda e, bi=bi: e.tensor_tensor(macc[:, :], macc[:, :], sg[bi][:, :], ALU.add), r=['macc', ('sg', bi)], w=['macc'])
                            else:
                                P.op('dve', lambda e, bi=bi, oi=oi: e.tensor_tensor(mo[oi][:, :], macc[:, :], sg[bi][:, :], ALU.add), r=['macc', ('sg', bi)], w=[('mo', oi)])
                    c8 = cb * 4 + cc
                    P.dma('sp', G.mgT[c8 * 128:(c8 + 1) * 128, tg * 512:(tg + 1) * 512], mo[oi][:, :], r=[('mo', oi)], w=[('mgT', c8, tg)])
        G.ws = ws_save
        G.ws_rr = 0


def router_tile(G, R, tile, pss, slot=0):
    P = G.P
    wr, hTfs, lgs, rrs = R
    hTf, lg, rr_ = hTfs[slot], lgs[slot], rrs[slot]
    k = lambda n: (n, slot)
    for hb, (ps, pk) in enumerate(pss):
        P.op('act', lambda e, ps=ps, hb=hb: e.activation(hTf[:, hb * 4:(hb + 1) * 4, :], ps[:, :].rearrange("p (a t) -> p a t", t=128), AF.Copy),
             r=[pk], w=[k(('hTf', hb))])
        yield
    ps, pk = next_ps(G)

    def fr(e, ps=ps):
        ins = None
        for kk in range(KC):
            ins = e.matmul(ps[:, 0:NEXP], hTf[:, kk, :], wr[:, kk, :], start=(kk == 0), stop=(kk == KC - 1))
        return ins
    P.op('pe', fr, r=[k(('hTf', 0)), k(('hTf', 1)), 'wr'], w=[pk])
    yield
    P.op('dve', lambda e, ps=ps: e.tensor_copy(lg[:, 0:8], ps[:, 0:NEXP]), r=[pk], w=[k('lg')])
    yield
    P.op('dve', lambda e: e.tensor_reduce(rr_[:, 0:1], lg[:, 0:8], AX.X, ALU.max), r=[k('lg')], w=[k('rr0')])
    yield
    P.op('dve', lambda e: e.tensor_scalar(lg[:, 8:16], lg[:, 0:8], rr_[:, 0:1], None, ALU.is_equal), r=[k('lg'), k('rr0')], w=[k('lg1')])
    yield
    P.op('dve', lambda e: e.scalar_tensor_tensor(lg[:, 8:16], lg[:, 8:16], -1e30, lg[:, 0:8], ALU.mult, ALU.add), r=[k('lg'), k('lg1')], w=[k('lg1')])
    yield
    P.op('dve', lambda e: e.tensor_reduce(rr_[:, 1:2], lg[:, 8:16], AX.X, ALU.max), r=[k('lg1')], w=[k('rr1')])
    yield
    P.op('dve', lambda e: e.tensor_scalar(lg[:, 8:16], lg[:, 0:8], rr_[:, 1:2], None, ALU.is_ge), r=[k('lg'), k('rr1'), k('lg1')], w=[k('lg1')])
    P.op('dve', lambda e: e.tensor_scalar(rr_[:, 2:3], rr_[:, 0:1], -1.0, None, ALU.mult), r=[k('rr0')], w=[k('rr2')])
    yield
    P.op('act', lambda e: e.activation(lg[:, 16:24], lg[:, 0:8], AF.Exp, bias=rr_[:, 2:3], scale=1.0), r=[k('lg'), k('rr2')], w=[k('lg2')])
    yield
    P.op('dve', lambda e: e.tensor_tensor(lg[:, 16:24], lg[:, 16:24], lg[:, 8:16], ALU.mult), r=[k('lg2'), k('lg1')], w=[k('lg2')])
    yield
    P.op('dve', lambda e: e.tensor_reduce(rr_[:, 3:4], lg[:, 16:24], AX.X, ALU.add), r=[k('lg2')], w=[k('rr3')])
    yield
    P.op('dve', lambda e: e.reciprocal(rr_[:, 3:4], rr_[:, 3:4]), r=[k('rr3')], w=[k('rr3')])
    yield
    P.op('dve', lambda e, tile=tile: e.tensor_scalar(G.gate_all[:, tile, :], lg[:, 16:24], rr_[:, 3:4], None, ALU.mult), r=[k('lg2'), k('rr3')], w=[('gate', tile)])
    yield


def phase_wo(G, l):
    P = G.P
    G.ps_sel = list(range(8))
    with contextlib.ExitStack() as st:
        sb = lambda n, s, d: G.sb(n, s, d, st)
        mg = sb("o_mg", [128, KC, S], BF16)
        G.xt = [sb("o_xt%d" % i, [128, D], F32) for i in range(LN_W + 1)]
        G.gbc = sb("o_gbc", [128, D], F32)
        G.bbc = sb("o_bbc", [128, D], F32)
        R = None
        if l % 2 == 1:
            wr = sb("o_wr", [128, KC, NEXP], F32)
            hTfs = [sb("o_hTf%d" % i, [128, KC, 128], F32) for i in range(LN_W)]
            lgs = [sb("o_lg%d" % i, [128, 24], F32) for i in range(LN_W)]
            rrs = [sb("o_rr%d" % i, [128, 4], F32) for i in range(LN_W)]
            R = (wr, hTfs, lgs, rrs)
            P.dma('sp', wr[:, :, :], G.W['moe_router'][l // 2].rearrange("(k p) e -> p k e", p=128), w=['wr'])
        for k in range(KC):
            P.dma('sp', mg[:, k, :], G.mgT[k * 128:(k + 1) * 128, :], w=[('mg', k)])
        mgk = [('mg', k) for k in range(KC)]
        load_ln_params(G, G.W['ln_mix_g'][l], G.W['ln_mix_b'][l])
        w0 = load_w(G, G.W['w_o'][l], 0, 512, tag=('wo', l))
        w1 = load_w(G, G.W['w_o'][l], 512, 512)

        def unit(t):
            xt, xk = next_xt(G)
            P.dma('sp', xt[:, :], G.h_res[t * 128:(t + 1) * 128, :], r=[('h_res', t)], w=[xk])
            yield
            for hf, (wt, wk) in enumerate((w0, w1)):
                ps, pk = tm_group(G, wt, wk, t, rkeys=mgk, src=mg)
                yield
                P.op('dve', lambda e, ps=ps, xt=xt, hf=hf: e.scalar_tensor_tensor(
                    xt[:, hf * 512:(hf + 1) * 512], xt[:, hf * 512:(hf + 1) * 512], DN_ALPHA, ps[:, :], ALU.mult, ALU.add), r=[pk, xk], w=[xk])
                yield
            pss = []
            yield from ln_tile(G, xt, xk, t, slot=t % LN_W, pss=pss)
            if R is not None:
                yield from router_tile(G, R, t, pss, slot=t % LN_W)
        rr((unit(t) for t in range(NT)), LN_W)


def phase_ffn(G, l):
    P = G.P
    G.ps_sel = list(range(8))
    moe = (l % 2 == 1)
    jx = l // 2
    last = (l == DEPTH - 1)
    with contextlib.ExitStack() as st:
        sb = lambda n, s, d: G.sb(n, s, d, st)
        yacc = sb("f_y", [128, NT, D], F32)
        hid = sb("f_hid", [128, 4, S], BF16)
        wds = [sb("f_wd%d" % i, [128, 4, D], BF16) for i in range(2)]
        sl = [sb("f_sl%d" % i, [128, 512], F32) for i in range(2)]
        G.xt = [sb("f_xt%d" % i, [128, D], F32) for i in range(LN_W + 1)]
        G.xt_rr = 0
        G.gbc = sb("f_gbc", [128, D], F32)
        G.bbc = sb("f_bbc", [128, D], F32)
        if moe:
            experts = [(G.W['moe_w_gate'][jx][e], G.W['moe_w_up'][jx][e], G.W['moe_w_down'][jx][e], e) for e in range(NEXP)]
            F = D_EXP
        else:
            experts = [(G.W['ffn_w_gate'][jx], G.W['ffn_w_up'][jx], G.W['ffn_w_down'][jx], None)]
            F = D_FF
        nblk = (F + 511) // 512
        load_ln_params(G, G.W['ln_ffn_g'][l], G.W['ln_ffn_b'][l])
        ln_active = []

        def unit(t):
            xt, xk = next_xt(G)
            P.dma('sp', xt[:, :], G.h_res[t * 128:(t + 1) * 128, :], r=[('h_res', t)], w=[xk])
            yield
            P.op('dve', lambda e, xt=xt, t=t: e.scalar_tensor_tensor(xt[:, :], xt[:, :], DN_ALPHA, yacc[:, t, :], ALU.mult, ALU.add),
                 r=[xk, ('y', t, 0), ('y', t, 1)], w=[xk])
            yield
            yield from ln_tile(G, xt, xk, t, slot=t % LN_W, out_final=(G.out if last else None))

        first = True
        n = 0
        wdi = 0
        for (wg_d, wu_d, wd_d, e_idx) in experts:
            for blk in range(nblk):
                ncols = min(512, F - blk * 512)
                nj = ncols // 128
                wg, wgk = load_w(G, wg_d, blk * 512, ncols, tag=('ffn', l, e_idx, blk))
                wu, wuk = load_w(G, wu_d, blk * 512, ncols)
                wd = wds[wdi % 2]
                wdk = ('wd', wdi % 2)
                wdi += 1
                P.dma('pool', wd[:, 0:nj, :], wd_d[blk * 512:blk * 512 + ncols, :].rearrange("(j p) c -> p j c", p=128), w=[wdk])
                for j in range(nj):
                    for tg in range(4):
                        si = n % 2
                        n += 1
                        psg, pkg = fm_group(G, wg, wgk, j, tg)
                        psu, pku = fm_group(G, wu, wuk, j, tg)
                        P.op('act', lambda e, psg=psg, si=si: e.activation(sl[si][:, :], psg[:, :], AF.Silu), r=[pkg], w=[('sl', si)])
                        P.op('dve', lambda e, psu=psu, si=si, j=j, tg=tg: e.tensor_tensor(hid[:, j, tg * 512:(tg + 1) * 512], sl[si][:, :], psu[:, :], ALU.mult),
                             r=[('sl', si), pku], w=[('hid', j, tg)])
                is_last = (e_idx is None or e_idx == NEXP - 1) and blk == nblk - 1
                for t in range(NT):
                    for hf in range(2):
                        ps, pk = next_ps(G)

                        def fd(e, ps=ps, t=t, hf=hf, nj=nj, wd=wd):
                            ins = None
                            for j in range(nj):
                                ins = e.matmul(ps[:, :], hid[:, j, t * 128:(t + 1) * 128], wd[:, j, hf * 512:(hf + 1) * 512], start=(j == 0), stop=(j == nj - 1))
                            return ins
                        P.op('pe', fd, r=[('hid', j, t // 4) for j in range(nj)] + [wdk], w=[pk])
                        ya = yacc[:, t, hf * 512:(hf + 1) * 512]
                        yk = ('y', t, hf)
                        if e_idx is None:
                            if first:
                                P.op('act', lambda e, ps=ps, ya=ya: e.activation(ya, ps[:, :], AF.Copy), r=[pk], w=[yk])
                            else:
                                P.op('dve', lambda e, ps=ps, ya=ya: e.tensor_tensor(ya, ya, ps[:, :], ALU.add), r=[pk, yk], w=[yk])
                        else:
                            gsc = G.gate_all[:, t, e_idx:e_idx + 1]
                            if first:
                                P.op('act', lambda e, ps=ps, ya=ya, gsc=gsc: e.activation(ya, ps[:, :], AF.Copy, scale=gsc), r=[pk, ('gate', t)], w=[yk])
                            else:
                                P.op('dve', lambda e, ps=ps, ya=ya, gsc=gsc: e.scalar_tensor_tensor(ya, ps[:, :], gsc, ya, ALU.mult, ALU.add),
                                     r=[pk, yk, ('gate', t)], w=[yk])
                    if is_last:
                        while len(ln_active) >= LN_W:
                            for g_ in list(ln_active):
                                try:
                                    next(g_)
                                except StopIteration:
                                    ln_active.remove(g_)
                        ln_active.append(unit(t))
                        for _ in range(4):
                            for g_ in list(ln_active):
                                try:
                                    next(g_)
                                except StopIteration:
                                    ln_active.remove(g_)
                first = False
        while ln_active:
            for g_ in list(ln_active):
                try:
                    next(g_)
                except StopIteration:
                    ln_active.remove(g_)


_CACHE = {}


def kernel(**inputs):
    if 'nc' not in _CACHE:
        _CACHE['nc'] = build()
    nc = _CACHE['nc']
    C = _consts()
    x = np.asarray(inputs['x'], np.float32)
    B = x.shape[0]
    base = {name: np.ascontiguousarray(np.asarray(inputs[name], np.float32)) for name, _ in W_SPECS}
    for name in CONST_NAMES:
        base["c_" + name] = np.ascontiguousarray(C[name])
    in_maps = []
    for b in range(B):
        m = dict(base)
        m['x'] = np.ascontiguousarray(x[b])
        in_maps.append(m)
    res = run_bass_kernel_spmd(nc, in_maps, core_ids=list(range(B)))
    return np.stack([np.asarray(r['out'], np.float32) for r in res.results], 0)
```

```python
import contextlib
import math
import numpy as np
import concourse.bass as bass
import concourse.mybir as mybir
from concourse.bass_utils import run_bass_kernel_spmd

F32 = mybir.dt.float32
BF16 = mybir.dt.bfloat16
ALU = mybir.AluOpType
AF = mybir.ActivationFunctionType
AX = mybir.AxisListType

S = 2048
D = 1024
NT = 16
KC = 8
DEPTH = 2
N_IN = 11776
D_FF = 2816
D_EXP = 3584
NEXP = 8
LN_EPS = 1e-5
RMS_EPS = 1e-6
DN_ALPHA = (2.0 * DEPTH) ** 0.25
OFF = dict(rq=0, rk=512, rv=1024, rg=2048, hq=3072, hf=4096, hi=5120, hg=6144,
           sq=7168, sk=8192, sv=8448, ga=8704, gb=9728, gc=10752)
HL = 32

COMPUTE = ('pe', 'act', 'dve', 'pool')
ALL_ENG = ('pe', 'act', 'dve', 'pool', 'sp')
NDMASEM = 6


class Op:
    __slots__ = ('eng', 'fn', 'deps', 'dma', 'sem', 'ticket', 'idx', 'prev_ticket', 'semkey')


class Prog:
    def __init__(self, nc, sems, dma_sems, same_engine_sync=True):
        self.nc = nc
        self.sems = sems
        self.dma_sems = dma_sems
        self.count = {e: 0 for e in COMPUTE}
        self.dma_count = {(q, i): 0 for q in dma_sems for i in range(len(dma_sems[q]))}
        self.dma_rr = {q: 0 for q in dma_sems}
        self.waited = {e: {} for e in ALL_ENG}
        self.same_engine_sync = same_engine_sync
        self.ops = []
        self.lw = {}
        self.rd = {}
        self.prev_final = []

    def op(self, eng, fn, r=(), w=(), dma=False):
        deps = set()
        for b in r:
            if b in self.lw:
                deps.add(self.lw[b])
        for b in w:
            if b in self.lw:
                deps.add(self.lw[b])
            for x in self.rd.get(b, ()):
                deps.add(x)
        o = Op()
        o.eng = eng; o.fn = fn; o.dma = dma; o.sem = None; o.ticket = None
        o.idx = len(self.ops)
        o.deps = deps
        self.ops.append(o)
        for b in r:
            self.rd.setdefault(b, []).append(o.idx)
        for b in w:
            self.lw[b] = o.idx
            self.rd[b] = []
        return o.idx

    def dma(self, q, out, in_, r=(), w=(), **kw):
        def fn(e):
            return e.dma_start(out=out, in_=in_, **kw)
        return self.op(q, fn, r, w, dma=True)

    def emit(self, final_wait=False):
        nc = self.nc
        ops = self.ops
        needed = set()
        for o in ops:
            for d in o.deps:
                od = ops[d]
                if od.eng == o.eng and not od.dma:
                    if od.eng == 'pe' or not self.same_engine_sync:
                        continue
                needed.add(d)
        last = {}
        for o in ops:
            if not o.dma:
                last[o.eng] = o.idx
        for e, i in last.items():
            needed.add(i)
        for o in ops:
            if o.dma:
                q = o.eng
                i = self.dma_rr[q]
                self.dma_rr[q] = (i + 1) % len(self.dma_sems[q])
                o.prev_ticket = self.dma_count[(q, i)]
                self.dma_count[(q, i)] += 16
                o.sem = self.dma_sems[q][i]
                o.semkey = ('d', q, i)
                o.ticket = self.dma_count[(q, i)]
            elif o.idx in needed:
                self.count[o.eng] += 1
                o.sem = self.sems[o.eng]
                o.semkey = ('c', o.eng)
                o.ticket = self.count[o.eng]
        by_eng = {e: [o for o in ops if o.eng == e] for e in ALL_ENG}
        prev_final = self.prev_final
        waited = self.waited
        same_sync = self.same_engine_sync

        def run(engname, eh):
            wd = waited[engname]

            def wait(semkey, sem, ticket):
                if wd.get(semkey, 0) >= ticket:
                    return
                eh.wait_ge(sem, ticket)
                wd[semkey] = ticket
            for (semkey, sem, ticket) in prev_final:
                if semkey == ('c', engname) and (engname == 'pe' or not same_sync):
                    continue
                wait(semkey, sem, ticket)
            for o in by_eng[engname]:
                for d in sorted(o.deps):
                    od = ops[d]
                    if od.ticket is None:
                        continue
                    if od.eng == o.eng and not od.dma and (engname == 'pe' or not same_sync):
                        continue
                    wait(od.semkey, od.sem, od.ticket)
                if o.dma and o.prev_ticket > 0:
                    wait(o.semkey, o.sem, o.prev_ticket)
                inst = o.fn(eh)
                if o.ticket is not None:
                    inst.then_inc(o.sem, 16 if o.dma else 1)
            if engname in self.dma_sems:
                for i, sem in enumerate(self.dma_sems[engname]):
                    t = self.dma_count[(engname, i)]
                    if t > 0:
                        wait(('d', engname, i), sem, t)
            if final_wait and engname == 'sp':
                for q in self.dma_sems:
                    for i, sem in enumerate(self.dma_sems[q]):
                        t = self.dma_count[(q, i)]
                        if t > 0:
                            wait(('d', q, i), sem, t)

        with nc.Block() as block:
            @block.tensor
            def _(e):
                run('pe', e)

            @block.scalar
            def _(e):
                run('act', e)

            @block.vector
            def _(e):
                run('dve', e)

            @block.gpsimd
            def _(e):
                run('pool', e)

            @block.sync
            def _(e):
                run('sp', e)
        fin = []
        for e in COMPUTE:
            if self.count[e] > 0:
                fin.append((('c', e), self.sems[e], self.count[e]))
        for (q, i), t in self.dma_count.items():
            if t > 0:
                fin.append((('d', q, i), self.dma_sems[q][i], t))
        self.prev_final = fin
        self.ops = []
        self.lw = {}
        self.rd = {}


class Ctx:
    pass


G_SKIP = set()
import os
HG_LEVEL = int(os.environ.get('HG_LEVEL', '4'))
SW_LEVEL = int(os.environ.get('SW_LEVEL', '3'))
SW_SUB = int(os.environ.get("SW_SUB", "3"))
SW_W = int(os.environ.get("SW_W", "4"))
LN_W = int(os.environ.get("LN_W", "4"))
HG_MERGE = bool(int(os.environ.get("HG_MERGE", "0")))
SAME_SYNC = bool(int(os.environ.get('SAME_SYNC', '1')))


def _consts():
    c = {}
    half = 64
    inv = (1.0 / (10000.0 ** np.linspace(0.0, 1.0, half, dtype=np.float32))).astype(np.float32)
    pos = np.arange(S, dtype=np.float32)
    ang = pos[:, None] * inv[None, :]
    cos = np.cos(ang).astype(np.float32).T
    sin = np.sin(ang).astype(np.float32).T
    c['cosT'] = np.ascontiguousarray(np.concatenate([cos, cos], 0))
    c['sinT'] = np.ascontiguousarray(np.concatenate([sin, sin], 0))
    pm = np.zeros((128, 128), np.float32)
    for d in range(64):
        pm[d + 64, d] = -1.0
        pm[d, d + 64] = 1.0
    c['pm'] = pm
    c['ident'] = np.eye(128, dtype=np.float32)
    lg = np.log(1.0 - 2.0 ** (-5.0 - np.arange(4, dtype=np.float64)))
    idx = np.arange(128, dtype=np.float64)
    dt = np.zeros((128, 4, 128), np.float32)
    qd = np.zeros((128, 4, 128), np.float32)
    kd = np.zeros((128, 4), np.float32)
    for h in range(4):
        diff = idx[None, :] - idx[:, None]
        dt[:, h, :] = np.where(diff >= 0, np.exp(np.maximum(diff, 0) * lg[h]), 0.0) * 128 ** -0.5
        qd[:, h, :] = np.exp((idx + 1.0) * lg[h])[None, :]
        kd[:, h] = np.exp((127.0 - idx) * lg[h]) * 128 ** -0.5
    c['ret_dt'] = dt
    c['ret_qd'] = qd
    c['ret_kd'] = kd
    c['ret_cd'] = [float(np.exp(128.0 * lg[h])) for h in range(4)]
    s_ = np.arange(128)
    bm = ((s_[:, None] // HL) == (s_[None, :] // HL)) & (s_[:, None] <= s_[None, :])
    c['hg_bm'] = bm.astype(np.float32)
    sm = np.ones((128, 512), np.float32)
    sm[:, ::HL] = 0.0
    c['hg_scanm'] = sm
    c['hg_blk'] = (s_[:, None] // HL == np.arange(128 // HL)[None, :]).astype(np.float32)
    NEG = -30000.0
    m1 = np.zeros((128, 256), np.float32)
    m1[:64, 192:256] = NEG
    m1[64:, 0:64] = NEG
    m0 = m1.copy()
    m0[:, 0:128] = NEG
    c['swa_mask'] = np.stack([m0, m1], 1)
    return c


CONST_NAMES = ['cosT', 'sinT', 'pm', 'ident', 'ret_dt', 'ret_qd', 'ret_kd', 'hg_bm', 'hg_scanm', 'hg_blk', 'swa_mask']
W_SPECS = [
    ('ln_in_g', [D]), ('ln_in_b', [D]), ('w_in', [DEPTH, D, N_IN]), ('ret_w_out', [DEPTH, D, D]),
    ('hgrn_lower_bounds', [DEPTH, D]), ('hgrn_norm_g', [DEPTH, 128]), ('hgrn_w_out', [DEPTH, D, D]),
    ('swa_sinks', [DEPTH, 16]), ('swa_w_out', [DEPTH, D, D]), ('w_o', [DEPTH, D, D]),
    ('ln_mix_g', [DEPTH, D]), ('ln_mix_b', [DEPTH, D]),
    ('ffn_w_gate', [1, D, D_FF]), ('ffn_w_up', [1, D, D_FF]), ('ffn_w_down', [1, D_FF, D]),
    ('moe_router', [1, D, NEXP]), ('moe_w_gate', [1, NEXP, D, D_EXP]), ('moe_w_up', [1, NEXP, D, D_EXP]),
    ('moe_w_down', [1, NEXP, D_EXP, D]), ('ln_ffn_g', [DEPTH, D]), ('ln_ffn_b', [DEPTH, D]),
]


def build(stop_after=None, debug=False):
    C = _consts()
    nc = bass.Bass("TRN2", target_bir_lowering=False)
    G = Ctx()
    G.nc = nc
    G.x = nc.dram_tensor("x", [S, D], F32, kind="ExternalInput").ap()
    G.W = {}
    for name, shp in W_SPECS:
        G.W[name] = nc.dram_tensor(name, shp, F32, kind="ExternalInput").ap()
    G.K = {}
    for name in CONST_NAMES:
        G.K[name] = nc.dram_tensor("c_" + name, list(C[name].shape), F32, kind="ExternalInput").ap()
    G.out = nc.dram_tensor("out", [S, D], F32, kind="ExternalOutput").ap()
    skind = "ExternalOutput" if debug else "Internal"
    G.h_res = nc.dram_tensor("h_res", [S, D], F32, kind=skind).ap()
    G.xT = {b: nc.dram_tensor("x%sT" % b, [D, S], BF16, kind=skind).ap() for b in 'abc'}
    G.mgT = nc.dram_tensor("mgT", [D, S], BF16, kind=skind).ap()
    G.cd = C['ret_cd']

    with contextlib.ExitStack() as es:
        uid = [0]

        def sb(name, shape, dt, st=es):
            uid[0] += 1
            return st.enter_context(nc.sbuf_tensor("%s_u%d" % (name, uid[0]), shape, dt))
        sems = {e: es.enter_context(nc.semaphore("s_" + e)) for e in COMPUTE}
        dsems = {q: [es.enter_context(nc.semaphore("d_%s%d" % (q, i))) for i in range(NDMASEM)]
                 for q in ('sp', 'pool')}
        P = Prog(nc, sems, dsems, same_engine_sync=SAME_SYNC)
        G.P = P
        G.sb = sb
        G.hT = sb("hT", [128, KC, S], BF16)
        G.ident_f = sb("ident_f", [128, 128], F32)
        G.ident_b = sb("ident_b", [128, 128], BF16)
        G.ws = [sb("ws%d" % i, [128, KC, 512], BF16) for i in range(4)]
        G.ws_rr = 0
        G.xt_rr = 0
        G.small = sb("small", [128, 64], F32)
        G.ps = [es.enter_context(nc.psum_tensor("ps%d" % i, [128, 512], F32)) for i in range(8)]
        G.ps_rr = 0
        G.gate_all = sb("gate_all", [128, NT, NEXP], F32)

        phases = []
        phases.append(('ln_in', lambda: phase_ln_in(G)))
        for l in range(DEPTH):
            phases.append(('ret%d' % l, lambda l=l: phase_ret(G, l)))
            phases.append(('hgrn%d' % l, lambda l=l: phase_hgrn(G, l)))
            phases.append(('swa%d' % l, lambda l=l: phase_swa(G, l)))
            phases.append(('merge%d' % l, lambda l=l: phase_merge(G, l)))
            phases.append(('wo%d' % l, lambda l=l: phase_wo(G, l)))
            phases.append(('ffn%d' % l, lambda l=l: phase_ffn(G, l)))
        for i, (name, fn) in enumerate(phases):
            if G_SKIP and name.rstrip('0123456789') in G_SKIP and name != stop_after:
                continue
            fn()
            lastp = (i == len(phases) - 1) or (name == stop_after)
            if not lastp and not G_SKIP:
                nxt = phases[i + 1][0]
                ln_ = int(nxt[-1]) if nxt[-1].isdigit() else 0
                kind = nxt.rstrip('0123456789')
                w_in_n = G.W['w_in'][ln_]
                if kind == 'ret':
                    prefetch_w(G, ('rv', ln_, 0), w_in_n, OFF['rv'], 512)
                elif kind == 'hgrn':
                    prefetch_w(G, ('hi', ln_, 0), w_in_n, OFF['hi'], 512)
                elif kind == 'swa':
                    prefetch_w(G, ('sq', ln_, 0), w_in_n, OFF['sq'], 512)
                elif kind == 'merge':
                    G.ws_rr = 0
                    prefetch_w(G, ('my', ln_, 'a', 0), G.W['ret_w_out'][ln_], 0, 512)
                    prefetch_w(G, ('my', ln_, 'b', 0), G.W['hgrn_w_out'][ln_], 0, 512)
                    prefetch_w(G, ('my', ln_, 'c', 0), G.W['swa_w_out'][ln_], 0, 512)
                    prefetch_w(G, ('mg', ln_, 'a', 0), w_in_n, OFF['ga'], 512)
                elif kind == 'wo':
                    prefetch_w(G, ('wo', ln_), G.W['w_o'][ln_], 0, 512)
                elif kind == 'ffn':
                    if ln_ % 2 == 1:
                        prefetch_w(G, ('ffn', ln_, 0, 0), G.W['moe_w_gate'][ln_ // 2][0], 0, 512)
                    else:
                        prefetch_w(G, ('ffn', ln_, None, 0), G.W['ffn_w_gate'][ln_ // 2], 0, 512)
            P.emit(final_wait=lastp)
            if lastp:
                break
    return nc


def next_ps(G):
    i = G.ps_rr % len(G.ps_sel)
    G.ps_rr = (i + 1) % len(G.ps_sel)
    j = G.ps_sel[i]
    return G.ps[j], ('ps', j)


def next_ws(G):
    i = G.ws_rr
    G.ws_rr = (i + 1) % len(G.ws)
    return G.ws[i], ('ws', i)


def load_w(G, wdram, c0, ncols, q='pool', tag=None):
    pf = getattr(G, 'prefetched', None)
    if tag is not None and pf and tag in pf:
        wt, key = pf.pop(tag)
        return wt, key
    wt, key = next_ws(G)
    G.P.dma(q, wt[:, :, 0:ncols], wdram[:, c0:c0 + ncols].rearrange("(k p) c -> p k c", p=128), w=[key])
    return wt, key


def prefetch_w(G, tag, wdram, c0, ncols):
    if len(G.ws) != 4:
        return
    wt, key = load_w(G, wdram, c0, ncols)
    if getattr(G, 'prefetched', None) is None:
        G.prefetched = {}
    G.prefetched[tag] = (wt, key)


def fm_group(G, wt, wkey, cc, tg, rkeys=('hT',), src=None, ncols_tok=512, t0=None):
    src = G.hT if src is None else src
    ps, pk = next_ps(G)
    t0 = tg * 512 if t0 is None else t0

    def fn(e, ps=ps, wt=wt, cc=cc, t0=t0, src=src):
        ins = None
        for k in range(KC):
            ins = e.matmul(ps[:, 0:ncols_tok], wt[:, k, cc * 128:(cc + 1) * 128], src[:, k, t0:t0 + ncols_tok],
                           start=(k == 0), stop=(k == KC - 1))
        return ins
    G.P.op('pe', fn, r=[wkey] + list(rkeys), w=[pk])
    return ps, pk


def tm_group(G, wt, wkey, tile, ncols=512, rkeys=('hT',), src=None):
    src = G.hT if src is None else src
    ps, pk = next_ps(G)

    def fn(e, ps=ps, wt=wt, tile=tile, src=src):
        ins = None
        for k in range(KC):
            ins = e.matmul(ps[:, 0:ncols], src[:, k, tile * 128:(tile + 1) * 128], wt[:, k, 0:ncols],
                           start=(k == 0), stop=(k == KC - 1))
        return ins
    G.P.op('pe', fn, r=[wkey] + list(rkeys), w=[pk])
    return ps, pk


def load_consts_common(G):
    P = G.P
    P.dma('sp', G.ident_f[:, :], G.K['ident'], w=['ident_f'])
    P.dma('pool', G.ident_b[:, :], G.K['ident'], w=['ident_b'])


def rr(gens, width):
    active = []
    it = iter(gens)
    done = False
    while True:
        while len(active) < width and not done:
            try:
                active.append(next(it))
            except StopIteration:
                done = True
        if not active:
            break
        for g in list(active):
            try:
                next(g)
            except StopIteration:
                active.remove(g)


def ln_tile(G, xt, xkey, tile, slot=0, out_final=None, pss=None):
    P = G.P
    sm = G.small
    o = slot * 16
    st = sm[:, o:o + 12]
    mv = sm[:, o + 12:o + 14]
    rs = sm[:, o + 14:o + 15]
    k = lambda n: (n, slot)
    P.op('dve', lambda e: e.bn_stats(st[:, 0:6], xt[:, 0:512]), r=[xkey], w=[k('ln_st0')])
    P.op('dve', lambda e: e.bn_stats(st[:, 6:12], xt[:, 512:1024]), r=[xkey], w=[k('ln_st1')])
    yield
    P.op('dve', lambda e: e.bn_aggr(mv, st), r=[k('ln_st0'), k('ln_st1')], w=[k('ln_mv')])
    yield
    P.op('act', lambda e: e.activation(rs, mv[:, 1:2], AF.Sqrt, bias=LN_EPS, scale=1.0), r=[k('ln_mv')], w=[k('ln_rs')])
    yield
    P.op('dve', lambda e: e.reciprocal(rs, rs), r=[k('ln_rs')], w=[k('ln_rs')])
    yield
    P.op('dve', lambda e: e.scalar_tensor_tensor(xt[:, :], xt[:, :], mv[:, 0:1], G.gbc[:, :], ALU.subtract, ALU.mult),
         r=[xkey, k('ln_mv'), 'gbc'], w=[xkey])
    yield
    P.op('dve', lambda e: e.scalar_tensor_tensor(xt[:, :], xt[:, :], rs, G.bbc[:, :], ALU.mult, ALU.add),
         r=[xkey, k('ln_rs'), 'bbc'], w=[xkey])
    yield
    rows = slice(tile * 128, (tile + 1) * 128)
    if out_final is not None:
        P.dma('sp', out_final[rows, :], xt[:, :], r=[xkey], w=[('out', tile)])
    else:
        P.dma('sp', G.h_res[rows, :], xt[:, :], r=[xkey], w=[('h_res', tile)])
        for hb in range(2):
            ps, pk = next_ps(G)

            def fn(e, ps=ps, hb=hb):
                ins = None
                for i in range(4):
                    kk = hb * 4 + i
                    ins = e.matmul(ps[:, i * 128:(i + 1) * 128], xt[:, kk * 128:(kk + 1) * 128], G.ident_f[:, :],
                                   start=True, stop=True)
                return ins
            P.op('pe', fn, r=[xkey, 'ident_f'], w=[pk])
            yield
            P.op('act', lambda e, ps=ps, hb=hb: e.activation(
                G.hT[:, hb * 4:(hb + 1) * 4, tile * 128:(tile + 1) * 128],
                ps[:, :].rearrange("p (a t) -> p a t", t=128), AF.Copy), r=[pk], w=['hT'])
            if pss is not None:
                pss.append((ps, pk))
            yield


def load_ln_params(G, g_ap, b_ap):
    G.P.dma('sp', G.gbc[:, :], g_ap.partition_broadcast(128), w=['gbc'])
    G.P.dma('sp', G.bbc[:, :], b_ap.partition_broadcast(128), w=['bbc'])


def next_xt(G):
    i = G.xt_rr
    G.xt_rr = (i + 1) % len(G.xt)
    return G.xt[i], ('xt', i)


def phase_ln_in(G):
    P = G.P
    G.ps_sel = list(range(8))
    with contextlib.ExitStack() as st:
        sb = lambda n, s, d: G.sb(n, s, d, st)
        G.xt = [sb("i_xt%d" % i, [128, D], F32) for i in range(LN_W + 1)]
        G.gbc = sb("i_gbc", [128, D], F32)
        G.bbc = sb("i_bbc", [128, D], F32)
        load_consts_common(G)
        load_ln_params(G, G.W['ln_in_g'], G.W['ln_in_b'])
        def unit(t):
            xt, xk = next_xt(G)
            P.dma('sp', xt[:, :], G.x[t * 128:(t + 1) * 128, :], w=[xk])
            yield
            yield from ln_tile(G, xt, xk, t, slot=t % LN_W)
        rr((unit(t) for t in range(NT)), LN_W)


def phase_ret(G, l):
    P = G.P
    nc = G.nc
    w_in = G.W['w_in'][l]
    G.ps_sel = [0, 1]
    with contextlib.ExitStack() as st:
        sb = lambda n, s, d: G.sb(n, s, d, st)
        v_tok = sb("r_v", [128, NT, 512], BF16)
        g_tok = sb("r_g", [128, NT, 512], BF16)
        qT = sb("r_q", [128, 2, S], BF16)
        kT = sb("r_k", [128, 2, S], BF16)
        cs = [sb("r_cs%d" % i, [128, 2, 512], F32) for i in range(2)]
        xf = [sb("r_xf%d" % i, [128, 512], F32) for i in range(2)]
        t1 = [sb("r_t1%d" % i, [128, 512], F32) for i in range(2)]
        pm = sb("r_pm", [128, 128], F32)
        dtm = sb("r_dt", [128, 4, 128], F32)
        qdm = sb("r_qd", [128, 4, 128], F32)
        kdm = sb("r_kd", [128, 4], F32)
        scm = [sb("r_scm%d" % i, [128, 128], BF16) for i in range(2)]
        qd = [sb("r_qdd%d" % i, [128, 128], BF16) for i in range(2)]
        kd = [sb("r_kdd%d" % i, [128, 128], BF16) for i in range(2)]
        stf = [sb("r_stf%d" % i, [128, 256], F32) for i in range(2)]
        stb = [sb("r_stb%d" % i, [128, 256], BF16) for i in range(2)]
        xg = [sb("r_xg%d" % i, [128, 512], BF16) for i in range(2)]
        xo = [sb("r_xo%d" % i, [128, 4, 128], BF16) for i in range(2)]
        ss = sb("r_ss", [128, 4], F32)
        junk = sb("r_junk", [128, 256], F32)
        ps_o = [G.ps[2], G.ps[3]]
        ps_s = [G.ps[4], G.ps[5]]
        ps_m = [G.ps[6], G.ps[7]]
        P.dma('sp', pm[:, :], G.K['pm'], w=['pm'])
        P.dma('sp', dtm[:, :, :], G.K['ret_dt'], w=['dtm'])
        P.dma('sp', qdm[:, :, :], G.K['ret_qd'], w=['qdm'])
        P.dma('sp', kdm[:, :], G.K['ret_kd'], w=['kdm'])
        for hp in range(2):
            G.ps_sel = list(range(8))
            wt, wk = load_w(G, w_in, OFF['rv'] + hp * 512, 512, tag=('rv', l, hp))
            for t in range(NT):
                ps, pk = tm_group(G, wt, wk, t)
                P.op('act', lambda e, ps=ps, t=t: e.activation(v_tok[:, t, :], ps[:, :], AF.Copy), r=[pk], w=[('v', t)])
            wt, wk = load_w(G, w_in, OFF['rg'] + hp * 512, 512)
            for t in range(NT):
                ps, pk = tm_group(G, wt, wk, t)
                P.op('act', lambda e, ps=ps, t=t: e.activation(g_tok[:, t, :], ps[:, :], AF.Silu), r=[pk], w=[('g', t)])
            for (nm, dst) in (('rq', qT), ('rk', kT)):
                wt, wk = load_w(G, w_in, OFF[nm] + hp * 256, 256)
                for tg in range(4):
                    ci = tg % 2
                    P.dma('sp', cs[ci][:, 0, :], G.K['cosT'][:, tg * 512:(tg + 1) * 512], w=[('cs', ci, 0)])
                    P.dma('sp', cs[ci][:, 1, :], G.K['sinT'][:, tg * 512:(tg + 1) * 512], w=[('cs', ci, 1)])
                    for j in range(2):
                        bi = (tg * 2 + j) % 2
                        ps, pk = fm_group(G, wt, wk, j, tg)
                        P.op('act', lambda e, ps=ps, bi=bi: e.activation(xf[bi][:, :], ps[:, :], AF.Copy), r=[pk], w=[('xf', bi)])
                        ps2, pk2 = next_ps(G)
                        P.op('pe', lambda e, ps2=ps2, bi=bi: e.matmul(ps2[:, :], pm[:, :], xf[bi][:, :], start=True, stop=True),
                             r=[('xf', bi), 'pm'], w=[pk2])
                        P.op('dve', lambda e, bi=bi, ci=ci: e.tensor_tensor(t1[bi][:, :], xf[bi][:, :], cs[ci][:, 0, :], ALU.mult),
                             r=[('xf', bi), ('cs', ci, 0)], w=[('t1', bi)])
                        P.op('dve', lambda e, ps2=ps2, bi=bi, ci=ci: e.tensor_tensor(xf[bi][:, :], ps2[:, :], cs[ci][:, 1, :], ALU.mult),
                             r=[pk2, ('cs', ci, 1)], w=[('xf', bi)])
                        P.op('dve', lambda e, bi=bi, dst=dst, j=j, tg=tg: e.tensor_tensor(
                            dst[:, j, tg * 512:(tg + 1) * 512], t1[bi][:, :], xf[bi][:, :], ALU.add),
                            r=[('t1', bi), ('xf', bi)], w=[(nm, j, tg)])
            G.ps_sel = [0, 1]
            for n in range(NT):
                pso = ps_o[n % 2]
                pok = ('ps', 2 + n % 2)
                tgk = n // 4
                cols = slice(n * 128, (n + 1) * 128)
                for j in range(2):
                    h = hp * 2 + j
                    psm = ps_m[j]
                    pmk = ('ps', 6 + j)
                    pss_ = ps_s[j]
                    psk = ('ps', 4 + j)
                    P.op('pe', lambda e, pss_=pss_, j=j, cols=cols: e.matmul(pss_[:, 0:128], kT[:, j, cols], qT[:, j, cols], start=True, stop=True),
                         r=[('rk', j, tgk), ('rq', j, tgk)], w=[psk])
                    P.op('dve', lambda e, pss_=pss_, j=j, h=h: e.tensor_tensor(scm[j][:, :], pss_[:, 0:128], dtm[:, h, :], ALU.mult),
                         r=[psk, 'dtm'], w=[('scm', j)])
                    if n > 0:
                        P.op('dve', lambda e, j=j, h=h, cols=cols: e.tensor_tensor(qd[j][:, :], qT[:, j, cols], qdm[:, h, :], ALU.mult),
                             r=[('rq', j, tgk), 'qdm'], w=[('qd', j)])

                    def fo(e, pso=pso, j=j, n=n):
                        ins = e.matmul(pso[:, j * 256:(j + 1) * 256], scm[j][:, :], v_tok[:, n, j * 256:(j + 1) * 256],
                                       start=True, stop=(n == 0))
                        if n > 0:
                            ins = e.matmul(pso[:, j * 256:(j + 1) * 256], qd[j][:, :], stb[j][:, :], start=False, stop=True)
                        return ins
                    P.op('pe', fo, r=[('scm', j), ('v', n), ('qd', j), ('stb', j)], w=[pok])
                    if n < NT - 1:
                        P.op('pe', lambda e, psm=psm, j=j, cols=cols: e.matmul(psm[:, 0:128], kT[:, j, cols], G.ident_b[:, :], start=True, stop=True),
                             r=[('rk', j, tgk), 'ident_b'], w=[pmk])
                        P.op('act', lambda e, psm=psm, j=j, h=h: e.activation(kd[j][:, :], psm[:, 0:128], AF.Copy, scale=kdm[:, h:h + 1]),
                             r=[pmk, 'kdm'], w=[('kd', j)])
                        P.op('pe', lambda e, psm=psm, j=j, n=n: e.matmul(psm[:, 128:384], kd[j][:, :], v_tok[:, n, j * 256:(j + 1) * 256], start=True, stop=True),
                             r=[('kd', j), ('v', n)], w=[pmk])
                        if n == 0:
                            P.op('dve', lambda e, psm=psm, j=j: e.tensor_copy(stf[j][:, :], psm[:, 128:384]), r=[pmk], w=[('stf', j)])
                        else:
                            P.op('dve', lambda e, psm=psm, j=j, h=h: e.scalar_tensor_tensor(
                                stf[j][:, :], stf[j][:, :], G.cd[h], psm[:, 128:384], ALU.mult, ALU.add),
                                r=[pmk, ('stf', j)], w=[('stf', j)])
                        P.op('act', lambda e, j=j: e.activation(stb[j][:, :], stf[j][:, :], AF.Copy), r=[('stf', j)], w=[('stb', j)])
                xi = n % 2
                for j in range(2):
                    P.op('act', lambda e, pso=pso, j=j: e.activation(junk[:, :], pso[:, j * 256:(j + 1) * 256], AF.Square, accum_out=ss[:, j:j + 1]),
                         r=[pok], w=['junk', ('ss', j)])
                P.op('act', lambda e: e.activation(ss[:, 2:4], ss[:, 0:2], AF.Sqrt, bias=RMS_EPS, scale=1.0 / 256.0),
                     r=[('ss', 0), ('ss', 1)], w=['rstd'])
                P.op('dve', lambda e: e.reciprocal(ss[:, 2:4], ss[:, 2:4]), r=['rstd'], w=['rstd'])
                for j in range(2):
                    P.op('dve', lambda e, pso=pso, j=j, n=n, xi=xi: e.scalar_tensor_tensor(
                        xg[xi][:, j * 256:(j + 1) * 256], pso[:, j * 256:(j + 1) * 256], ss[:, 2 + j:3 + j],
                        g_tok[:, n, j * 256:(j + 1) * 256], ALU.mult, ALU.mult),
                        r=[pok, 'rstd', ('g', n)], w=[('xg', xi, j)])
                ps, pk = next_ps(G)

                def ft(e, ps=ps, xi=xi):
                    ins = None
                    for i in range(4):
                        ins = e.matmul(ps[:, i * 128:(i + 1) * 128], xg[xi][:, i * 128:(i + 1) * 128], G.ident_b[:, :], start=True, stop=True)
                    return ins
                P.op('pe', ft, r=[('xg', xi, 0), ('xg', xi, 1), 'ident_b'], w=[pk])
                P.op('act', lambda e, ps=ps, xi=xi: e.activation(xo[xi][:, :, :], ps[:, :].rearrange("p (a t) -> p a t", t=128), AF.Copy),
                     r=[pk], w=[('xo', xi)])
                P.dma('sp', G.xT['a'][hp * 512:(hp + 1) * 512, cols].rearrange("(i p) t -> p i t", p=128), xo[xi][:, :, :],
                      r=[('xo', xi)], w=[('xaT', hp, n)])


def phase_hgrn(G, l):
    P = G.P
    w_in = G.W['w_in'][l]
    G.ps_sel = [0, 1]
    with contextlib.ExitStack() as st:
        sb = lambda n, s, d: G.sb(n, s, d, st)
        v_tok = sb("h_v", [128, NT, 512], BF16)
        qtT = sb("h_qt", [128, 4, S], BF16)
        ktT = sb("h_kt", [128, 4, S], BF16)
        ksT = sb("h_ks", [128, 4, S], BF16)
        sgT = sb("h_sg", [128, 4, S], BF16)
        bdT = sb("h_bd", [128, 4, 64], F32)
        T = [[sb("h_t%d_%d" % (a, i), [128, 512], F32) for i in range(5)] for a in range(2)]
        lbr = sb("h_lbr", [128, 2, 8], F32)
        lbt = sb("h_lb", [128, 8], F32)
        oml = sb("h_oml", [128, 8], F32)
        gn = sb("h_gn", [128, 1], F32)
        bm = sb("h_bm", [128, 128], F32)
        scanm = sb("h_scanm", [128, 512], F32)
        ones_f = sb("h_ones", [128, 128], BF16)
        am = [[sb("h_am%d_%d" % (a, j), [128, 128], BF16) for j in range(4)] for a in range(2)]
        kst = [[sb("h_kst%d_%d" % (a, j), [128, 128], BF16) for j in range(4)] for a in range(2)]
        stf = [sb("h_stf%d" % j, [128, 128], F32) for j in range(4)]
        stb = [[sb("h_stb%d_%d" % (j, a), [128, 128], BF16) for a in range(2)] for j in range(4)]
        osb = [sb("h_osb%d" % a, [128, 512], F32) for a in range(2)]
        sq = [sb("h_sq%d" % a, [128, 512], BF16) for a in range(2)]
        rt = [sb("h_rt%d" % a, [128, 512], F32) for a in range(2)]
        xo = [sb("h_xo%d" % i, [128, 4, 128], BF16) for i in range(2)]
        blkm = sb("h_blkm", [128, 128 // HL], F32)
        vblk = [[sb("h_vblk%d_%d" % (a, j), [128, 128 // HL, 128], BF16) for j in range(4)] for a in range(2)]
        psU = [G.ps[2], G.ps[3], G.ps[6], G.ps[7]]
        P.dma('sp', blkm[:, :], G.K['hg_blk'], w=['blkm'])
        P.dma('sp', bm[:, :], G.K['hg_bm'], w=['bm'])
        P.dma('sp', scanm[:, :], G.K['hg_scanm'], w=['scanm'])
        P.dma('sp', gn[:, :], G.W['hgrn_norm_g'][l].rearrange("(p o) -> p o", o=1), w=['gn'])
        P.op('pool', lambda e: e.memset(ones_f[:, :], 1.0), w=['ones_f'])
        if l == 0:
            P.op('pool', lambda e: e.memset(lbt[:, :], 0.0), w=['lbt'])
        else:
            for a in range(2):
                P.dma('sp', lbr[:, a, :], G.W['hgrn_lower_bounds'][a].rearrange("(h d) -> d h", d=128), w=[('lbr', a)],
                      allow_slow_non_contiguous=True)
            P.op('dve', lambda e: e.tensor_tensor(lbt[:, :], lbr[:, 1, :], lbr[:, 0, :], ALU.subtract), r=[('lbr', 0), ('lbr', 1)], w=['lbt'])
            P.op('act', lambda e: e.activation(lbt[:, :], lbt[:, :], AF.Sigmoid), r=['lbt'], w=['lbt'])
        P.op('dve', lambda e: e.tensor_scalar(oml[:, :], lbt[:, :], -1.0, 1.0, ALU.mult, ALU.add), r=['lbt'], w=['oml'])
        NB = 128 // HL
        for hg in range(2):
            G.ps_sel = [0, 1, 2, 3, 6, 7]
            wt, wk = load_w(G, w_in, OFF['hi'] + hg * 512, 512, tag=('hi', l, hg))
            for t in range(NT):
                ps, pk = tm_group(G, wt, wk, t)
                P.op('act', lambda e, ps=ps, t=t: e.activation(v_tok[:, t, :], ps[:, :], AF.Copy), r=[pk], w=[('v', t)])
            wq, wqk = load_w(G, w_in, OFF['hq'] + hg * 512, 512)
            wf, wfk = load_w(G, w_in, OFF['hf'] + hg * 512, 512)
            wg, wgk = load_w(G, w_in, OFF['hg'] + hg * 512, 512)

            def h2unit(j, tg, a):
                h = hg * 4 + j
                t1, t2, t3, t4, t5 = T[a]
                tk = lambda i, a=a: ('T', a, i)
                cols = slice(tg * 512, (tg + 1) * 512)
                psz, pkz = fm_group(G, wf, wfk, j, tg)
                yield
                P.op('act', lambda e: e.activation(t2[:, :], psz[:, :], AF.Exp, scale=-1.0), r=[pkz], w=[tk(2)])
                yield
                P.op('act', lambda e: e.activation(t1[:, :], t2[:, :], AF.Ln, bias=1.0, scale=1.0), r=[tk(2)], w=[tk(1)])
                yield
                P.op('act', lambda e: e.activation(t1[:, :], t1[:, :], AF.Exp, scale=-1.0), r=[tk(1)], w=[tk(1)])
                yield
                P.op('dve', lambda e: e.tensor_tensor(t2[:, :], t2[:, :], t1[:, :], ALU.mult), r=[tk(1), tk(2)], w=[tk(2)])
                psq, pkq = fm_group(G, wq, wqk, j, tg)
                yield
                P.op('dve', lambda e: e.tensor_scalar(t1[:, :], t1[:, :], oml[:, h:h + 1], lbt[:, h:h + 1], ALU.mult, ALU.add),
                     r=[tk(1), 'oml', 'lbt'], w=[tk(1)])
                yield
                P.op('act', lambda e: e.activation(t3[:, :], t1[:, :], AF.Ln), r=[tk(1)], w=[tk(3)])
                yield
                P.op('dve', lambda e: e.tensor_tensor_scan(t4[:, :], scanm[:, :], t3[:, :], 0.0, ALU.mult, ALU.add),
                     r=[tk(3), 'scanm'], w=[tk(4)])
                yield
                P.op('act', lambda e: e.activation(t1[:, :], t4[:, :], AF.Exp), r=[tk(4)], w=[tk(1)])
                P.op('act', lambda e: e.activation(t3[:, :], t4[:, :], AF.Exp, scale=-1.0), r=[tk(4)], w=[tk(3)])
                yield
                P.op('dve', lambda e: e.tensor_tensor(qtT[:, j, cols], psq[:, :], t1[:, :], ALU.mult),
                     r=[pkq, tk(1)], w=[('qt', j, tg)])
                yield
                P.op('dve', lambda e: e.scalar_tensor_tensor(t2[:, :], t2[:, :], oml[:, h:h + 1], t3[:, :], ALU.mult, ALU.mult),
                     r=[tk(2), tk(3), 'oml'], w=[tk(2)])
                yield
                P.op('act', lambda e: e.activation(ktT[:, j, cols], t2[:, :], AF.Copy), r=[tk(2)], w=[('kt', j, tg)])
                yield
                P.op('dve', lambda e: e.tensor_tensor(
                    ksT[:, j, cols].rearrange("p (b l) -> p b l", l=HL),
                    t2[:, :].rearrange("p (b l) -> p b l", l=HL),
                    t1[:, :].rearrange("p (b l) -> p b l", l=HL)[:, :, HL - 1:HL].broadcast_to([128, 512 // HL, HL]), ALU.mult),
                    r=[tk(1), tk(2)], w=[('ks', j, tg)])
                yield
                nb = 512 // HL
                P.op('dve', lambda e: e.tensor_copy(
                    bdT[:, j, tg * nb:(tg + 1) * nb],
                    t1[:, :].rearrange("p (b l) -> p b l", l=HL)[:, :, HL - 1:HL].rearrange("p b o -> p (b o)")),
                    r=[tk(1)], w=[('bd', j, tg)])
                yield
            for j in range(4):
                for tg in range(4):
                    psg, pkg = fm_group(G, wg, wgk, j, tg)
                    P.op('act', lambda e, psg=psg, j=j, tg=tg: e.activation(sgT[:, j, tg * 512:(tg + 1) * 512], psg[:, :], AF.Silu), r=[pkg], w=[('sg', j, tg)])
            units = [(j, tg) for j in range(4) for tg in range(4)]
            rr((h2unit(j, tg, n % 2) for n, (j, tg) in enumerate(units)), 2)
            if HG_LEVEL < 2:
                continue
            G.ps_sel = [0, 1]
            psok = [('ps', 4), ('ps', 5)]
            psUk = [('ps', 2), ('ps', 3), ('ps', 6), ('ps', 7)]

            def front(i):
                tg = i // 4
                cols = slice(i * 128, (i + 1) * 128)
                a = i % 2
                for j in range(4):
                    ps, pk = next_ps(G)
                    P.op('pe', lambda e, ps=ps, j=j: e.matmul(ps[:, 0:128], ktT[:, j, cols], qtT[:, j, cols], start=True, stop=True),
                         r=[('kt', j, tg), ('qt', j, tg)], w=[pk])
                    P.op('dve', lambda e, ps=ps, j=j: e.tensor_tensor(am[a][j][:, :], ps[:, 0:128], bm[:, :], ALU.mult), r=[pk, 'bm'], w=[('am', a, j)])
                    ps, pk = next_ps(G)
                    P.op('pe', lambda e, ps=ps, j=j: e.matmul(ps[:, 128:256], ksT[:, j, cols], G.ident_b[:, :], start=True, stop=True),
                         r=[('ks', j, tg), 'ident_b'], w=[pk])
                    P.op('act', lambda e, ps=ps, j=j: e.activation(kst[a][j][:, :], ps[:, 128:256], AF.Copy), r=[pk], w=[('kst', a, j)])
                    P.op('dve', lambda e, j=j: e.tensor_tensor(
                        vblk[a][j][:, :, :], v_tok[:, i, j * 128:(j + 1) * 128].rearrange("p (o v) -> p o v", o=1).broadcast_to([128, NB, 128]),
                        blkm[:, :].rearrange("p (b o) -> p b o", o=1).broadcast_to([128, NB, 128]), ALU.mult),
                        r=[('v', i), 'blkm'], w=[('vblk', a, j)])

            def umat(i):
                a = i % 2
                for j in range(4):
                    P.op('pe', lambda e, j=j: e.matmul(psU[j][:, :], kst[a][j][:, :], vblk[a][j][:, :, :].rearrange("p b v -> p (b v)"), start=True, stop=True),
                         r=[('kst', a, j), ('vblk', a, j)], w=[psUk[j]])

            def mid(i):
                tg = i // 4
                a = i % 2
                pso = G.ps[4 + a]
                pok = psok[a]
                if HG_MERGE:
                    for j in range(4):
                        P.op('pe', lambda e, j=j: e.matmul(pso[:, j * 128:(j + 1) * 128], v_tok[:, i, j * 128:(j + 1) * 128], am[a][j][:, :],
                                                           start=True, stop=False, skip_group_check=True),
                             r=[('v', i), ('am', a, j)], w=[pok])
                for b in range(NB):
                    gb = i * NB + b
                    for j in range(4):
                        def fo(e, j=j, b=b, gb=gb):
                            oc = pso[:, j * 128 + b * HL: j * 128 + (b + 1) * HL]
                            ins = None
                            if not HG_MERGE:
                                ins = e.matmul(oc, v_tok[:, i, j * 128:(j + 1) * 128], am[a][j][:, b * HL:(b + 1) * HL], start=True, stop=(gb == 0))
                            if gb > 0:
                                ins = e.matmul(oc, stb[j][gb % 2][:, :], qtT[:, j, i * 128 + b * HL: i * 128 + (b + 1) * HL], start=False, stop=True,
                                               skip_group_check=HG_MERGE)
                            return ins
                        if HG_MERGE and gb == 0:
                            continue
                        P.op('pe', fo, r=[('v', i), ('am', a, j), ('stb', j, gb % 2), ('qt', j, tg)], w=[pok])
                    if gb < S // HL - 1:
                        for j in range(4):
                            if gb == 0:
                                P.op('dve', lambda e, j=j, b=b: e.tensor_copy(stf[j][:, :], psU[j][:, b * 128:(b + 1) * 128]), r=[psUk[j]], w=[('stf', j)])
                            else:
                                P.op('dve', lambda e, j=j, gb=gb, b=b: e.scalar_tensor_tensor(
                                    stf[j][:, :], stf[j][:, :], bdT[:, j, gb:gb + 1], psU[j][:, b * 128:(b + 1) * 128], ALU.mult, ALU.add),
                                    r=[psUk[j], ('stf', j), ('bd', j, gb // (512 // HL))], w=[('stf', j)])
                            P.op('act', lambda e, j=j, gb=gb: e.activation(stb[j][(gb + 1) % 2][:, :], stf[j][:, :], AF.Copy),
                                 r=[('stf', j)], w=[('stb', j, (gb + 1) % 2)])

            def back(i):
                tg = i // 4
                cols = slice(i * 128, (i + 1) * 128)
                a = i % 2
                pso = G.ps[4 + a]
                pok = psok[a]
                osb_, sq_, rt_ = osb[a], sq[a], rt[a]
                P.op('act', lambda e: e.activation(osb_[:, :], pso[:, :], AF.Copy), r=[pok], w=[('osb', a)])
                P.op('act', lambda e: e.activation(sq_[:, :], pso[:, :], AF.Square), r=[pok], w=[('sq', a)])
                ps, pk = next_ps(G)
                P.op('pe', lambda e, ps=ps: e.matmul(ps[:, :], ones_f[:, :], sq_[:, :], start=True, stop=True), r=[('sq', a), 'ones_f'], w=[pk])
                P.op('act', lambda e, ps=ps: e.activation(rt_[:, :], ps[:, :], AF.Ln, bias=RMS_EPS, scale=1.0 / 128.0), r=[pk], w=[('rt', a)])
                P.op('act', lambda e: e.activation(rt_[:, :], rt_[:, :], AF.Exp, scale=-0.5), r=[('rt', a)], w=[('rt', a)])
                P.op('dve', lambda e: e.scalar_tensor_tensor(osb_[:, :], osb_[:, :], gn[:, 0:1], rt_[:, :], ALU.mult, ALU.mult), r=[('osb', a), ('rt', a), 'gn'], w=[('osb', a)])
                P.op('dve', lambda e: e.tensor_tensor(
                    xo[a][:, :, :], osb_[:, :].rearrange("p (a t) -> p a t", t=128), sgT[:, :, cols], ALU.mult),
                    r=[('osb', a)] + [('sg', j, tg) for j in range(4)], w=[('xo', a)])
                P.dma('sp', G.xT['b'][hg * 512:(hg + 1) * 512, cols].rearrange("(i p) t -> p i t", p=128), xo[a][:, :, :],
                      r=[('xo', a)], w=[('xbT', hg, i)])

            front(0)
            umat(0)
            for sstep in range(NT):
                if sstep + 1 < NT:
                    front(sstep + 1)
                mid(sstep)
                if sstep + 1 < NT:
                    umat(sstep + 1)
                back(sstep)


def phase_swa(G, l):
    P = G.P
    w_in = G.W['w_in'][l]
    G.ps_sel = list(range(8))
    with contextlib.ExitStack() as st:
        sb = lambda n, s, d: G.sb(n, s, d, st)
        qT = sb("s_q", [128, 8, S], BF16)
        kT = sb("s_k", [128, 4, 2, 128 + S], BF16)
        vt = sb("s_v", [128, NT + 1, 4, 2, 128], BF16)
        mask = sb("s_mask", [128, 2, 256], F32)
        sink = sb("s_sink", [128, 16], F32)
        with contextlib.ExitStack() as st1:
            wkd = G.sb("s_wkd", [128, KC, 4, 2, 128], BF16, st1)
            P.dma('sp', mask[:, :, :], G.K['swa_mask'], w=['mask'])
            P.dma('sp', sink[:, :], G.W['swa_sinks'][l].partition_broadcast(128), w=['sink'])
            P.op('pool', lambda e: e.memset(kT[:, :, :, 0:128], 0.0), w=['kpad'])
            P.op('pool', lambda e: e.memset(vt[:, :, :, :, :].rearrange("p a b c d -> p (a b c d)"), 0.0), w=['vt0'])
            P.op('pool', lambda e: e.memset(wkd[:, :, :, :, :].rearrange("p a b c d -> p (a b c d)"), 0.0), w=['wkd0'])
            for g in range(4):
                for var in range(2):
                    P.dma('pool', wkd[:, :, g, var, var * 64:(var + 1) * 64],
                          w_in[:, OFF['sk'] + g * 64: OFF['sk'] + (g + 1) * 64].rearrange("(k p) c -> p k c", p=128),
                          r=['wkd0'], w=[('wkd', g, var)])
            for blk in range(2):
                wt, wk = load_w(G, w_in, OFF['sq'] + blk * 512, 512, tag=('sq', l, blk))
                for cc in range(4):
                    for tg in range(4):
                        ps, pk = fm_group(G, wt, wk, cc, tg)
                        P.op('act', lambda e, ps=ps, c8=blk * 4 + cc, tg=tg: e.activation(qT[:, c8, tg * 512:(tg + 1) * 512], ps[:, :], AF.Copy, scale=0.125),
                             r=[pk], w=[('q', blk * 4 + cc, tg)])
            for g in range(4):
                for var in range(2):
                    for tg in range(4):
                        ps, pk = next_ps(G)

                        def fk(e, ps=ps, g=g, var=var, tg=tg):
                            ins = None
                            for k in range(KC):
                                ins = e.matmul(ps[:, :], wkd[:, k, g, var, :], G.hT[:, k, tg * 512:(tg + 1) * 512], start=(k == 0), stop=(k == KC - 1))
                            return ins
                        P.op('pe', fk, r=[('wkd', g, var), 'hT'], w=[pk])
                        P.op('act', lambda e, ps=ps, g=g, var=var, tg=tg: e.activation(kT[:, g, var, 128 + tg * 512:128 + (tg + 1) * 512], ps[:, :], AF.Copy),
                             r=[pk, 'kpad'], w=[('k', g, var, tg)])
            wt, wk = load_w(G, w_in, OFF['sv'], 256)
            for t in range(NT):
                ps, pk = tm_group(G, wt, wk, t, ncols=256)
                P.op('act', lambda e, ps=ps, t=t: e.activation(vt[:, t + 1, :, 0, 0:64], ps[:, 0:256].rearrange("p (g d) -> p g d", d=64), AF.Copy),
                     r=[pk, 'vt0'], w=[('vt', t + 1, 0)])
                P.op('act', lambda e, ps=ps, t=t: e.activation(vt[:, t + 1, :, 1, 64:128], ps[:, 0:256].rearrange("p (g d) -> p g d", d=64), AF.Copy),
                     r=[pk, 'vt0'], w=[('vt', t + 1, 1)])
            P.emit()
        NSL = SW_W
        sms = [sb("s_sm%d" % i, [128, 4, 256], F32) for i in range(NSL)]
        ps_ = [sb("s_p%d" % i, [128, 4, 256], BF16) for i in range(NSL)]
        mxs = [sb("s_mx%d" % i, [128, 16], F32) for i in range(NSL)]
        pTs = [sb("s_pT%d" % i, [128, 8, 128], BF16) for i in range(NSL)]
        xos = [sb("s_xo%d" % i, [128, 2, 128], BF16) for i in range(NSL)]

        def s2unit(i, g, slot):
            cols = slice(i * 128, (i + 1) * 128)
            mi = 0 if i == 0 else 1
            sm, p, mx, pT, xo = sms[slot], ps_[slot], mxs[slot], pTs[slot], xos[slot]
            bk = [slot * 2, slot * 2 + 1]
            kk = lambda n: (n, slot)
            for a in range(4):
                hq = 4 * g + a
                c8, var = hq // 2, hq % 2
                bank = bk[a // 2]
                P.op('pe', lambda e, a=a, c8=c8, var=var, bank=bank: e.matmul(
                    G.ps[bank][:, (a % 2) * 256:(a % 2 + 1) * 256], qT[:, c8, cols], kT[:, g, var, i * 128:i * 128 + 256], start=True, stop=True),
                    w=[('ps', bank)])
            yield
            for hh in range(2):
                bank = bk[hh]
                P.op('dve', lambda e, hh=hh, bank=bank: e.tensor_tensor(
                    sm[:, hh * 2:(hh + 1) * 2, :], G.ps[bank][:, :].rearrange("p (a k) -> p a k", k=256),
                    mask[:, mi:mi + 1, :].broadcast_to([128, 2, 256]), ALU.add),
                    r=[('ps', bank)], w=[kk(('sm', hh))])
            yield
            P.op('dve', lambda e: e.tensor_reduce(mx[:, 0:4], sm[:, :, :], AX.X, ALU.max), r=[kk(('sm', 0)), kk(('sm', 1))], w=[kk('mx_m')])
            yield
            P.op('dve', lambda e: e.tensor_tensor(mx[:, 0:4], mx[:, 0:4], sink[:, 4 * g:4 * g + 4], ALU.max), r=[kk('mx_m')], w=[kk('mx_m')])
            yield
            P.op('dve', lambda e: e.tensor_scalar(mx[:, 4:8], mx[:, 0:4], -1.0, None, ALU.mult), r=[kk('mx_m')], w=[kk('mx_n')])
            yield
            for a in range(4):
                P.op('act', lambda e, a=a: e.activation(p[:, a, :], sm[:, a, :], AF.Exp, bias=mx[:, 4 + a:5 + a], scale=1.0, accum_out=mx[:, 8 + a:9 + a]),
                     r=[kk(('sm', a // 2)), kk('mx_n')], w=[kk('p'), kk(('mx_s', a))])
            P.op('dve', lambda e: e.tensor_tensor(mx[:, 12:16], sink[:, 4 * g:4 * g + 4], mx[:, 4:8], ALU.add), r=[kk('mx_n')], w=[kk('mx_d')])
            yield
            P.op('act', lambda e: e.activation(mx[:, 12:16], mx[:, 12:16], AF.Exp), r=[kk('mx_d')], w=[kk('mx_d')])
            yield
            P.op('dve', lambda e: e.tensor_tensor(mx[:, 12:16], mx[:, 12:16], mx[:, 8:12], ALU.add), r=[kk('mx_d')] + [kk(('mx_s', a)) for a in range(4)], w=[kk('mx_d')])
            yield
            P.op('dve', lambda e: e.reciprocal(mx[:, 12:16], mx[:, 12:16]), r=[kk('mx_d')], w=[kk('mx_d')])
            yield
            P.op('dve', lambda e: e.tensor_tensor(p[:, :, :], p[:, :, :], mx[:, 12:16].rearrange("p (a o) -> p a o", o=1).broadcast_to([128, 4, 256]), ALU.mult),
                 r=[kk('p'), kk('mx_d')], w=[kk('p')])
            yield
            for hh in range(2):
                def ftr(e, hh=hh):
                    ins = None
                    for q in range(4):
                        idx = hh * 4 + q
                        a, kt = idx // 2, idx % 2
                        ins = e.matmul(G.ps[bk[hh]][:, q * 128:(q + 1) * 128], p[:, a, kt * 128:(kt + 1) * 128], G.ident_b[:, :], start=True, stop=True)
                    return ins
                P.op('pe', ftr, r=[kk('p'), 'ident_b', kk(('sm', hh))], w=[('ps', bk[hh])])
            yield
            P.op('act', lambda e: e.activation(pT[:, 0:4, :], G.ps[bk[0]][:, :].rearrange("p (a t) -> p a t", t=128), AF.Copy), r=[('ps', bk[0])], w=[kk(('pT', 0))])
            P.op('dve', lambda e: e.tensor_copy(pT[:, 4:8, :], G.ps[bk[1]][:, :].rearrange("p (a t) -> p a t", t=128)), r=[('ps', bk[1])], w=[kk(('pT', 1))])
            yield
            for pr in range(2):
                pso = G.ps[bk[pr]]

                def fpv(e, pso=pso, pr=pr):
                    ins = None
                    n = 0
                    for (a, var) in ((2 * pr, 0), (2 * pr + 1, 1)):
                        for kt in range(2):
                            ins = e.matmul(pso[:, 0:128], vt[:, i + kt, g, var, :], pT[:, a * 2 + kt, :], start=(n == 0), stop=(n == 3))
                            n += 1
                    return ins
                P.op('pe', fpv, r=[kk(('pT', pr))], w=[('ps', bk[pr])])
                yield
                P.op('act', lambda e, pso=pso, pr=pr: e.activation(xo[:, pr, :], pso[:, 0:128], AF.Copy), r=[('ps', bk[pr])], w=[kk(('xo', pr))])
                yield
            P.dma('sp', G.xT['c'][g * 256:(g + 1) * 256, cols].rearrange("(i p) t -> p i t", p=128), xo[:, :, :],
                  r=[kk(('xo', 0)), kk(('xo', 1))], w=[('xcT', g, i)])
            yield
        units = [(i, g) for i in range(NT) for g in range(4)]
        rr((s2unit(i, g, n % NSL) for n, (i, g) in enumerate(units)), NSL)


def phase_merge(G, l):
    P = G.P
    G.ps_sel = list(range(8))
    w_in = G.W['w_in'][l]
    with contextlib.ExitStack() as st:
        sb = lambda n, s, d: G.sb(n, s, d, st)
        xs = {b: sb("m_x" + b, [128, KC, S], BF16) for b in 'abc'}
        ws_save = G.ws
        G.ws = list(G.ws) + [sb("m_ws%d" % i, [128, KC, 512], BF16) for i in range(2)]
        G.ws_rr = 4 if getattr(G, 'prefetched', None) else 0
        sg = [sb("m_sg%d" % i, [128, 512], F32) for i in range(3)]
        macc = sb("m_acc", [128, 512], F32)
        mo = [sb("m_o%d" % i, [128, 512], BF16) for i in range(2)]
        for b in 'abc':
            for k in range(KC):
                P.dma('sp', xs[b][:, k, :], G.xT[b][k * 128:(k + 1) * 128, :], w=[('x', b)] if k == 0 else [('x', b, k)])
        xkeys = {b: [('x', b)] + [('x', b, k) for k in range(1, KC)] for b in 'abc'}
        wouts = {'a': G.W['ret_w_out'][l], 'b': G.W['hgrn_w_out'][l], 'c': G.W['swa_w_out'][l]}
        goff = {'a': OFF['ga'], 'b': OFF['gb'], 'c': OFF['gc']}
        n = 0
        for cb in range(2):
            wy = {b: load_w(G, wouts[b], cb * 512, 512, tag=('my', l, b, cb)) for b in 'abc'}
            wg = {b: load_w(G, w_in, goff[b] + cb * 512, 512, tag=('mg', l, b, cb)) for b in 'abc'}
            for cc in range(4):
                for tg in range(4):
                    oi = n % 2
                    n += 1
                    for bi, b in enumerate('abc'):
                        psy, pky = fm_group(G, wy[b][0], wy[b][1], cc, tg, rkeys=xkeys[b], src=xs[b])
                        psg, pkg = fm_group(G, wg[b][0], wg[b][1], cc, tg)
                        P.op('act', lambda e, psg=psg, bi=bi: e.activation(sg[bi][:, :], psg[:, :], AF.Sigmoid), r=[pkg], w=[('sg', bi)])
                        if bi == 0:
                            P.op('dve', lambda e, psy=psy, bi=bi: e.tensor_tensor(macc[:, :], sg[bi][:, :], psy[:, :], ALU.mult), r=[('sg', bi), pky], w=['macc'])
                        else:
                            P.op('dve', lambda e, psy=psy, bi=bi: e.tensor_tensor(sg[bi][:, :], sg[bi][:, :], psy[:, :], ALU.mult), r=[('sg', bi), pky], w=[('sg', bi)])
                            if bi == 1:
                                P.op('dve', lambda e, bi=bi: e.tensor_tensor(macc[:, :], macc[:, :], sg[bi][:, :], ALU.add), r=['macc', ('sg', bi)], w=['macc'])
                            else:
                                P.op('dve', lambda e, bi=bi, oi=oi: e.tensor_tensor(mo[oi][:, :], macc[:, :], sg[bi][:, :], ALU.add), r=['macc', ('sg', bi)], w=[('mo', oi)])
                    c8 = cb * 4 + cc
                    P.dma('sp', G.mgT[c8 * 128:(c8 + 1) * 128, tg * 512:(tg + 1) * 512], mo[oi][:, :], r=[('mo', oi)], w=[('mgT', c8, tg)])
        G.ws = ws_save
        G.ws_rr = 0


def router_tile(G, R, tile, pss, slot=0):
    P = G.P
    wr, hTfs, lgs, rrs = R
    hTf, lg, rr_ = hTfs[slot], lgs[slot], rrs[slot]
    k = lambda n: (n, slot)
    for hb, (ps, pk) in enumerate(pss):
        P.op('act', lambda e, ps=ps, hb=hb: e.activation(hTf[:, hb * 4:(hb + 1) * 4, :], ps[:, :].rearrange("p (a t) -> p a t", t=128), AF.Copy),
             r=[pk], w=[k(('hTf', hb))])
        yield
    ps, pk = next_ps(G)

    def fr(e, ps=ps):
        ins = None
        for kk in range(KC):
            ins = e.matmul(ps[:, 0:NEXP], hTf[:, kk, :], wr[:, kk, :], start=(kk == 0), stop=(kk == KC - 1))
        return ins
    P.op('pe', fr, r=[k(('hTf', 0)), k(('hTf', 1)), 'wr'], w=[pk])
    yield
    P.op('dve', lambda e, ps=ps: e.tensor_copy(lg[:, 0:8], ps[:, 0:NEXP]), r=[pk], w=[k('lg')])
    yield
    P.op('dve', lambda e: e.tensor_reduce(rr_[:, 0:1], lg[:, 0:8], AX.X, ALU.max), r=[k('lg')], w=[k('rr0')])
    yield
    P.op('dve', lambda e: e.tensor_scalar(lg[:, 8:16], lg[:, 0:8], rr_[:, 0:1], None, ALU.is_equal), r=[k('lg'), k('rr0')], w=[k('lg1')])
    yield
    P.op('dve', lambda e: e.scalar_tensor_tensor(lg[:, 8:16], lg[:, 8:16], -1e30, lg[:, 0:8], ALU.mult, ALU.add), r=[k('lg'), k('lg1')], w=[k('lg1')])
    yield
    P.op('dve', lambda e: e.tensor_reduce(rr_[:, 1:2], lg[:, 8:16], AX.X, ALU.max), r=[k('lg1')], w=[k('rr1')])
    yield
    P.op('dve', lambda e: e.tensor_scalar(lg[:, 8:16], lg[:, 0:8], rr_[:, 1:2], None, ALU.is_ge), r=[k('lg'), k('rr1'), k('lg1')], w=[k('lg1')])
    P.op('dve', lambda e: e.tensor_scalar(rr_[:, 2:3], rr_[:, 0:1], -1.0, None, ALU.mult), r=[k('rr0')], w=[k('rr2')])
    yield
    P.op('act', lambda e: e.activation(lg[:, 16:24], lg[:, 0:8], AF.Exp, bias=rr_[:, 2:3], scale=1.0), r=[k('lg'), k('rr2')], w=[k('lg2')])
    yield
    P.op('dve', lambda e: e.tensor_tensor(lg[:, 16:24], lg[:, 16:24], lg[:, 8:16], ALU.mult), r=[k('lg2'), k('lg1')], w=[k('lg2')])
    yield
    P.op('dve', lambda e: e.tensor_reduce(rr_[:, 3:4], lg[:, 16:24], AX.X, ALU.add), r=[k('lg2')], w=[k('rr3')])
    yield
    P.op('dve', lambda e: e.reciprocal(rr_[:, 3:4], rr_[:, 3:4]), r=[k('rr3')], w=[k('rr3')])
    yield
    P.op('dve', lambda e, tile=tile: e.tensor_scalar(G.gate_all[:, tile, :], lg[:, 16:24], rr_[:, 3:4], None, ALU.mult), r=[k('lg2'), k('rr3')], w=[('gate', tile)])
    yield


def phase_wo(G, l):
    P = G.P
    G.ps_sel = list(range(8))
    with contextlib.ExitStack() as st:
        sb = lambda n, s, d: G.sb(n, s, d, st)
        mg = sb("o_mg", [128, KC, S], BF16)
        G.xt = [sb("o_xt%d" % i, [128, D], F32) for i in range(LN_W + 1)]
        G.gbc = sb("o_gbc", [128, D], F32)
        G.bbc = sb("o_bbc", [128, D], F32)
        R = None
        if l % 2 == 1:
            wr = sb("o_wr", [128, KC, NEXP], F32)
            hTfs = [sb("o_hTf%d" % i, [128, KC, 128], F32) for i in range(LN_W)]
            lgs = [sb("o_lg%d" % i, [128, 24], F32) for i in range(LN_W)]
            rrs = [sb("o_rr%d" % i, [128, 4], F32) for i in range(LN_W)]
            R = (wr, hTfs, lgs, rrs)
            P.dma('sp', wr[:, :, :], G.W['moe_router'][l // 2].rearrange("(k p) e -> p k e", p=128), w=['wr'])
        for k in range(KC):
            P.dma('sp', mg[:, k, :], G.mgT[k * 128:(k + 1) * 128, :], w=[('mg', k)])
        mgk = [('mg', k) for k in range(KC)]
        load_ln_params(G, G.W['ln_mix_g'][l], G.W['ln_mix_b'][l])
        w0 = load_w(G, G.W['w_o'][l], 0, 512, tag=('wo', l))
        w1 = load_w(G, G.W['w_o'][l], 512, 512)

        def unit(t):
            xt, xk = next_xt(G)
            P.dma('sp', xt[:, :], G.h_res[t * 128:(t + 1) * 128, :], r=[('h_res', t)], w=[xk])
            yield
            for hf, (wt, wk) in enumerate((w0, w1)):
                ps, pk = tm_group(G, wt, wk, t, rkeys=mgk, src=mg)
                yield
                P.op('dve', lambda e, ps=ps, xt=xt, hf=hf: e.scalar_tensor_tensor(
                    xt[:, hf * 512:(hf + 1) * 512], xt[:, hf * 512:(hf + 1) * 512], DN_ALPHA, ps[:, :], ALU.mult, ALU.add), r=[pk, xk], w=[xk])
                yield
            pss = []
            yield from ln_tile(G, xt, xk, t, slot=t % LN_W, pss=pss)
            if R is not None:
                yield from router_tile(G, R, t, pss, slot=t % LN_W)
        rr((unit(t) for t in range(NT)), LN_W)


def phase_ffn(G, l):
    P = G.P
    G.ps_sel = list(range(8))
    moe = (l % 2 == 1)
    jx = l // 2
    last = (l == DEPTH - 1)
    with contextlib.ExitStack() as st:
        sb = lambda n, s, d: G.sb(n, s, d, st)
        yacc = sb("f_y", [128, NT, D], F32)
        hid = sb("f_hid", [128, 4, S], BF16)
        wds = [sb("f_wd%d" % i, [128, 4, D], BF16) for i in range(2)]
        sl = [sb("f_sl%d" % i, [128, 512], F32) for i in range(2)]
        G.xt = [sb("f_xt%d" % i, [128, D], F32) for i in range(LN_W + 1)]
        G.xt_rr = 0
        G.gbc = sb("f_gbc", [128, D], F32)
        G.bbc = sb("f_bbc", [128, D], F32)
        if moe:
            experts = [(G.W['moe_w_gate'][jx][e], G.W['moe_w_up'][jx][e], G.W['moe_w_down'][jx][e], e) for e in range(NEXP)]
            F = D_EXP
        else:
            experts = [(G.W['ffn_w_gate'][jx], G.W['ffn_w_up'][jx], G.W['ffn_w_down'][jx], None)]
            F = D_FF
        nblk = (F + 511) // 512
        load_ln_params(G, G.W['ln_ffn_g'][l], G.W['ln_ffn_b'][l])
        ln_active = []

        def unit(t):
            xt, xk = next_xt(G)
            P.dma('sp', xt[:, :], G.h_res[t * 128:(t + 1) * 128, :], r=[('h_res', t)], w=[xk])
            yield
            P.op('dve', lambda e, xt=xt, t=t: e.scalar_tensor_tensor(xt[:, :], xt[:, :], DN_ALPHA, yacc[:, t, :], ALU.mult, ALU.add),
                 r=[xk, ('y', t, 0), ('y', t, 1)], w=[xk])
            yield
            yield from ln_tile(G, xt, xk, t, slot=t % LN_W, out_final=(G.out if last else None))

        first = True
        n = 0
        wdi = 0
        for (wg_d, wu_d, wd_d, e_idx) in experts:
            for blk in range(nblk):
                ncols = min(512, F - blk * 512)
                nj = ncols // 128
                wg, wgk = load_w(G, wg_d, blk * 512, ncols, tag=('ffn', l, e_idx, blk))
                wu, wuk = load_w(G, wu_d, blk * 512, ncols)
                wd = wds[wdi % 2]
                wdk = ('wd', wdi % 2)
                wdi += 1
                P.dma('pool', wd[:, 0:nj, :], wd_d[blk * 512:blk * 512 + ncols, :].rearrange("(j p) c -> p j c", p=128), w=[wdk])
                for j in range(nj):
                    for tg in range(4):
                        si = n % 2
                        n += 1
                        psg, pkg = fm_group(G, wg, wgk, j, tg)
                        psu, pku = fm_group(G, wu, wuk, j, tg)
                        P.op('act', lambda e, psg=psg, si=si: e.activation(sl[si][:, :], psg[:, :], AF.Silu), r=[pkg], w=[('sl', si)])
                        P.op('dve', lambda e, psu=psu, si=si, j=j, tg=tg: e.tensor_tensor(hid[:, j, tg * 512:(tg + 1) * 512], sl[si][:, :], psu[:, :], ALU.mult),
                             r=[('sl', si), pku], w=[('hid', j, tg)])
                is_last = (e_idx is None or e_idx == NEXP - 1) and blk == nblk - 1
                for t in range(NT):
                    for hf in range(2):
                        ps, pk = next_ps(G)

                        def fd(e, ps=ps, t=t, hf=hf, nj=nj, wd=wd):
                            ins = None
                            for j in range(nj):
                                ins = e.matmul(ps[:, :], hid[:, j, t * 128:(t + 1) * 128], wd[:, j, hf * 512:(hf + 1) * 512], start=(j == 0), stop=(j == nj - 1))
                            return ins
                        P.op('pe', fd, r=[('hid', j, t // 4) for j in range(nj)] + [wdk], w=[pk])
                        ya = yacc[:, t, hf * 512:(hf + 1) * 512]
                        yk = ('y', t, hf)
                        if e_idx is None:
                            if first:
                                P.op('act', lambda e, ps=ps, ya=ya: e.activation(ya, ps[:, :], AF.Copy), r=[pk], w=[yk])
                            else:
                                P.op('dve', lambda e, ps=ps, ya=ya: e.tensor_tensor(ya, ya, ps[:, :], ALU.add), r=[pk, yk], w=[yk])
                        else:
                            gsc = G.gate_all[:, t, e_idx:e_idx + 1]
                            if first:
                                P.op('act', lambda e, ps=ps, ya=ya, gsc=gsc: e.activation(ya, ps[:, :], AF.Copy, scale=gsc), r=[pk, ('gate', t)], w=[yk])
                            else:
                                P.op('dve', lambda e, ps=ps, ya=ya, gsc=gsc: e.scalar_tensor_tensor(ya, ps[:, :], gsc, ya, ALU.mult, ALU.add),
                                     r=[pk, yk, ('gate', t)], w=[yk])
                    if is_last:
                        while len(ln_active) >= LN_W:
                            for g_ in list(ln_active):
                                try:
                                    next(g_)
                                except StopIteration:
                                    ln_active.remove(g_)
                        ln_active.append(unit(t))
                        for _ in range(4):
                            for g_ in list(ln_active):
                                try:
                                    next(g_)
                                except StopIteration:
                                    ln_active.remove(g_)
                first = False
        while ln_active:
            for g_ in list(ln_active):
                try:
                    next(g_)
                except StopIteration:
                    ln_active.remove(g_)


_CACHE = {}


def kernel(**inputs):
    if 'nc' not in _CACHE:
        _CACHE['nc'] = build()
    nc = _CACHE['nc']
    C = _consts()
    x = np.asarray(inputs['x'], np.float32)
    B = x.shape[0]
    base = {name: np.ascontiguousarray(np.asarray(inputs[name], np.float32)) for name, _ in W_SPECS}
    for name in CONST_NAMES:
        base["c_" + name] = np.ascontiguousarray(C[name])
    in_maps = []
    for b in range(B):
        m = dict(base)
        m['x'] = np.ascontiguousarray(x[b])
        in_maps.append(m)
    res = run_bass_kernel_spmd(nc, in_maps, core_ids=list(range(B)))
    return np.stack([np.asarray(r['out'], np.float32) for r in res.results], 0)
```

```python
import contextlib
import math
import numpy as np
import concourse.bass as bass
import concourse.mybir as mybir
from concourse.bass_utils import run_bass_kernel_spmd

F32 = mybir.dt.float32
BF16 = mybir.dt.bfloat16
ALU = mybir.AluOpType
AF = mybir.ActivationFunctionType
AX = mybir.AxisListType

S = 2048
D = 1024
NT = 16
KC = 8
DEPTH = 2
N_IN = 11776
D_FF = 2816
D_EXP = 3584
NEXP = 8
LN_EPS = 1e-5
RMS_EPS = 1e-6
DN_ALPHA = (2.0 * DEPTH) ** 0.25
OFF = dict(rq=0, rk=512, rv=1024, rg=2048, hq=3072, hf=4096, hi=5120, hg=6144,
           sq=7168, sk=8192, sv=8448, ga=8704, gb=9728, gc=10752)
HL = 32

COMPUTE = ('pe', 'act', 'dve', 'pool')
ALL_ENG = ('pe', 'act', 'dve', 'pool', 'sp')
NDMASEM = 6


class Op:
    __slots__ = ('eng', 'fn', 'deps', 'dma', 'sem', 'ticket', 'idx', 'prev_ticket', 'semkey')


class Prog:
    def __init__(self, nc, sems, dma_sems, same_engine_sync=True):
        self.nc = nc
        self.sems = sems
        self.dma_sems = dma_sems
        self.count = {e: 0 for e in COMPUTE}
        self.dma_count = {(q, i): 0 for q in dma_sems for i in range(len(dma_sems[q]))}
        self.dma_rr = {q: 0 for q in dma_sems}
        self.waited = {e: {} for e in ALL_ENG}
        self.same_engine_sync = same_engine_sync
        self.ops = []
        self.lw = {}
        self.rd = {}
        self.prev_final = []

    def op(self, eng, fn, r=(), w=(), dma=False):
        deps = set()
        for b in r:
            if b in self.lw:
                deps.add(self.lw[b])
        for b in w:
            if b in self.lw:
                deps.add(self.lw[b])
            for x in self.rd.get(b, ()):
                deps.add(x)
        o = Op()
        o.eng = eng; o.fn = fn; o.dma = dma; o.sem = None; o.ticket = None
        o.idx = len(self.ops)
        o.deps = deps
        self.ops.append(o)
        for b in r:
            self.rd.setdefault(b, []).append(o.idx)
        for b in w:
            self.lw[b] = o.idx
            self.rd[b] = []
        return o.idx

    def dma(self, q, out, in_, r=(), w=(), **kw):
        def fn(e):
            return e.dma_start(out=out, in_=in_, **kw)
        return self.op(q, fn, r, w, dma=True)

    def emit(self, final_wait=False):
        nc = self.nc
        ops = self.ops
        needed = set()
        for o in ops:
            for d in o.deps:
                od = ops[d]
                if od.eng == o.eng and not od.dma:
                    if od.eng == 'pe' or not self.same_engine_sync:
                        continue
                needed.add(d)
        last = {}
        for o in ops:
            if not o.dma:
                last[o.eng] = o.idx
        for e, i in last.items():
            needed.add(i)
        for o in ops:
            if o.dma:
                q = o.eng
                i = self.dma_rr[q]
                self.dma_rr[q] = (i + 1) % len(self.dma_sems[q])
                o.prev_ticket = self.dma_count[(q, i)]
                self.dma_count[(q, i)] += 16
                o.sem = self.dma_sems[q][i]
                o.semkey = ('d', q, i)
                o.ticket = self.dma_count[(q, i)]
            elif o.idx in needed:
                self.count[o.eng] += 1
                o.sem = self.sems[o.eng]
                o.semkey = ('c', o.eng)
                o.ticket = self.count[o.eng]
        by_eng = {e: [o for o in ops if o.eng == e] for e in ALL_ENG}
        prev_final = self.prev_final
        waited = self.waited
        same_sync = self.same_engine_sync

        def run(engname, eh):
            wd = waited[engname]

            def wait(semkey, sem, ticket):
                if wd.get(semkey, 0) >= ticket:
                    return
                eh.wait_ge(sem, ticket)
                wd[semkey] = ticket
            for (semkey, sem, ticket) in prev_final:
                if semkey == ('c', engname) and (engname == 'pe' or not same_sync):
                    continue
                wait(semkey, sem, ticket)
            for o in by_eng[engname]:
                for d in sorted(o.deps):
                    od = ops[d]
                    if od.ticket is None:
                        continue
                    if od.eng == o.eng and not od.dma and (engname == 'pe' or not same_sync):
                        continue
                    wait(od.semkey, od.sem, od.ticket)
                if o.dma and o.prev_ticket > 0:
                    wait(o.semkey, o.sem, o.prev_ticket)
                inst = o.fn(eh)
                if o.ticket is not None:
                    inst.then_inc(o.sem, 16 if o.dma else 1)
            if engname in self.dma_sems:
                for i, sem in enumerate(self.dma_sems[engname]):
                    t = self.dma_count[(engname, i)]
                    if t > 0:
                        wait(('d', engname, i), sem, t)
            if final_wait and engname == 'sp':
                for q in self.dma_sems:
                    for i, sem in enumerate(self.dma_sems[q]):
                        t = self.dma_count[(q, i)]
                        if t > 0:
                            wait(('d', q, i), sem, t)

        with nc.Block() as block:
            @block.tensor
            def _(e):
                run('pe', e)

            @block.scalar
            def _(e):
                run('act', e)

            @block.vector
            def _(e):
                run('dve', e)

            @block.gpsimd
            def _(e):
                run('pool', e)

            @block.sync
            def _(e):
                run('sp', e)
        fin = []
        for e in COMPUTE:
            if self.count[e] > 0:
                fin.append((('c', e), self.sems[e], self.count[e]))
        for (q, i), t in self.dma_count.items():
            if t > 0:
                fin.append((('d', q, i), self.dma_sems[q][i], t))
        self.prev_final = fin
        self.ops = []
        self.lw = {}
        self.rd = {}


class Ctx:
    pass


G_SKIP = set()
import os
HG_LEVEL = int(os.environ.get('HG_LEVEL', '4'))
SW_LEVEL = int(os.environ.get('SW_LEVEL', '3'))
SW_SUB = int(os.environ.get("SW_SUB", "3"))
SW_W = int(os.environ.get("SW_W", "4"))
LN_W = int(os.environ.get("LN_W", "4"))
HG_MERGE = bool(int(os.environ.get("HG_MERGE", "0")))
SAME_SYNC = bool(int(os.environ.get('SAME_SYNC', '1')))


def _consts():
    c = {}
    half = 64
    inv = (1.0 / (10000.0 ** np.linspace(0.0, 1.0, half, dtype=np.float32))).astype(np.float32)
    pos = np.arange(S, dtype=np.float32)
    ang = pos[:, None] * inv[None, :]
    cos = np.cos(ang).astype(np.float32).T
    sin = np.sin(ang).astype(np.float32).T
    c['cosT'] = np.ascontiguousarray(np.concatenate([cos, cos], 0))
    c['sinT'] = np.ascontiguousarray(np.concatenate([sin, sin], 0))
    pm = np.zeros((128, 128), np.float32)
    for d in range(64):
        pm[d + 64, d] = -1.0
        pm[d, d + 64] = 1.0
    c['pm'] = pm
    c['ident'] = np.eye(128, dtype=np.float32)
    lg = np.log(1.0 - 2.0 ** (-5.0 - np.arange(4, dtype=np.float64)))
    idx = np.arange(128, dtype=np.float64)
    dt = np.zeros((128, 4, 128), np.float32)
    qd = np.zeros((128, 4, 128), np.float32)
    kd = np.zeros((128, 4), np.float32)
    for h in range(4):
        diff = idx[None, :] - idx[:, None]
        dt[:, h, :] = np.where(diff >= 0, np.exp(np.maximum(diff, 0) * lg[h]), 0.0) * 128 ** -0.5
        qd[:, h, :] = np.exp((idx + 1.0) * lg[h])[None, :]
        kd[:, h] = np.exp((127.0 - idx) * lg[h]) * 128 ** -0.5
    c['ret_dt'] = dt
    c['ret_qd'] = qd
    c['ret_kd'] = kd
    c['ret_cd'] = [float(np.exp(128.0 * lg[h])) for h in range(4)]
    s_ = np.arange(128)
    bm = ((s_[:, None] // HL) == (s_[None, :] // HL)) & (s_[:, None] <= s_[None, :])
    c['hg_bm'] = bm.astype(np.float32)
    sm = np.ones((128, 512), np.float32)
    sm[:, ::HL] = 0.0
    c['hg_scanm'] = sm
    c['hg_blk'] = (s_[:, None] // HL == np.arange(128 // HL)[None, :]).astype(np.float32)
    NEG = -30000.0
    m1 = np.zeros((128, 256), np.float32)
    m1[:64, 192:256] = NEG
    m1[64:, 0:64] = NEG
    m0 = m1.copy()
    m0[:, 0:128] = NEG
    c['swa_mask'] = np.stack([m0, m1], 1)
    return c


CONST_NAMES = ['cosT', 'sinT', 'pm', 'ident', 'ret_dt', 'ret_qd', 'ret_kd', 'hg_bm', 'hg_scanm', 'hg_blk', 'swa_mask']
W_SPECS = [
    ('ln_in_g', [D]), ('ln_in_b', [D]), ('w_in', [DEPTH, D, N_IN]), ('ret_w_out', [DEPTH, D, D]),
    ('hgrn_lower_bounds', [DEPTH, D]), ('hgrn_norm_g', [DEPTH, 128]), ('hgrn_w_out', [DEPTH, D, D]),
    ('swa_sinks', [DEPTH, 16]), ('swa_w_out', [DEPTH, D, D]), ('w_o', [DEPTH, D, D]),
    ('ln_mix_g', [DEPTH, D]), ('ln_mix_b', [DEPTH, D]),
    ('ffn_w_gate', [1, D, D_FF]), ('ffn_w_up', [1, D, D_FF]), ('ffn_w_down', [1, D_FF, D]),
    ('moe_router', [1, D, NEXP]), ('moe_w_gate', [1, NEXP, D, D_EXP]), ('moe_w_up', [1, NEXP, D, D_EXP]),
    ('moe_w_down', [1, NEXP, D_EXP, D]), ('ln_ffn_g', [DEPTH, D]), ('ln_ffn_b', [DEPTH, D]),
]


def build(stop_after=None, debug=False):
    C = _consts()
    nc = bass.Bass("TRN2", target_bir_lowering=False)
    G = Ctx()
    G.nc = nc
    G.x = nc.dram_tensor("x", [S, D], F32, kind="ExternalInput").ap()
    G.W = {}
    for name, shp in W_SPECS:
        G.W[name] = nc.dram_tensor(name, shp, F32, kind="ExternalInput").ap()
    G.K = {}
    for name in CONST_NAMES:
        G.K[name] = nc.dram_tensor("c_" + name, list(C[name].shape), F32, kind="ExternalInput").ap()
    G.out = nc.dram_tensor("out", [S, D], F32, kind="ExternalOutput").ap()
    skind = "ExternalOutput" if debug else "Internal"
    G.h_res = nc.dram_tensor("h_res", [S, D], F32, kind=skind).ap()
    G.xT = {b: nc.dram_tensor("x%sT" % b, [D, S], BF16, kind=skind).ap() for b in 'abc'}
    G.mgT = nc.dram_tensor("mgT", [D, S], BF16, kind=skind).ap()
    G.cd = C['ret_cd']

    with contextlib.ExitStack() as es:
        uid = [0]

        def sb(name, shape, dt, st=es):
            uid[0] += 1
            return st.enter_context(nc.sbuf_tensor("%s_u%d" % (name, uid[0]), shape, dt))
        sems = {e: es.enter_context(nc.semaphore("s_" + e)) for e in COMPUTE}
        dsems = {q: [es.enter_context(nc.semaphore("d_%s%d" % (q, i))) for i in range(NDMASEM)]
                 for q in ('sp', 'pool')}
        P = Prog(nc, sems, dsems, same_engine_sync=SAME_SYNC)
        G.P = P
        G.sb = sb
        G.hT = sb("hT", [128, KC, S], BF16)
        G.ident_f = sb("ident_f", [128, 128], F32)
        G.ident_b = sb("ident_b", [128, 128], BF16)
        G.ws = [sb("ws%d" % i, [128, KC, 512], BF16) for i in range(4)]
        G.ws_rr = 0
        G.xt_rr = 0
        G.small = sb("small", [128, 64], F32)
        G.ps = [es.enter_context(nc.psum_tensor("ps%d" % i, [128, 512], F32)) for i in range(8)]
        G.ps_rr = 0
        G.gate_all = sb("gate_all", [128, NT, NEXP], F32)

        phases = []
        phases.append(('ln_in', lambda: phase_ln_in(G)))
        for l in range(DEPTH):
            phases.append(('ret%d' % l, lambda l=l: phase_ret(G, l)))
            phases.append(('hgrn%d' % l, lambda l=l: phase_hgrn(G, l)))
            phases.append(('swa%d' % l, lambda l=l: phase_swa(G, l)))
            phases.append(('merge%d' % l, lambda l=l: phase_merge(G, l)))
            phases.append(('wo%d' % l, lambda l=l: phase_wo(G, l)))
            phases.append(('ffn%d' % l, lambda l=l: phase_ffn(G, l)))
        for i, (name, fn) in enumerate(phases):
            if G_SKIP and name.rstrip('0123456789') in G_SKIP and name != stop_after:
                continue
            fn()
            lastp = (i == len(phases) - 1) or (name == stop_after)
            if not lastp and not G_SKIP:
                nxt = phases[i + 1][0]
                ln_ = int(nxt[-1]) if nxt[-1].isdigit() else 0
                kind = nxt.rstrip('0123456789')
                w_in_n = G.W['w_in'][ln_]
                if kind == 'ret':
                    prefetch_w(G, ('rv', ln_, 0), w_in_n, OFF['rv'], 512)
                elif kind == 'hgrn':
                    prefetch_w(G, ('hi', ln_, 0), w_in_n, OFF['hi'], 512)
                elif kind == 'swa':
                    prefetch_w(G, ('sq', ln_, 0), w_in_n, OFF['sq'], 512)
                elif kind == 'merge':
                    G.ws_rr = 0
                    prefetch_w(G, ('my', ln_, 'a', 0), G.W['ret_w_out'][ln_], 0, 512)
                    prefetch_w(G, ('my', ln_, 'b', 0), G.W['hgrn_w_out'][ln_], 0, 512)
                    prefetch_w(G, ('my', ln_, 'c', 0), G.W['swa_w_out'][ln_], 0, 512)
                    prefetch_w(G, ('mg', ln_, 'a', 0), w_in_n, OFF['ga'], 512)
                elif kind == 'wo':
                    prefetch_w(G, ('wo', ln_), G.W['w_o'][ln_], 0, 512)
                elif kind == 'ffn':
                    if ln_ % 2 == 1:
                        prefetch_w(G, ('ffn', ln_, 0, 0), G.W['moe_w_gate'][ln_ // 2][0], 0, 512)
                    else:
                        prefetch_w(G, ('ffn', ln_, None, 0), G.W['ffn_w_gate'][ln_ // 2], 0, 512)
            P.emit(final_wait=lastp)
            if lastp:
                break
    return nc


def next_ps(G):
    i = G.ps_rr % len(G.ps_sel)
    G.ps_rr = (i + 1) % len(G.ps_sel)
    j = G.ps_sel[i]
    return G.ps[j], ('ps', j)


def next_ws(G):
    i = G.ws_rr
    G.ws_rr = (i + 1) % len(G.ws)
    return G.ws[i], ('ws', i)


def load_w(G, wdram, c0, ncols, q='pool', tag=None):
    pf = getattr(G, 'prefetched', None)
    if tag is not None and pf and tag in pf:
        wt, key = pf.pop(tag)
        return wt, key
    wt, key = next_ws(G)
    G.P.dma(q, wt[:, :, 0:ncols], wdram[:, c0:c0 + ncols].rearrange("(k p) c -> p k c", p=128), w=[key])
    return wt, key


def prefetch_w(G, tag, wdram, c0, ncols):
    if len(G.ws) != 4:
        return
    wt, key = load_w(G, wdram, c0, ncols)
    if getattr(G, 'prefetched', None) is None:
        G.prefetched = {}
    G.prefetched[tag] = (wt, key)


def fm_group(G, wt, wkey, cc, tg, rkeys=('hT',), src=None, ncols_tok=512, t0=None):
    src = G.hT if src is None else src
    ps, pk = next_ps(G)
    t0 = tg * 512 if t0 is None else t0

    def fn(e, ps=ps, wt=wt, cc=cc, t0=t0, src=src):
        ins = None
        for k in range(KC):
            ins = e.matmul(ps[:, 0:ncols_tok], wt[:, k, cc * 128:(cc + 1) * 128], src[:, k, t0:t0 + ncols_tok],
                           start=(k == 0), stop=(k == KC - 1))
        return ins
    G.P.op('pe', fn, r=[wkey] + list(rkeys), w=[pk])
    return ps, pk


def tm_group(G, wt, wkey, tile, ncols=512, rkeys=('hT',), src=None):
    src = G.hT if src is None else src
    ps, pk = next_ps(G)

    def fn(e, ps=ps, wt=wt, tile=tile, src=src):
        ins = None
        for k in range(KC):
            ins = e.matmul(ps[:, 0:ncols], src[:, k, tile * 128:(tile + 1) * 128], wt[:, k, 0:ncols],
                           start=(k == 0), stop=(k == KC - 1))
        return ins
    G.P.op('pe', fn, r=[wkey] + list(rkeys), w=[pk])
    return ps, pk


def load_consts_common(G):
    P = G.P
    P.dma('sp', G.ident_f[:, :], G.K['ident'], w=['ident_f'])
    P.dma('pool', G.ident_b[:, :], G.K['ident'], w=['ident_b'])


def rr(gens, width):
    active = []
    it = iter(gens)
    done = False
    while True:
        while len(active) < width and not done:
            try:
                active.append(next(it))
            except StopIteration:
                done = True
        if not active:
            break
        for g in list(active):
            try:
                next(g)
            except StopIteration:
                active.remove(g)


def ln_tile(G, xt, xkey, tile, slot=0, out_final=None, pss=None):
    P = G.P
    sm = G.small
    o = slot * 16
    st = sm[:, o:o + 12]
    mv = sm[:, o + 12:o + 14]
    rs = sm[:, o + 14:o + 15]
    k = lambda n: (n, slot)
    P.op('dve', lambda e: e.bn_stats(st[:, 0:6], xt[:, 0:512]), r=[xkey], w=[k('ln_st0')])
    P.op('dve', lambda e: e.bn_stats(st[:, 6:12], xt[:, 512:1024]), r=[xkey], w=[k('ln_st1')])
    yield
    P.op('dve', lambda e: e.bn_aggr(mv, st), r=[k('ln_st0'), k('ln_st1')], w=[k('ln_mv')])
    yield
    P.op('act', lambda e: e.activation(rs, mv[:, 1:2], AF.Sqrt, bias=LN_EPS, scale=1.0), r=[k('ln_mv')], w=[k('ln_rs')])
    yield
    P.op('dve', lambda e: e.reciprocal(rs, rs), r=[k('ln_rs')], w=[k('ln_rs')])
    yield
    P.op('dve', lambda e: e.scalar_tensor_tensor(xt[:, :], xt[:, :], mv[:, 0:1], G.gbc[:, :], ALU.subtract, ALU.mult),
         r=[xkey, k('ln_mv'), 'gbc'], w=[xkey])
    yield
    P.op('dve', lambda e: e.scalar_tensor_tensor(xt[:, :], xt[:, :], rs, G.bbc[:, :], ALU.mult, ALU.add),
         r=[xkey, k('ln_rs'), 'bbc'], w=[xkey])
    yield
    rows = slice(tile * 128, (tile + 1) * 128)
    if out_final is not None:
        P.dma('sp', out_final[rows, :], xt[:, :], r=[xkey], w=[('out', tile)])
    else:
        P.dma('sp', G.h_res[rows, :], xt[:, :], r=[xkey], w=[('h_res', tile)])
        for hb in range(2):
            ps, pk = next_ps(G)

            def fn(e, ps=ps, hb=hb):
                ins = None
                for i in range(4):
                    kk = hb * 4 + i
                    ins = e.matmul(ps[:, i * 128:(i + 1) * 128], xt[:, kk * 128:(kk + 1) * 128], G.ident_f[:, :],
                                   start=True, stop=True)
                return ins
            P.op('pe', fn, r=[xkey, 'ident_f'], w=[pk])
            yield
            P.op('act', lambda e, ps=ps, hb=hb: e.activation(
                G.hT[:, hb * 4:(hb + 1) * 4, tile * 128:(tile + 1) * 128],
                ps[:, :].rearrange("p (a t) -> p a t", t=128), AF.Copy), r=[pk], w=['hT'])
            if pss is not None:
                pss.append((ps, pk))
            yield


def load_ln_params(G, g_ap, b_ap):
    G.P.dma('sp', G.gbc[:, :], g_ap.partition_broadcast(128), w=['gbc'])
    G.P.dma('sp', G.bbc[:, :], b_ap.partition_broadcast(128), w=['bbc'])


def next_xt(G):
    i = G.xt_rr
    G.xt_rr = (i + 1) % len(G.xt)
    return G.xt[i], ('xt', i)


def phase_ln_in(G):
    P = G.P
    G.ps_sel = list(range(8))
    with contextlib.ExitStack() as st:
        sb = lambda n, s, d: G.sb(n, s, d, st)
        G.xt = [sb("i_xt%d" % i, [128, D], F32) for i in range(LN_W + 1)]
        G.gbc = sb("i_gbc", [128, D], F32)
        G.bbc = sb("i_bbc", [128, D], F32)
        load_consts_common(G)
        load_ln_params(G, G.W['ln_in_g'], G.W['ln_in_b'])
        def unit(t):
            xt, xk = next_xt(G)
            P.dma('sp', xt[:, :], G.x[t * 128:(t + 1) * 128, :], w=[xk])
            yield
            yield from ln_tile(G, xt, xk, t, slot=t % LN_W)
        rr((unit(t) for t in range(NT)), LN_W)


def phase_ret(G, l):
    P = G.P
    nc = G.nc
    w_in = G.W['w_in'][l]
    G.ps_sel = [0, 1]
    with contextlib.ExitStack() as st:
        sb = lambda n, s, d: G.sb(n, s, d, st)
        v_tok = sb("r_v", [128, NT, 512], BF16)
        g_tok = sb("r_g", [128, NT, 512], BF16)
        qT = sb("r_q", [128, 2, S], BF16)
        kT = sb("r_k", [128, 2, S], BF16)
        cs = [sb("r_cs%d" % i, [128, 2, 512], F32) for i in range(2)]
        xf = [sb("r_xf%d" % i, [128, 512], F32) for i in range(2)]
        t1 = [sb("r_t1%d" % i, [128, 512], F32) for i in range(2)]
        pm = sb("r_pm", [128, 128], F32)
        dtm = sb("r_dt", [128, 4, 128], F32)
        qdm = sb("r_qd", [128, 4, 128], F32)
        kdm = sb("r_kd", [128, 4], F32)
        scm = [sb("r_scm%d" % i, [128, 128], BF16) for i in range(2)]
        qd = [sb("r_qdd%d" % i, [128, 128], BF16) for i in range(2)]
        kd = [sb("r_kdd%d" % i, [128, 128], BF16) for i in range(2)]
        stf = [sb("r_stf%d" % i, [128, 256], F32) for i in range(2)]
        stb = [sb("r_stb%d" % i, [128, 256], BF16) for i in range(2)]
        xg = [sb("r_xg%d" % i, [128, 512], BF16) for i in range(2)]
        xo = [sb("r_xo%d" % i, [128, 4, 128], BF16) for i in range(2)]
        ss2 = [sb("r_ss%d" % i, [128, 4], F32) for i in range(2)]
        junk = sb("r_junk", [128, 256], F32)
        ps_o = [G.ps[2], G.ps[3]]
        ps_s = [G.ps[4], G.ps[5]]
        ps_m = [G.ps[6], G.ps[7]]
        P.dma('sp', pm[:, :], G.K['pm'], w=['pm'])
        P.dma('sp', dtm[:, :, :], G.K['ret_dt'], w=['dtm'])
        P.dma('sp', qdm[:, :, :], G.K['ret_qd'], w=['qdm'])
        P.dma('sp', kdm[:, :], G.K['ret_kd'], w=['kdm'])
        for hp in range(2):
            G.ps_sel = list(range(8))
            wt, wk = load_w(G, w_in, OFF['rv'] + hp * 512, 512, tag=('rv', l, hp))
            for t in range(NT):
                ps, pk = tm_group(G, wt, wk, t)
                P.op('act', lambda e, ps=ps, t=t: e.activation(v_tok[:, t, :], ps[:, :], AF.Copy), r=[pk], w=[('v', t)])
            wt, wk = load_w(G, w_in, OFF['rg'] + hp * 512, 512)
            for t in range(NT):
                ps, pk = tm_group(G, wt, wk, t)
                P.op('act', lambda e, ps=ps, t=t: e.activation(g_tok[:, t, :], ps[:, :], AF.Silu), r=[pk], w=[('g', t)])
            for (nm, dst) in (('rq', qT), ('rk', kT)):
                wt, wk = load_w(G, w_in, OFF[nm] + hp * 256, 256)
                for tg in range(4):
                    ci = tg % 2
                    P.dma('sp', cs[ci][:, 0, :], G.K['cosT'][:, tg * 512:(tg + 1) * 512], w=[('cs', ci, 0)])
                    P.dma('sp', cs[ci][:, 1, :], G.K['sinT'][:, tg * 512:(tg + 1) * 512], w=[('cs', ci, 1)])
                    for j in range(2):
                        bi = (tg * 2 + j) % 2
                        ps, pk = fm_group(G, wt, wk, j, tg)
                        P.op('act', lambda e, ps=ps, bi=bi: e.activation(xf[bi][:, :], ps[:, :], AF.Copy), r=[pk], w=[('xf', bi)])
                        ps2, pk2 = next_ps(G)
                        P.op('pe', lambda e, ps2=ps2, bi=bi: e.matmul(ps2[:, :], pm[:, :], xf[bi][:, :], start=True, stop=True),
                             r=[('xf', bi), 'pm'], w=[pk2])
                        P.op('dve', lambda e, bi=bi, ci=ci: e.tensor_tensor(t1[bi][:, :], xf[bi][:, :], cs[ci][:, 0, :], ALU.mult),
                             r=[('xf', bi), ('cs', ci, 0)], w=[('t1', bi)])
                        P.op('dve', lambda e, ps2=ps2, bi=bi, ci=ci: e.tensor_tensor(xf[bi][:, :], ps2[:, :], cs[ci][:, 1, :], ALU.mult),
                             r=[pk2, ('cs', ci, 1)], w=[('xf', bi)])
                        P.op('dve', lambda e, bi=bi, dst=dst, j=j, tg=tg: e.tensor_tensor(
                            dst[:, j, tg * 512:(tg + 1) * 512], t1[bi][:, :], xf[bi][:, :], ALU.add),
                            r=[('t1', bi), ('xf', bi)], w=[(nm, j, tg)])
            G.ps_sel = [0, 1]
            pending = None
            for n in range(NT):
                pso = ps_o[n % 2]
                pok = ('ps', 2 + n % 2)
                tgk = n // 4
                cols = slice(n * 128, (n + 1) * 128)
                for j in range(2):
                    h = hp * 2 + j
                    pss_ = ps_s[j]
                    psk = ('ps', 4 + j)
                    P.op('pe', lambda e, pss_=pss_, j=j, cols=cols: e.matmul(pss_[:, 0:128], kT[:, j, cols], qT[:, j, cols], start=True, stop=True),
                         r=[('rk', j, tgk), ('rq', j, tgk)], w=[psk])
                    P.op('dve', lambda e, pss_=pss_, j=j, h=h: e.tensor_tensor(scm[j][:, :], pss_[:, 0:128], dtm[:, h, :], ALU.mult),
                         r=[psk, 'dtm'], w=[('scm', j)])
                    if n > 0:
                        P.op('dve', lambda e, j=j, h=h, cols=cols: e.tensor_tensor(qd[j][:, :], qT[:, j, cols], qdm[:, h, :], ALU.mult),
                             r=[('rq', j, tgk), 'qdm'], w=[('qd', j)])
                if n < NT - 1:
                    for j in range(2):
                        h = hp * 2 + j
                        psm = ps_m[j]
                        pmk = ('ps', 6 + j)
                        P.op('pe', lambda e, psm=psm, j=j, cols=cols: e.matmul(psm[:, 0:128], kT[:, j, cols], G.ident_b[:, :], start=True, stop=True),
                             r=[('rk', j, tgk), 'ident_b'], w=[pmk])
                        P.op('act', lambda e, psm=psm, j=j, h=h: e.activation(kd[j][:, :], psm[:, 0:128], AF.Copy, scale=kdm[:, h:h + 1]),
                             r=[pmk, 'kdm'], w=[('kd', j)])
                for j in range(2):
                    def fo(e, pso=pso, j=j, n=n):
                        ins = e.matmul(pso[:, j * 256:(j + 1) * 256], scm[j][:, :], v_tok[:, n, j * 256:(j + 1) * 256],
                                       start=True, stop=(n == 0))
                        if n > 0:
                            ins = e.matmul(pso[:, j * 256:(j + 1) * 256], qd[j][:, :], stb[j][:, :], start=False, stop=True)
                        return ins
                    P.op('pe', fo, r=[('scm', j), ('v', n), ('qd', j), ('stb', j)], w=[pok])
                if n < NT - 1:
                    for j in range(2):
                        h = hp * 2 + j
                        psm = ps_m[j]
                        pmk = ('ps', 6 + j)
                        P.op('pe', lambda e, psm=psm, j=j, n=n: e.matmul(psm[:, 128:384], kd[j][:, :], v_tok[:, n, j * 256:(j + 1) * 256], start=True, stop=True),
                             r=[('kd', j), ('v', n)], w=[pmk])
                        if n == 0:
                            P.op('dve', lambda e, psm=psm, j=j: e.tensor_copy(stf[j][:, :], psm[:, 128:384]), r=[pmk], w=[('stf', j)])
                        else:
                            P.op('dve', lambda e, psm=psm, j=j, h=h: e.scalar_tensor_tensor(
                                stf[j][:, :], stf[j][:, :], G.cd[h], psm[:, 128:384], ALU.mult, ALU.add),
                                r=[pmk, ('stf', j)], w=[('stf', j)])
                        P.op('act', lambda e, j=j: e.activation(stb[j][:, :], stf[j][:, :], AF.Copy), r=[('stf', j)], w=[('stb', j)])
                xi = n % 2
                ss = ss2[xi]
                for j in range(2):
                    P.op('act', lambda e, pso=pso, j=j, ss=ss: e.activation(junk[:, :], pso[:, j * 256:(j + 1) * 256], AF.Square, accum_out=ss[:, j:j + 1]),
                         r=[pok], w=['junk', ('ss', xi, j)])
                P.op('act', lambda e, ss=ss: e.activation(ss[:, 2:4], ss[:, 0:2], AF.Sqrt, bias=RMS_EPS, scale=1.0 / 256.0),
                     r=[('ss', xi, 0), ('ss', xi, 1)], w=[('rstd', xi)])
                P.op('dve', lambda e, ss=ss: e.reciprocal(ss[:, 2:4], ss[:, 2:4]), r=[('rstd', xi)], w=[('rstd', xi)])
                for j in range(2):
                    P.op('dve', lambda e, pso=pso, j=j, n=n, xi=xi, ss=ss: e.scalar_tensor_tensor(
                        xg[xi][:, j * 256:(j + 1) * 256], pso[:, j * 256:(j + 1) * 256], ss[:, 2 + j:3 + j],
                        g_tok[:, n, j * 256:(j + 1) * 256], ALU.mult, ALU.mult),
                        r=[pok, ('rstd', xi), ('g', n)], w=[('xg', xi, j)])
                if pending is not None:
                    pending()

                def back_b(n=n, xi=xi, cols=cols, hp=hp):
                    ps, pk = next_ps(G)

                    def ft(e, ps=ps, xi=xi):
                        ins = None
                        for i in range(4):
                            ins = e.matmul(ps[:, i * 128:(i + 1) * 128], xg[xi][:, i * 128:(i + 1) * 128], G.ident_b[:, :], start=True, stop=True)
                        return ins
                    P.op('pe', ft, r=[('xg', xi, 0), ('xg', xi, 1), 'ident_b'], w=[pk])
                    P.op('act', lambda e, ps=ps, xi=xi: e.activation(xo[xi][:, :, :], ps[:, :].rearrange("p (a t) -> p a t", t=128), AF.Copy),
                         r=[pk], w=[('xo', xi)])
                    P.dma('sp', G.xT['a'][hp * 512:(hp + 1) * 512, cols].rearrange("(i p) t -> p i t", p=128), xo[xi][:, :, :],
                          r=[('xo', xi)], w=[('xaT', hp, n)])
                pending = back_b
            if pending is not None:
                pending()
                pending = None


def phase_hgrn(G, l):
    P = G.P
    w_in = G.W['w_in'][l]
    G.ps_sel = [0, 1]
    with contextlib.ExitStack() as st:
        sb = lambda n, s, d: G.sb(n, s, d, st)
        v_tok = sb("h_v", [128, NT, 512], BF16)
        qtT = sb("h_qt", [128, 4, S], BF16)
        ktT = sb("h_kt", [128, 4, S], BF16)
        ksT = sb("h_ks", [128, 4, S], BF16)
        sgT = sb("h_sg", [128, 4, S], BF16)
        bdT = sb("h_bd", [128, 4, 64], F32)
        T = [[sb("h_t%d_%d" % (a, i), [128, 512], F32) for i in range(5)] for a in range(2)]
        lbr = sb("h_lbr", [128, 2, 8], F32)
        lbt = sb("h_lb", [128, 8], F32)
        oml = sb("h_oml", [128, 8], F32)
        gn = sb("h_gn", [128, 1], F32)
        bm = sb("h_bm", [128, 128], F32)
        scanm = sb("h_scanm", [128, 512], F32)
        ones_f = sb("h_ones", [128, 128], BF16)
        am = [[sb("h_am%d_%d" % (a, j), [128, 128], BF16) for j in range(4)] for a in range(2)]
        kst = [[sb("h_kst%d_%d" % (a, j), [128, 128], BF16) for j in range(4)] for a in range(2)]
        stf = [sb("h_stf%d" % j, [128, 128], F32) for j in range(4)]
        stb = [[sb("h_stb%d_%d" % (j, a), [128, 128], BF16) for a in range(2)] for j in range(4)]
        osb = [sb("h_osb%d" % a, [128, 512], F32) for a in range(2)]
        sq = [sb("h_sq%d" % a, [128, 512], BF16) for a in range(2)]
        rt = [sb("h_rt%d" % a, [128, 512], F32) for a in range(2)]
        xo = [sb("h_xo%d" % i, [128, 4, 128], BF16) for i in range(2)]
        blkm = sb("h_blkm", [128, 128 // HL], F32)
        vblk = [[sb("h_vblk%d_%d" % (a, j), [128, 128 // HL, 128], BF16) for j in range(4)] for a in range(2)]
        psU = [G.ps[2], G.ps[3], G.ps[6], G.ps[7]]
        P.dma('sp', blkm[:, :], G.K['hg_blk'], w=['blkm'])
        P.dma('sp', bm[:, :], G.K['hg_bm'], w=['bm'])
        P.dma('sp', scanm[:, :], G.K['hg_scanm'], w=['scanm'])
        P.dma('sp', gn[:, :], G.W['hgrn_norm_g'][l].rearrange("(p o) -> p o", o=1), w=['gn'])
        P.op('pool', lambda e: e.memset(ones_f[:, :], 1.0), w=['ones_f'])
        if l == 0:
            P.op('pool', lambda e: e.memset(lbt[:, :], 0.0), w=['lbt'])
        else:
            for a in range(2):
                P.dma('sp', lbr[:, a, :], G.W['hgrn_lower_bounds'][a].rearrange("(h d) -> d h", d=128), w=[('lbr', a)],
                      allow_slow_non_contiguous=True)
            P.op('dve', lambda e: e.tensor_tensor(lbt[:, :], lbr[:, 1, :], lbr[:, 0, :], ALU.subtract), r=[('lbr', 0), ('lbr', 1)], w=['lbt'])
            P.op('act', lambda e: e.activation(lbt[:, :], lbt[:, :], AF.Sigmoid), r=['lbt'], w=['lbt'])
        P.op('dve', lambda e: e.tensor_scalar(oml[:, :], lbt[:, :], -1.0, 1.0, ALU.mult, ALU.add), r=['lbt'], w=['oml'])
        NB = 128 // HL
        for hg in range(2):
            G.ps_sel = [0, 1, 2, 3, 6, 7]
            wt, wk = load_w(G, w_in, OFF['hi'] + hg * 512, 512, tag=('hi', l, hg))
            for t in range(NT):
                ps, pk = tm_group(G, wt, wk, t)
                P.op('act', lambda e, ps=ps, t=t: e.activation(v_tok[:, t, :], ps[:, :], AF.Copy), r=[pk], w=[('v', t)])
            wq, wqk = load_w(G, w_in, OFF['hq'] + hg * 512, 512)
            wf, wfk = load_w(G, w_in, OFF['hf'] + hg * 512, 512)
            wg, wgk = load_w(G, w_in, OFF['hg'] + hg * 512, 512)

            def h2unit(j, tg, a):
                h = hg * 4 + j
                t1, t2, t3, t4, t5 = T[a]
                tk = lambda i, a=a: ('T', a, i)
                cols = slice(tg * 512, (tg + 1) * 512)
                psz, pkz = fm_group(G, wf, wfk, j, tg)
                yield
                P.op('act', lambda e: e.activation(t2[:, :], psz[:, :], AF.Exp, scale=-1.0), r=[pkz], w=[tk(2)])
                yield
                P.op('act', lambda e: e.activation(t1[:, :], t2[:, :], AF.Ln, bias=1.0, scale=1.0), r=[tk(2)], w=[tk(1)])
                yield
                P.op('act', lambda e: e.activation(t1[:, :], t1[:, :], AF.Exp, scale=-1.0), r=[tk(1)], w=[tk(1)])
                yield
                P.op('dve', lambda e: e.tensor_tensor(t2[:, :], t2[:, :], t1[:, :], ALU.mult), r=[tk(1), tk(2)], w=[tk(2)])
                psq, pkq = fm_group(G, wq, wqk, j, tg)
                yield
                P.op('dve', lambda e: e.tensor_scalar(t1[:, :], t1[:, :], oml[:, h:h + 1], lbt[:, h:h + 1], ALU.mult, ALU.add),
                     r=[tk(1), 'oml', 'lbt'], w=[tk(1)])
                yield
                P.op('act', lambda e: e.activation(t3[:, :], t1[:, :], AF.Ln), r=[tk(1)], w=[tk(3)])
                yield
                P.op('dve', lambda e: e.tensor_tensor_scan(t4[:, :], scanm[:, :], t3[:, :], 0.0, ALU.mult, ALU.add),
                     r=[tk(3), 'scanm'], w=[tk(4)])
                yield
                P.op('act', lambda e: e.activation(t1[:, :], t4[:, :], AF.Exp), r=[tk(4)], w=[tk(1)])
                P.op('act', lambda e: e.activation(t3[:, :], t4[:, :], AF.Exp, scale=-1.0), r=[tk(4)], w=[tk(3)])
                yield
                P.op('dve', lambda e: e.tensor_tensor(qtT[:, j, cols], psq[:, :], t1[:, :], ALU.mult),
                     r=[pkq, tk(1)], w=[('qt', j, tg)])
                yield
                P.op('dve', lambda e: e.scalar_tensor_tensor(t2[:, :], t2[:, :], oml[:, h:h + 1], t3[:, :], ALU.mult, ALU.mult),
                     r=[tk(2), tk(3), 'oml'], w=[tk(2)])
                yield
                P.op('act', lambda e: e.activation(ktT[:, j, cols], t2[:, :], AF.Copy), r=[tk(2)], w=[('kt', j, tg)])
                yield
                P.op('dve', lambda e: e.tensor_tensor(
                    ksT[:, j, cols].rearrange("p (b l) -> p b l", l=HL),
                    t2[:, :].rearrange("p (b l) -> p b l", l=HL),
                    t1[:, :].rearrange("p (b l) -> p b l", l=HL)[:, :, HL - 1:HL].broadcast_to([128, 512 // HL, HL]), ALU.mult),
                    r=[tk(1), tk(2)], w=[('ks', j, tg)])
                yield
                nb = 512 // HL
                P.op('dve', lambda e: e.tensor_copy(
                    bdT[:, j, tg * nb:(tg + 1) * nb],
                    t1[:, :].rearrange("p (b l) -> p b l", l=HL)[:, :, HL - 1:HL].rearrange("p b o -> p (b o)")),
                    r=[tk(1)], w=[('bd', j, tg)])
                yield
            for j in range(4):
                for tg in range(4):
                    psg, pkg = fm_group(G, wg, wgk, j, tg)
                    P.op('act', lambda e, psg=psg, j=j, tg=tg: e.activation(sgT[:, j, tg * 512:(tg + 1) * 512], psg[:, :], AF.Silu), r=[pkg], w=[('sg', j, tg)])
            units = [(j, tg) for j in range(4) for tg in range(4)]
            rr((h2unit(j, tg, n % 2) for n, (j, tg) in enumerate(units)), 2)
            if HG_LEVEL < 2:
                continue
            G.ps_sel = [0, 1]
            psok = [('ps', 4), ('ps', 5)]
            psUk = [('ps', 2), ('ps', 3), ('ps', 6), ('ps', 7)]

            def front(i):
                tg = i // 4
                cols = slice(i * 128, (i + 1) * 128)
                a = i % 2
                for j in range(4):
                    ps, pk = next_ps(G)
                    P.op('pe', lambda e, ps=ps, j=j: e.matmul(ps[:, 0:128], ktT[:, j, cols], qtT[:, j, cols], start=True, stop=True),
                         r=[('kt', j, tg), ('qt', j, tg)], w=[pk])
                    P.op('dve', lambda e, ps=ps, j=j: e.tensor_tensor(am[a][j][:, :], ps[:, 0:128], bm[:, :], ALU.mult), r=[pk, 'bm'], w=[('am', a, j)])
                    ps, pk = next_ps(G)
                    P.op('pe', lambda e, ps=ps, j=j: e.matmul(ps[:, 128:256], ksT[:, j, cols], G.ident_b[:, :], start=True, stop=True),
                         r=[('ks', j, tg), 'ident_b'], w=[pk])
                    P.op('act', lambda e, ps=ps, j=j: e.activation(kst[a][j][:, :], ps[:, 128:256], AF.Copy), r=[pk], w=[('kst', a, j)])
                    P.op('dve', lambda e, j=j: e.tensor_tensor(
                        vblk[a][j][:, :, :], v_tok[:, i, j * 128:(j + 1) * 128].rearrange("p (o v) -> p o v", o=1).broadcast_to([128, NB, 128]),
                        blkm[:, :].rearrange("p (b o) -> p b o", o=1).broadcast_to([128, NB, 128]), ALU.mult),
                        r=[('v', i), 'blkm'], w=[('vblk', a, j)])

            def umat(i):
                a = i % 2
                for j in range(4):
                    P.op('pe', lambda e, j=j: e.matmul(psU[j][:, :], kst[a][j][:, :], vblk[a][j][:, :, :].rearrange("p b v -> p (b v)"), start=True, stop=True),
                         r=[('kst', a, j), ('vblk', a, j)], w=[psUk[j]])

            def mid(i):
                tg = i // 4
                a = i % 2
                pso = G.ps[4 + a]
                pok = psok[a]
                if HG_MERGE:
                    for j in range(4):
                        P.op('pe', lambda e, j=j: e.matmul(pso[:, j * 128:(j + 1) * 128], v_tok[:, i, j * 128:(j + 1) * 128], am[a][j][:, :],
                                                           start=True, stop=False, skip_group_check=True),
                             r=[('v', i), ('am', a, j)], w=[pok])
                for b in range(NB):
                    gb = i * NB + b
                    for j in range(4):
                        def fo(e, j=j, b=b, gb=gb):
                            oc = pso[:, j * 128 + b * HL: j * 128 + (b + 1) * HL]
                            ins = None
                            if not HG_MERGE:
                                ins = e.matmul(oc, v_tok[:, i, j * 128:(j + 1) * 128], am[a][j][:, b * HL:(b + 1) * HL], start=True, stop=(gb == 0))
                            if gb > 0:
                                ins = e.matmul(oc, stb[j][gb % 2][:, :], qtT[:, j, i * 128 + b * HL: i * 128 + (b + 1) * HL], start=False, stop=True,
                                               skip_group_check=HG_MERGE)
                            return ins
                        if HG_MERGE and gb == 0:
                            continue
                        P.op('pe', fo, r=[('v', i), ('am', a, j), ('stb', j, gb % 2), ('qt', j, tg)], w=[pok])
                    if gb < S // HL - 1:
                        for j in range(4):
                            if gb == 0:
                                P.op('dve', lambda e, j=j, b=b: e.tensor_copy(stf[j][:, :], psU[j][:, b * 128:(b + 1) * 128]), r=[psUk[j]], w=[('stf', j)])
                            else:
                                P.op('dve', lambda e, j=j, gb=gb, b=b: e.scalar_tensor_tensor(
                                    stf[j][:, :], stf[j][:, :], bdT[:, j, gb:gb + 1], psU[j][:, b * 128:(b + 1) * 128], ALU.mult, ALU.add),
                                    r=[psUk[j], ('stf', j), ('bd', j, gb // (512 // HL))], w=[('stf', j)])
                            P.op('act', lambda e, j=j, gb=gb: e.activation(stb[j][(gb + 1) % 2][:, :], stf[j][:, :], AF.Copy),
                                 r=[('stf', j)], w=[('stb', j, (gb + 1) % 2)])

            def back(i):
                tg = i // 4
                cols = slice(i * 128, (i + 1) * 128)
                a = i % 2
                pso = G.ps[4 + a]
                pok = psok[a]
                osb_, sq_, rt_ = osb[a], sq[a], rt[a]
                P.op('act', lambda e: e.activation(osb_[:, :], pso[:, :], AF.Copy), r=[pok], w=[('osb', a)])
                P.op('act', lambda e: e.activation(sq_[:, :], pso[:, :], AF.Square), r=[pok], w=[('sq', a)])
                ps, pk = next_ps(G)
                P.op('pe', lambda e, ps=ps: e.matmul(ps[:, :], ones_f[:, :], sq_[:, :], start=True, stop=True), r=[('sq', a), 'ones_f'], w=[pk])
                P.op('act', lambda e, ps=ps: e.activation(rt_[:, :], ps[:, :], AF.Ln, bias=RMS_EPS, scale=1.0 / 128.0), r=[pk], w=[('rt', a)])
                P.op('act', lambda e: e.activation(rt_[:, :], rt_[:, :], AF.Exp, scale=-0.5), r=[('rt', a)], w=[('rt', a)])
                P.op('dve', lambda e: e.scalar_tensor_tensor(osb_[:, :], osb_[:, :], gn[:, 0:1], rt_[:, :], ALU.mult, ALU.mult), r=[('osb', a), ('rt', a), 'gn'], w=[('osb', a)])
                P.op('dve', lambda e: e.tensor_tensor(
                    xo[a][:, :, :], osb_[:, :].rearrange("p (a t) -> p a t", t=128), sgT[:, :, cols], ALU.mult),
                    r=[('osb', a)] + [('sg', j, tg) for j in range(4)], w=[('xo', a)])
                P.dma('sp', G.xT['b'][hg * 512:(hg + 1) * 512, cols].rearrange("(i p) t -> p i t", p=128), xo[a][:, :, :],
                      r=[('xo', a)], w=[('xbT', hg, i)])

            front(0)
            umat(0)
            for sstep in range(NT):
                if sstep + 1 < NT:
                    front(sstep + 1)
                mid(sstep)
                if sstep + 1 < NT:
                    umat(sstep + 1)
                back(sstep)


def phase_swa(G, l):
    P = G.P
    w_in = G.W['w_in'][l]
    G.ps_sel = list(range(8))
    with contextlib.ExitStack() as st:
        sb = lambda n, s, d: G.sb(n, s, d, st)
        qT = sb("s_q", [128, 8, S], BF16)
        kT = sb("s_k", [128, 4, 2, 128 + S], BF16)
        vt = sb("s_v", [128, NT + 1, 4, 2, 128], BF16)
        mask = sb("s_mask", [128, 2, 256], F32)
        sink = sb("s_sink", [128, 16], F32)
        with contextlib.ExitStack() as st1:
            wkd = G.sb("s_wkd", [128, KC, 4, 2, 128], BF16, st1)
            P.dma('sp', mask[:, :, :], G.K['swa_mask'], w=['mask'])
            P.dma('sp', sink[:, :], G.W['swa_sinks'][l].partition_broadcast(128), w=['sink'])
            P.op('pool', lambda e: e.memset(kT[:, :, :, 0:128], 0.0), w=['kpad'])
            P.op('pool', lambda e: e.memset(vt[:, :, :, :, :].rearrange("p a b c d -> p (a b c d)"), 0.0), w=['vt0'])
            P.op('pool', lambda e: e.memset(wkd[:, :, :, :, :].rearrange("p a b c d -> p (a b c d)"), 0.0), w=['wkd0'])
            for g in range(4):
                for var in range(2):
                    P.dma('pool', wkd[:, :, g, var, var * 64:(var + 1) * 64],
                          w_in[:, OFF['sk'] + g * 64: OFF['sk'] + (g + 1) * 64].rearrange("(k p) c -> p k c", p=128),
                          r=['wkd0'], w=[('wkd', g, var)])
            for blk in range(2):
                wt, wk = load_w(G, w_in, OFF['sq'] + blk * 512, 512, tag=('sq', l, blk))
                for cc in range(4):
                    for tg in range(4):
                        ps, pk = fm_group(G, wt, wk, cc, tg)
                        P.op('act', lambda e, ps=ps, c8=blk * 4 + cc, tg=tg: e.activation(qT[:, c8, tg * 512:(tg + 1) * 512], ps[:, :], AF.Copy, scale=0.125),
                             r=[pk], w=[('q', blk * 4 + cc, tg)])
            for g in range(4):
                for var in range(2):
                    for tg in range(4):
                        ps, pk = next_ps(G)

                        def fk(e, ps=ps, g=g, var=var, tg=tg):
                            ins = None
                            for k in range(KC):
                                ins = e.matmul(ps[:, :], wkd[:, k, g, var, :], G.hT[:, k, tg * 512:(tg + 1) * 512], start=(k == 0), stop=(k == KC - 1))
                            return ins
                        P.op('pe', fk, r=[('wkd', g, var), 'hT'], w=[pk])
                        P.op('act', lambda e, ps=ps, g=g, var=var, tg=tg: e.activation(kT[:, g, var, 128 + tg * 512:128 + (tg + 1) * 512], ps[:, :], AF.Copy),
                             r=[pk, 'kpad'], w=[('k', g, var, tg)])
            wt, wk = load_w(G, w_in, OFF['sv'], 256)
            for t in range(NT):
                ps, pk = tm_group(G, wt, wk, t, ncols=256)
                P.op('act', lambda e, ps=ps, t=t: e.activation(vt[:, t + 1, :, 0, 0:64], ps[:, 0:256].rearrange("p (g d) -> p g d", d=64), AF.Copy),
                     r=[pk, 'vt0'], w=[('vt', t + 1, 0)])
                P.op('act', lambda e, ps=ps, t=t: e.activation(vt[:, t + 1, :, 1, 64:128], ps[:, 0:256].rearrange("p (g d) -> p g d", d=64), AF.Copy),
                     r=[pk, 'vt0'], w=[('vt', t + 1, 1)])
            P.emit()
        NSL = SW_W
        sms = [sb("s_sm%d" % i, [128, 4, 256], F32) for i in range(NSL)]
        ps_ = [sb("s_p%d" % i, [128, 4, 256], BF16) for i in range(NSL)]
        mxs = [sb("s_mx%d" % i, [128, 16], F32) for i in range(NSL)]
        pTs = [sb("s_pT%d" % i, [128, 8, 128], BF16) for i in range(NSL)]
        xos = [sb("s_xo%d" % i, [128, 2, 128], BF16) for i in range(NSL)]

        def s2unit(i, g, slot):
            cols = slice(i * 128, (i + 1) * 128)
            mi = 0 if i == 0 else 1
            sm, p, mx, pT, xo = sms[slot], ps_[slot], mxs[slot], pTs[slot], xos[slot]
            bk = [slot * 2, slot * 2 + 1]
            kk = lambda n: (n, slot)
            for a in range(4):
                hq = 4 * g + a
                c8, var = hq // 2, hq % 2
                bank = bk[a // 2]
                P.op('pe', lambda e, a=a, c8=c8, var=var, bank=bank: e.matmul(
                    G.ps[bank][:, (a % 2) * 256:(a % 2 + 1) * 256], qT[:, c8, cols], kT[:, g, var, i * 128:i * 128 + 256], start=True, stop=True),
                    w=[('ps', bank)])
            yield
            for hh in range(2):
                bank = bk[hh]
                P.op('dve', lambda e, hh=hh, bank=bank: e.tensor_tensor(
                    sm[:, hh * 2:(hh + 1) * 2, :], G.ps[bank][:, :].rearrange("p (a k) -> p a k", k=256),
                    mask[:, mi:mi + 1, :].broadcast_to([128, 2, 256]), ALU.add),
                    r=[('ps', bank)], w=[kk(('sm', hh))])
            yield
            P.op('dve', lambda e: e.tensor_reduce(mx[:, 0:4], sm[:, :, :], AX.X, ALU.max), r=[kk(('sm', 0)), kk(('sm', 1))], w=[kk('mx_m')])
            yield
            P.op('dve', lambda e: e.tensor_tensor(mx[:, 0:4], mx[:, 0:4], sink[:, 4 * g:4 * g + 4], ALU.max), r=[kk('mx_m')], w=[kk('mx_m')])
            yield
            P.op('dve', lambda e: e.tensor_scalar(mx[:, 4:8], mx[:, 0:4], -1.0, None, ALU.mult), r=[kk('mx_m')], w=[kk('mx_n')])
            yield
            for a in range(4):
                P.op('act', lambda e, a=a: e.activation(p[:, a, :], sm[:, a, :], AF.Exp, bias=mx[:, 4 + a:5 + a], scale=1.0, accum_out=mx[:, 8 + a:9 + a]),
                     r=[kk(('sm', a // 2)), kk('mx_n')], w=[kk('p'), kk(('mx_s', a))])
            P.op('dve', lambda e: e.tensor_tensor(mx[:, 12:16], sink[:, 4 * g:4 * g + 4], mx[:, 4:8], ALU.add), r=[kk('mx_n')], w=[kk('mx_d')])
            yield
            P.op('act', lambda e: e.activation(mx[:, 12:16], mx[:, 12:16], AF.Exp), r=[kk('mx_d')], w=[kk('mx_d')])
            yield
            P.op('dve', lambda e: e.tensor_tensor(mx[:, 12:16], mx[:, 12:16], mx[:, 8:12], ALU.add), r=[kk('mx_d')] + [kk(('mx_s', a)) for a in range(4)], w=[kk('mx_d')])
            yield
            P.op('dve', lambda e: e.reciprocal(mx[:, 12:16], mx[:, 12:16]), r=[kk('mx_d')], w=[kk('mx_d')])
            yield
            P.op('dve', lambda e: e.tensor_tensor(p[:, :, :], p[:, :, :], mx[:, 12:16].rearrange("p (a o) -> p a o", o=1).broadcast_to([128, 4, 256]), ALU.mult),
                 r=[kk('p'), kk('mx_d')], w=[kk('p')])
            yield
            for hh in range(2):
                def ftr(e, hh=hh):
                    ins = None
                    for q in range(4):
                        idx = hh * 4 + q
                        a, kt = idx // 2, idx % 2
                        ins = e.matmul(G.ps[bk[hh]][:, q * 128:(q + 1) * 128], p[:, a, kt * 128:(kt + 1) * 128], G.ident_b[:, :], start=True, stop=True)
                    return ins
                P.op('pe', ftr, r=[kk('p'), 'ident_b', kk(('sm', hh))], w=[('ps', bk[hh])])
            yield
            P.op('act', lambda e: e.activation(pT[:, 0:4, :], G.ps[bk[0]][:, :].rearrange("p (a t) -> p a t", t=128), AF.Copy), r=[('ps', bk[0])], w=[kk(('pT', 0))])
            P.op('dve', lambda e: e.tensor_copy(pT[:, 4:8, :], G.ps[bk[1]][:, :].rearrange("p (a t) -> p a t", t=128)), r=[('ps', bk[1])], w=[kk(('pT', 1))])
            yield
            for pr in range(2):
                pso = G.ps[bk[pr]]

                def fpv(e, pso=pso, pr=pr):
                    ins = None
                    n = 0
                    for (a, var) in ((2 * pr, 0), (2 * pr + 1, 1)):
                        for kt in range(2):
                            ins = e.matmul(pso[:, 0:128], vt[:, i + kt, g, var, :], pT[:, a * 2 + kt, :], start=(n == 0), stop=(n == 3))
                            n += 1
                    return ins
                P.op('pe', fpv, r=[kk(('pT', pr))], w=[('ps', bk[pr])])
                yield
                P.op('act', lambda e, pso=pso, pr=pr: e.activation(xo[:, pr, :], pso[:, 0:128], AF.Copy), r=[('ps', bk[pr])], w=[kk(('xo', pr))])
                yield
            P.dma('sp', G.xT['c'][g * 256:(g + 1) * 256, cols].rearrange("(i p) t -> p i t", p=128), xo[:, :, :],
                  r=[kk(('xo', 0)), kk(('xo', 1))], w=[('xcT', g, i)])
            yield
        units = [(i, g) for i in range(NT) for g in range(4)]
        rr((s2unit(i, g, n % NSL) for n, (i, g) in enumerate(units)), NSL)


def phase_merge(G, l):
    P = G.P
    G.ps_sel = list(range(8))
    w_in = G.W['w_in'][l]
    with contextlib.ExitStack() as st:
        sb = lambda n, s, d: G.sb(n, s, d, st)
        xs = {b: sb("m_x" + b, [128, KC, S], BF16) for b in 'abc'}
        ws_save = G.ws
        G.ws = list(G.ws) + [sb("m_ws%d" % i, [128, KC, 512], BF16) for i in range(2)]
        G.ws_rr = 4 if getattr(G, 'prefetched', None) else 0
        sg = [sb("m_sg%d" % i, [128, 512], F32) for i in range(3)]
        macc = sb("m_acc", [128, 512], F32)
        mo = [sb("m_o%d" % i, [128, 512], BF16) for i in range(2)]
        for b in 'abc':
            for k in range(KC):
                P.dma('sp', xs[b][:, k, :], G.xT[b][k * 128:(k + 1) * 128, :], w=[('x', b)] if k == 0 else [('x', b, k)])
        xkeys = {b: [('x', b)] + [('x', b, k) for k in range(1, KC)] for b in 'abc'}
        wouts = {'a': G.W['ret_w_out'][l], 'b': G.W['hgrn_w_out'][l], 'c': G.W['swa_w_out'][l]}
        goff = {'a': OFF['ga'], 'b': OFF['gb'], 'c': OFF['gc']}
        n = 0
        for cb in range(2):
            wy = {b: load_w(G, wouts[b], cb * 512, 512, tag=('my', l, b, cb)) for b in 'abc'}
            wg = {b: load_w(G, w_in, goff[b] + cb * 512, 512, tag=('mg', l, b, cb)) for b in 'abc'}
            for cc in range(4):
                for tg in range(4):
                    oi = n % 2
                    n += 1
                    for bi, b in enumerate('abc'):
                        psy, pky = fm_group(G, wy[b][0], wy[b][1], cc, tg, rkeys=xkeys[b], src=xs[b])
                        psg, pkg = fm_group(G, wg[b][0], wg[b][1], cc, tg)
                        P.op('act', lambda e, psg=psg, bi=bi: e.activation(sg[bi][:, :], psg[:, :], AF.Sigmoid), r=[pkg], w=[('sg', bi)])
                        if bi == 0:
                            P.op('dve', lambda e, psy=psy, bi=bi: e.tensor_tensor(macc[:, :], sg[bi][:, :], psy[:, :], ALU.mult), r=[('sg', bi), pky], w=['macc'])
                        else:
                            P.op('dve', lambda e, psy=psy, bi=bi: e.tensor_tensor(sg[bi][:, :], sg[bi][:, :], psy[:, :], ALU.mult), r=[('sg', bi), pky], w=[('sg', bi)])
                            if bi == 1:
                                P.op('dve', lambda e, bi=bi: e.tensor_tensor(macc[:, :], macc[:, :], sg[bi][:, :], ALU.add), r=['macc', ('sg', bi)], w=['macc'])
                            else:
                                P.op('dve', lambda e, bi=bi, oi=oi: e.tensor_tensor(mo[oi][:, :], macc[:, :], sg[bi][:, :], ALU.add), r=['macc', ('sg', bi)], w=[('mo', oi)])
                    c8 = cb * 4 + cc
                    P.dma('sp', G.mgT[c8 * 128:(c8 + 1) * 128, tg * 512:(tg + 1) * 512], mo[oi][:, :], r=[('mo', oi)], w=[('mgT', c8, tg)])
        G.ws = ws_save
        G.ws_rr = 0


def router_tile(G, R, tile, pss, slot=0):
    P = G.P
    wr, hTfs, lgs, rrs = R
    hTf, lg, rr_ = hTfs[slot], lgs[slot], rrs[slot]
    k = lambda n: (n, slot)
    for hb, (ps, pk) in enumerate(pss):
        P.op('act', lambda e, ps=ps, hb=hb: e.activation(hTf[:, hb * 4:(hb + 1) * 4, :], ps[:, :].rearrange("p (a t) -> p a t", t=128), AF.Copy),
             r=[pk], w=[k(('hTf', hb))])
        yield
    ps, pk = next_ps(G)

    def fr(e, ps=ps):
        ins = None
        for kk in range(KC):
            ins = e.matmul(ps[:, 0:NEXP], hTf[:, kk, :], wr[:, kk, :], start=(kk == 0), stop=(kk == KC - 1))
        return ins
    P.op('pe', fr, r=[k(('hTf', 0)), k(('hTf', 1)), 'wr'], w=[pk])
    yield
    P.op('dve', lambda e, ps=ps: e.tensor_copy(lg[:, 0:8], ps[:, 0:NEXP]), r=[pk], w=[k('lg')])
    yield
    P.op('dve', lambda e: e.tensor_reduce(rr_[:, 0:1], lg[:, 0:8], AX.X, ALU.max), r=[k('lg')], w=[k('rr0')])
    yield
    P.op('dve', lambda e: e.tensor_scalar(lg[:, 8:16], lg[:, 0:8], rr_[:, 0:1], None, ALU.is_equal), r=[k('lg'), k('rr0')], w=[k('lg1')])
    yield
    P.op('dve', lambda e: e.scalar_tensor_tensor(lg[:, 8:16], lg[:, 8:16], -1e30, lg[:, 0:8], ALU.mult, ALU.add), r=[k('lg'), k('lg1')], w=[k('lg1')])
    yield
    P.op('dve', lambda e: e.tensor_reduce(rr_[:, 1:2], lg[:, 8:16], AX.X, ALU.max), r=[k('lg1')], w=[k('rr1')])
    yield
    P.op('dve', lambda e: e.tensor_scalar(lg[:, 8:16], lg[:, 0:8], rr_[:, 1:2], None, ALU.is_ge), r=[k('lg'), k('rr1'), k('lg1')], w=[k('lg1')])
    P.op('dve', lambda e: e.tensor_scalar(rr_[:, 2:3], rr_[:, 0:1], -1.0, None, ALU.mult), r=[k('rr0')], w=[k('rr2')])
    yield
    P.op('act', lambda e: e.activation(lg[:, 16:24], lg[:, 0:8], AF.Exp, bias=rr_[:, 2:3], scale=1.0), r=[k('lg'), k('rr2')], w=[k('lg2')])
    yield
    P.op('dve', lambda e: e.tensor_tensor(lg[:, 16:24], lg[:, 16:24], lg[:, 8:16], ALU.mult), r=[k('lg2'), k('lg1')], w=[k('lg2')])
    yield
    P.op('dve', lambda e: e.tensor_reduce(rr_[:, 3:4], lg[:, 16:24], AX.X, ALU.add), r=[k('lg2')], w=[k('rr3')])
    yield
    P.op('dve', lambda e: e.reciprocal(rr_[:, 3:4], rr_[:, 3:4]), r=[k('rr3')], w=[k('rr3')])
    yield
    P.op('dve', lambda e, tile=tile: e.tensor_scalar(G.gate_all[:, tile, :], lg[:, 16:24], rr_[:, 3:4], None, ALU.mult), r=[k('lg2'), k('rr3')], w=[('gate', tile)])
    yield


def phase_wo(G, l):
    P = G.P
    G.ps_sel = list(range(8))
    with contextlib.ExitStack() as st:
        sb = lambda n, s, d: G.sb(n, s, d, st)
        mg = sb("o_mg", [128, KC, S], BF16)
        G.xt = [sb("o_xt%d" % i, [128, D], F32) for i in range(LN_W + 1)]
        G.gbc = sb("o_gbc", [128, D], F32)
        G.bbc = sb("o_bbc", [128, D], F32)
        R = None
        if l % 2 == 1:
            wr = sb("o_wr", [128, KC, NEXP], F32)
            hTfs = [sb("o_hTf%d" % i, [128, KC, 128], F32) for i in range(LN_W)]
            lgs = [sb("o_lg%d" % i, [128, 24], F32) for i in range(LN_W)]
            rrs = [sb("o_rr%d" % i, [128, 4], F32) for i in range(LN_W)]
            R = (wr, hTfs, lgs, rrs)
            P.dma('sp', wr[:, :, :], G.W['moe_router'][l // 2].rearrange("(k p) e -> p k e", p=128), w=['wr'])
        for k in range(KC):
            P.dma('sp', mg[:, k, :], G.mgT[k * 128:(k + 1) * 128, :], w=[('mg', k)])
        mgk = [('mg', k) for k in range(KC)]
        load_ln_params(G, G.W['ln_mix_g'][l], G.W['ln_mix_b'][l])
        w0 = load_w(G, G.W['w_o'][l], 0, 512, tag=('wo', l))
        w1 = load_w(G, G.W['w_o'][l], 512, 512)

        def unit(t):
            xt, xk = next_xt(G)
            P.dma('sp', xt[:, :], G.h_res[t * 128:(t + 1) * 128, :], r=[('h_res', t)], w=[xk])
            yield
            for hf, (wt, wk) in enumerate((w0, w1)):
                ps, pk = tm_group(G, wt, wk, t, rkeys=mgk, src=mg)
                yield
                P.op('dve', lambda e, ps=ps, xt=xt, hf=hf: e.scalar_tensor_tensor(
                    xt[:, hf * 512:(hf + 1) * 512], xt[:, hf * 512:(hf + 1) * 512], DN_ALPHA, ps[:, :], ALU.mult, ALU.add), r=[pk, xk], w=[xk])
                yield
            pss = []
            yield from ln_tile(G, xt, xk, t, slot=t % LN_W, pss=pss)
            if R is not None:
                yield from router_tile(G, R, t, pss, slot=t % LN_W)
        rr((unit(t) for t in range(NT)), LN_W)


def phase_ffn(G, l):
    P = G.P
    G.ps_sel = list(range(8))
    moe = (l % 2 == 1)
    jx = l // 2
    last = (l == DEPTH - 1)
    with contextlib.ExitStack() as st:
        sb = lambda n, s, d: G.sb(n, s, d, st)
        yacc = sb("f_y", [128, NT, D], F32)
        hid = sb("f_hid", [128, 4, S], BF16)
        wds = [sb("f_wd%d" % i, [128, 4, D], BF16) for i in range(2)]
        sl = [sb("f_sl%d" % i, [128, 512], F32) for i in range(2)]
        G.xt = [sb("f_xt%d" % i, [128, D], F32) for i in range(LN_W + 1)]
        G.xt_rr = 0
        G.gbc = sb("f_gbc", [128, D], F32)
        G.bbc = sb("f_bbc", [128, D], F32)
        if moe:
            experts = [(G.W['moe_w_gate'][jx][e], G.W['moe_w_up'][jx][e], G.W['moe_w_down'][jx][e], e) for e in range(NEXP)]
            F = D_EXP
        else:
            experts = [(G.W['ffn_w_gate'][jx], G.W['ffn_w_up'][jx], G.W['ffn_w_down'][jx], None)]
            F = D_FF
        nblk = (F + 511) // 512
        load_ln_params(G, G.W['ln_ffn_g'][l], G.W['ln_ffn_b'][l])
        ln_active = []

        def unit(t):
            xt, xk = next_xt(G)
            P.dma('sp', xt[:, :], G.h_res[t * 128:(t + 1) * 128, :], r=[('h_res', t)], w=[xk])
            yield
            P.op('dve', lambda e, xt=xt, t=t: e.scalar_tensor_tensor(xt[:, :], xt[:, :], DN_ALPHA, yacc[:, t, :], ALU.mult, ALU.add),
                 r=[xk, ('y', t, 0), ('y', t, 1)], w=[xk])
            yield
            yield from ln_tile(G, xt, xk, t, slot=t % LN_W, out_final=(G.out if last else None))

        first = True
        n = 0
        wdi = 0
        for (wg_d, wu_d, wd_d, e_idx) in experts:
            for blk in range(nblk):
                ncols = min(512, F - blk * 512)
                nj = ncols // 128
                wg, wgk = load_w(G, wg_d, blk * 512, ncols, tag=('ffn', l, e_idx, blk))
                wu, wuk = load_w(G, wu_d, blk * 512, ncols)
                wd = wds[wdi % 2]
                wdk = ('wd', wdi % 2)
                wdi += 1
                P.dma('pool', wd[:, 0:nj, :], wd_d[blk * 512:blk * 512 + ncols, :].rearrange("(j p) c -> p j c", p=128), w=[wdk])
                for j in range(nj):
                    for tg in range(4):
                        si = n % 2
                        n += 1
                        psg, pkg = fm_group(G, wg, wgk, j, tg)
                        psu, pku = fm_group(G, wu, wuk, j, tg)
                        P.op('act', lambda e, psg=psg, si=si: e.activation(sl[si][:, :], psg[:, :], AF.Silu), r=[pkg], w=[('sl', si)])
                        P.op('dve', lambda e, psu=psu, si=si, j=j, tg=tg: e.tensor_tensor(hid[:, j, tg * 512:(tg + 1) * 512], sl[si][:, :], psu[:, :], ALU.mult),
                             r=[('sl', si), pku], w=[('hid', j, tg)])
                is_last = (e_idx is None or e_idx == NEXP - 1) and blk == nblk - 1
                for t in range(NT):
                    for hf in range(2):
                        ps, pk = next_ps(G)

                        def fd(e, ps=ps, t=t, hf=hf, nj=nj, wd=wd):
                            ins = None
                            for j in range(nj):
                                ins = e.matmul(ps[:, :], hid[:, j, t * 128:(t + 1) * 128], wd[:, j, hf * 512:(hf + 1) * 512], start=(j == 0), stop=(j == nj - 1))
                            return ins
                        P.op('pe', fd, r=[('hid', j, t // 4) for j in range(nj)] + [wdk], w=[pk])
                        ya = yacc[:, t, hf * 512:(hf + 1) * 512]
                        yk = ('y', t, hf)
                        if e_idx is None:
                            if first:
                                P.op('act', lambda e, ps=ps, ya=ya: e.activation(ya, ps[:, :], AF.Copy), r=[pk], w=[yk])
                            else:
                                P.op('dve', lambda e, ps=ps, ya=ya: e.tensor_tensor(ya, ya, ps[:, :], ALU.add), r=[pk, yk], w=[yk])
                        else:
                            gsc = G.gate_all[:, t, e_idx:e_idx + 1]
                            if first:
                                P.op('act', lambda e, ps=ps, ya=ya, gsc=gsc: e.activation(ya, ps[:, :], AF.Copy, scale=gsc), r=[pk, ('gate', t)], w=[yk])
                            else:
                                P.op('dve', lambda e, ps=ps, ya=ya, gsc=gsc: e.scalar_tensor_tensor(ya, ps[:, :], gsc, ya, ALU.mult, ALU.add),
                                     r=[pk, yk, ('gate', t)], w=[yk])
                    if is_last:
                        while len(ln_active) >= LN_W:
                            for g_ in list(ln_active):
                                try:
                                    next(g_)
                                except StopIteration:
                                    ln_active.remove(g_)
                        ln_active.append(unit(t))
                        for _ in range(4):
                            for g_ in list(ln_active):
                                try:
                                    next(g_)
                                except StopIteration:
                                    ln_active.remove(g_)
                first = False
        while ln_active:
            for g_ in list(ln_active):
                try:
                    next(g_)
                except StopIteration:
                    ln_active.remove(g_)


_CACHE = {}


def kernel(**inputs):
    if 'nc' not in _CACHE:
        _CACHE['nc'] = build()
    nc = _CACHE['nc']
    C = _consts()
    x = np.asarray(inputs['x'], np.float32)
    B = x.shape[0]
    base = {name: np.ascontiguousarray(np.asarray(inputs[name], np.float32)) for name, _ in W_SPECS}
    for name in CONST_NAMES:
        base["c_" + name] = np.ascontiguousarray(C[name])
    in_maps = []
    for b in range(B):
        m = dict(base)
        m['x'] = np.ascontiguousarray(x[b])
        in_maps.append(m)
    res = run_bass_kernel_spmd(nc, in_maps, core_ids=list(range(B)))
    return np.stack([np.asarray(r['out'], np.float32) for r in res.results], 0)
```

```python
import contextlib
import math
import numpy as np
import concourse.bass as bass
import concourse.mybir as mybir
from concourse.bass_utils import run_bass_kernel_spmd

F32 = mybir.dt.float32
BF16 = mybir.dt.bfloat16
ALU = mybir.AluOpType
AF = mybir.ActivationFunctionType
AX = mybir.AxisListType

S = 2048
D = 1024
NT = 16
KC = 8
DEPTH = 2
N_IN = 11776
D_FF = 2816
D_EXP = 3584
NEXP = 8
LN_EPS = 1e-5
RMS_EPS = 1e-6
DN_ALPHA = (2.0 * DEPTH) ** 0.25
OFF = dict(rq=0, rk=512, rv=1024, rg=2048, hq=3072, hf=4096, hi=5120, hg=6144,
           sq=7168, sk=8192, sv=8448, ga=8704, gb=9728, gc=10752)
HL = 32

COMPUTE = ('pe', 'act', 'dve', 'pool')
ALL_ENG = ('pe', 'act', 'dve', 'pool', 'sp')
NDMASEM = 6


class Op:
    __slots__ = ('eng', 'fn', 'deps', 'dma', 'sem', 'ticket', 'idx', 'prev_ticket', 'semkey')


class Prog:
    def __init__(self, nc, sems, dma_sems, same_engine_sync=True):
        self.nc = nc
        self.sems = sems
        self.dma_sems = dma_sems
        self.count = {e: 0 for e in COMPUTE}
        self.dma_count = {(q, i): 0 for q in dma_sems for i in range(len(dma_sems[q]))}
        self.dma_rr = {q: 0 for q in dma_sems}
        self.waited = {e: {} for e in ALL_ENG}
        self.same_engine_sync = same_engine_sync
        self.ops = []
        self.lw = {}
        self.rd = {}
        self.prev_final = []

    def op(self, eng, fn, r=(), w=(), dma=False):
        deps = set()
        for b in r:
            if b in self.lw:
                deps.add(self.lw[b])
        for b in w:
            if b in self.lw:
                deps.add(self.lw[b])
            for x in self.rd.get(b, ()):
                deps.add(x)
        o = Op()
        o.eng = eng; o.fn = fn; o.dma = dma; o.sem = None; o.ticket = None
        o.idx = len(self.ops)
        o.deps = deps
        self.ops.append(o)
        for b in r:
            self.rd.setdefault(b, []).append(o.idx)
        for b in w:
            self.lw[b] = o.idx
            self.rd[b] = []
        return o.idx

    def dma(self, q, out, in_, r=(), w=(), **kw):
        def fn(e):
            return e.dma_start(out=out, in_=in_, **kw)
        return self.op(q, fn, r, w, dma=True)

    def emit(self, final_wait=False):
        nc = self.nc
        ops = self.ops
        needed = set()
        for o in ops:
            for d in o.deps:
                od = ops[d]
                if od.eng == o.eng and not od.dma:
                    if od.eng == 'pe' or not self.same_engine_sync:
                        continue
                needed.add(d)
        last = {}
        for o in ops:
            if not o.dma:
                last[o.eng] = o.idx
        for e, i in last.items():
            needed.add(i)
        for o in ops:
            if o.dma:
                q = o.eng
                i = self.dma_rr[q]
                self.dma_rr[q] = (i + 1) % len(self.dma_sems[q])
                o.prev_ticket = self.dma_count[(q, i)]
                self.dma_count[(q, i)] += 16
                o.sem = self.dma_sems[q][i]
                o.semkey = ('d', q, i)
                o.ticket = self.dma_count[(q, i)]
            elif o.idx in needed:
                self.count[o.eng] += 1
                o.sem = self.sems[o.eng]
                o.semkey = ('c', o.eng)
                o.ticket = self.count[o.eng]
        by_eng = {e: [o for o in ops if o.eng == e] for e in ALL_ENG}
        prev_final = self.prev_final
        waited = self.waited
        same_sync = self.same_engine_sync

        def run(engname, eh):
            wd = waited[engname]

            def wait(semkey, sem, ticket):
                if wd.get(semkey, 0) >= ticket:
                    return
                eh.wait_ge(sem, ticket)
                wd[semkey] = ticket
            for (semkey, sem, ticket) in prev_final:
                if semkey == ('c', engname) and (engname == 'pe' or not same_sync):
                    continue
                wait(semkey, sem, ticket)
            for o in by_eng[engname]:
                for d in sorted(o.deps):
                    od = ops[d]
                    if od.ticket is None:
                        continue
                    if od.eng == o.eng and not od.dma and (engname == 'pe' or not same_sync):
                        continue
                    wait(od.semkey, od.sem, od.ticket)
                if o.dma and o.prev_ticket > 0:
                    wait(o.semkey, o.sem, o.prev_ticket)
                inst = o.fn(eh)
                if o.ticket is not None:
                    inst.then_inc(o.sem, 16 if o.dma else 1)
            if engname in self.dma_sems:
                for i, sem in enumerate(self.dma_sems[engname]):
                    t = self.dma_count[(engname, i)]
                    if t > 0:
                        wait(('d', engname, i), sem, t)
            if final_wait and engname == 'sp':
                for q in self.dma_sems:
                    for i, sem in enumerate(self.dma_sems[q]):
                        t = self.dma_count[(q, i)]
                        if t > 0:
                            wait(('d', q, i), sem, t)

        with nc.Block() as block:
            @block.tensor
            def _(e):
                run('pe', e)

            @block.scalar
            def _(e):
                run('act', e)

            @block.vector
            def _(e):
                run('dve', e)

            @block.gpsimd
            def _(e):
                run('pool', e)

            @block.sync
            def _(e):
                run('sp', e)
        fin = []
        for e in COMPUTE:
            if self.count[e] > 0:
                fin.append((('c', e), self.sems[e], self.count[e]))
        for (q, i), t in self.dma_count.items():
            if t > 0:
                fin.append((('d', q, i), self.dma_sems[q][i], t))
        self.prev_final = fin
        self.ops = []
        self.lw = {}
        self.rd = {}


class Ctx:
    pass


G_SKIP = set()
import os
HG_LEVEL = int(os.environ.get('HG_LEVEL', '4'))
SW_LEVEL = int(os.environ.get('SW_LEVEL', '3'))
SW_SUB = int(os.environ.get("SW_SUB", "3"))
SW_W = int(os.environ.get("SW_W", "4"))
LN_W = int(os.environ.get("LN_W", "4"))
HG_MERGE = bool(int(os.environ.get("HG_MERGE", "0")))
SAME_SYNC = bool(int(os.environ.get('SAME_SYNC', '1')))


def _consts():
    c = {}
    half = 64
    inv = (1.0 / (10000.0 ** np.linspace(0.0, 1.0, half, dtype=np.float32))).astype(np.float32)
    pos = np.arange(S, dtype=np.float32)
    ang = pos[:, None] * inv[None, :]
    cos = np.cos(ang).astype(np.float32).T
    sin = np.sin(ang).astype(np.float32).T
    c['cosT'] = np.ascontiguousarray(np.concatenate([cos, cos], 0))
    c['sinT'] = np.ascontiguousarray(np.concatenate([sin, sin], 0))
    pm = np.zeros((128, 128), np.float32)
    for d in range(64):
        pm[d + 64, d] = -1.0
        pm[d, d + 64] = 1.0
    c['pm'] = pm
    c['ident'] = np.eye(128, dtype=np.float32)
    lg = np.log(1.0 - 2.0 ** (-5.0 - np.arange(4, dtype=np.float64)))
    idx = np.arange(128, dtype=np.float64)
    dt = np.zeros((128, 4, 128), np.float32)
    qd = np.zeros((128, 4, 128), np.float32)
    kd = np.zeros((128, 4), np.float32)
    for h in range(4):
        diff = idx[None, :] - idx[:, None]
        dt[:, h, :] = np.where(diff >= 0, np.exp(np.maximum(diff, 0) * lg[h]), 0.0) * 128 ** -0.5
        qd[:, h, :] = np.exp((idx + 1.0) * lg[h])[None, :]
        kd[:, h] = np.exp((127.0 - idx) * lg[h]) * 128 ** -0.5
    c['ret_dt'] = dt
    c['ret_qd'] = qd
    c['ret_kd'] = kd
    c['ret_cd'] = [float(np.exp(128.0 * lg[h])) for h in range(4)]
    s_ = np.arange(128)
    bm = ((s_[:, None] // HL) == (s_[None, :] // HL)) & (s_[:, None] <= s_[None, :])
    c['hg_bm'] = bm.astype(np.float32)
    sm = np.ones((128, 512), np.float32)
    sm[:, ::HL] = 0.0
    c['hg_scanm'] = sm
    c['hg_blk'] = (s_[:, None] // HL == np.arange(128 // HL)[None, :]).astype(np.float32)
    NEG = -30000.0
    m1 = np.zeros((128, 256), np.float32)
    m1[:64, 192:256] = NEG
    m1[64:, 0:64] = NEG
    m0 = m1.copy()
    m0[:, 0:128] = NEG
    c['swa_mask'] = np.stack([m0, m1], 1)
    return c


CONST_NAMES = ['cosT', 'sinT', 'pm', 'ident', 'ret_dt', 'ret_qd', 'ret_kd', 'hg_bm', 'hg_scanm', 'hg_blk', 'swa_mask']
W_SPECS = [
    ('ln_in_g', [D]), ('ln_in_b', [D]), ('w_in', [DEPTH, D, N_IN]), ('ret_w_out', [DEPTH, D, D]),
    ('hgrn_lower_bounds', [DEPTH, D]), ('hgrn_norm_g', [DEPTH, 128]), ('hgrn_w_out', [DEPTH, D, D]),
    ('swa_sinks', [DEPTH, 16]), ('swa_w_out', [DEPTH, D, D]), ('w_o', [DEPTH, D, D]),
    ('ln_mix_g', [DEPTH, D]), ('ln_mix_b', [DEPTH, D]),
    ('ffn_w_gate', [1, D, D_FF]), ('ffn_w_up', [1, D, D_FF]), ('ffn_w_down', [1, D_FF, D]),
    ('moe_router', [1, D, NEXP]), ('moe_w_gate', [1, NEXP, D, D_EXP]), ('moe_w_up', [1, NEXP, D, D_EXP]),
    ('moe_w_down', [1, NEXP, D_EXP, D]), ('ln_ffn_g', [DEPTH, D]), ('ln_ffn_b', [DEPTH, D]),
]


def build(stop_after=None, debug=False):
    C = _consts()
    nc = bass.Bass("TRN2", target_bir_lowering=False)
    G = Ctx()
    G.nc = nc
    G.x = nc.dram_tensor("x", [S, D], F32, kind="ExternalInput").ap()
    G.W = {}
    for name, shp in W_SPECS:
        G.W[name] = nc.dram_tensor(name, shp, F32, kind="ExternalInput").ap()
    G.K = {}
    for name in CONST_NAMES:
        G.K[name] = nc.dram_tensor("c_" + name, list(C[name].shape), F32, kind="ExternalInput").ap()
    G.out = nc.dram_tensor("out", [S, D], F32, kind="ExternalOutput").ap()
    skind = "ExternalOutput" if debug else "Internal"
    G.h_res = nc.dram_tensor("h_res", [S, D], F32, kind=skind).ap()
    G.xT = {b: nc.dram_tensor("x%sT" % b, [D, S], BF16, kind=skind).ap() for b in 'abc'}
    G.mgT = nc.dram_tensor("mgT", [D, S], BF16, kind=skind).ap()
    G.cd = C['ret_cd']

    with contextlib.ExitStack() as es:
        uid = [0]

        def sb(name, shape, dt, st=es):
            uid[0] += 1
            return st.enter_context(nc.sbuf_tensor("%s_u%d" % (name, uid[0]), shape, dt))
        sems = {e: es.enter_context(nc.semaphore("s_" + e)) for e in COMPUTE}
        dsems = {q: [es.enter_context(nc.semaphore("d_%s%d" % (q, i))) for i in range(NDMASEM)]
                 for q in ('sp', 'pool')}
        P = Prog(nc, sems, dsems, same_engine_sync=SAME_SYNC)
        G.P = P
        G.sb = sb
        G.hT = sb("hT", [128, KC, S], BF16)
        G.ident_f = sb("ident_f", [128, 128], F32)
        G.ident_b = sb("ident_b", [128, 128], BF16)
        G.ws = [sb("ws%d" % i, [128, KC, 512], BF16) for i in range(4)]
        G.ws_rr = 0
        G.xt_rr = 0
        G.small = sb("small", [128, 64], F32)
        G.ps = [es.enter_context(nc.psum_tensor("ps%d" % i, [128, 512], F32)) for i in range(8)]
        G.ps_rr = 0
        G.gate_all = sb("gate_all", [128, NT, NEXP], F32)

        phases = []
        phases.append(('ln_in', lambda: phase_ln_in(G)))
        for l in range(DEPTH):
            phases.append(('ret%d' % l, lambda l=l: phase_ret(G, l)))
            phases.append(('hgrn%d' % l, lambda l=l: phase_hgrn(G, l)))
            phases.append(('swa%d' % l, lambda l=l: phase_swa(G, l)))
            phases.append(('merge%d' % l, lambda l=l: phase_merge(G, l)))
            phases.append(('wo%d' % l, lambda l=l: phase_wo(G, l)))
            phases.append(('ffn%d' % l, lambda l=l: phase_ffn(G, l)))
        for i, (name, fn) in enumerate(phases):
            if G_SKIP and name.rstrip('0123456789') in G_SKIP and name != stop_after:
                continue
            fn()
            lastp = (i == len(phases) - 1) or (name == stop_after)
            if not lastp and not G_SKIP:
                nxt = phases[i + 1][0]
                ln_ = int(nxt[-1]) if nxt[-1].isdigit() else 0
                kind = nxt.rstrip('0123456789')
                w_in_n = G.W['w_in'][ln_]
                if kind == 'ret':
                    prefetch_w(G, ('rv', ln_, 0), w_in_n, OFF['rv'], 512)
                elif kind == 'hgrn':
                    prefetch_w(G, ('hi', ln_, 0), w_in_n, OFF['hi'], 512)
                elif kind == 'swa':
                    prefetch_w(G, ('sq', ln_, 0), w_in_n, OFF['sq'], 512)
                elif kind == 'merge':
                    G.ws_rr = 0
                    prefetch_w(G, ('my', ln_, 'a', 0), G.W['ret_w_out'][ln_], 0, 512)
                    prefetch_w(G, ('my', ln_, 'b', 0), G.W['hgrn_w_out'][ln_], 0, 512)
                    prefetch_w(G, ('my', ln_, 'c', 0), G.W['swa_w_out'][ln_], 0, 512)
                    prefetch_w(G, ('mg', ln_, 'a', 0), w_in_n, OFF['ga'], 512)
                elif kind == 'wo':
                    prefetch_w(G, ('wo', ln_), G.W['w_o'][ln_], 0, 512)
                elif kind == 'ffn':
                    if ln_ % 2 == 1:
                        prefetch_w(G, ('ffn', ln_, 0, 0), G.W['moe_w_gate'][ln_ // 2][0], 0, 512)
                    else:
                        prefetch_w(G, ('ffn', ln_, None, 0), G.W['ffn_w_gate'][ln_ // 2], 0, 512)
            P.emit(final_wait=lastp)
            if lastp:
                break
    return nc


def next_ps(G):
    i = G.ps_rr % len(G.ps_sel)
    G.ps_rr = (i + 1) % len(G.ps_sel)
    j = G.ps_sel[i]
    return G.ps[j], ('ps', j)


def next_ws(G):
    i = G.ws_rr
    G.ws_rr = (i + 1) % len(G.ws)
    return G.ws[i], ('ws', i)


def load_w(G, wdram, c0, ncols, q='pool', tag=None):
    pf = getattr(G, 'prefetched', None)
    if tag is not None and pf and tag in pf:
        wt, key = pf.pop(tag)
        return wt, key
    wt, key = next_ws(G)
    G.P.dma(q, wt[:, :, 0:ncols], wdram[:, c0:c0 + ncols].rearrange("(k p) c -> p k c", p=128), w=[key])
    return wt, key


def prefetch_w(G, tag, wdram, c0, ncols):
    if len(G.ws) != 4:
        return
    wt, key = load_w(G, wdram, c0, ncols)
    if getattr(G, 'prefetched', None) is None:
        G.prefetched = {}
    G.prefetched[tag] = (wt, key)


def fm_group(G, wt, wkey, cc, tg, rkeys=('hT',), src=None, ncols_tok=512, t0=None):
    src = G.hT if src is None else src
    ps, pk = next_ps(G)
    t0 = tg * 512 if t0 is None else t0

    def fn(e, ps=ps, wt=wt, cc=cc, t0=t0, src=src):
        ins = None
        for k in range(KC):
            ins = e.matmul(ps[:, 0:ncols_tok], wt[:, k, cc * 128:(cc + 1) * 128], src[:, k, t0:t0 + ncols_tok],
                           start=(k == 0), stop=(k == KC - 1))
        return ins
    G.P.op('pe', fn, r=[wkey] + list(rkeys), w=[pk])
    return ps, pk


def tm_group(G, wt, wkey, tile, ncols=512, rkeys=('hT',), src=None):
    src = G.hT if src is None else src
    ps, pk = next_ps(G)

    def fn(e, ps=ps, wt=wt, tile=tile, src=src):
        ins = None
        for k in range(KC):
            ins = e.matmul(ps[:, 0:ncols], src[:, k, tile * 128:(tile + 1) * 128], wt[:, k, 0:ncols],
                           start=(k == 0), stop=(k == KC - 1))
        return ins
    G.P.op('pe', fn, r=[wkey] + list(rkeys), w=[pk])
    return ps, pk


def load_consts_common(G):
    P = G.P
    P.dma('sp', G.ident_f[:, :], G.K['ident'], w=['ident_f'])
    P.dma('pool', G.ident_b[:, :], G.K['ident'], w=['ident_b'])


def rr(gens, width):
    active = []
    it = iter(gens)
    done = False
    while True:
        while len(active) < width and not done:
            try:
                active.append(next(it))
            except StopIteration:
                done = True
        if not active:
            break
        for g in list(active):
            try:
                next(g)
            except StopIteration:
                active.remove(g)


def ln_tile(G, xt, xkey, tile, slot=0, out_final=None, pss=None):
    P = G.P
    sm = G.small
    o = slot * 16
    st = sm[:, o:o + 12]
    mv = sm[:, o + 12:o + 14]
    rs = sm[:, o + 14:o + 15]
    k = lambda n: (n, slot)
    P.op('dve', lambda e: e.bn_stats(st[:, 0:6], xt[:, 0:512]), r=[xkey], w=[k('ln_st0')])
    P.op('dve', lambda e: e.bn_stats(st[:, 6:12], xt[:, 512:1024]), r=[xkey], w=[k('ln_st1')])
    yield
    P.op('dve', lambda e: e.bn_aggr(mv, st), r=[k('ln_st0'), k('ln_st1')], w=[k('ln_mv')])
    yield
    P.op('act', lambda e: e.activation(rs, mv[:, 1:2], AF.Sqrt, bias=LN_EPS, scale=1.0), r=[k('ln_mv')], w=[k('ln_rs')])
    yield
    P.op('dve', lambda e: e.reciprocal(rs, rs), r=[k('ln_rs')], w=[k('ln_rs')])
    yield
    P.op('dve', lambda e: e.scalar_tensor_tensor(xt[:, :], xt[:, :], mv[:, 0:1], G.gbc[:, :], ALU.subtract, ALU.mult),
         r=[xkey, k('ln_mv'), 'gbc'], w=[xkey])
    yield
    P.op('dve', lambda e: e.scalar_tensor_tensor(xt[:, :], xt[:, :], rs, G.bbc[:, :], ALU.mult, ALU.add),
         r=[xkey, k('ln_rs'), 'bbc'], w=[xkey])
    yield
    rows = slice(tile * 128, (tile + 1) * 128)
    if out_final is not None:
        P.dma('sp', out_final[rows, :], xt[:, :], r=[xkey], w=[('out', tile)])
    else:
        P.dma('sp', G.h_res[rows, :], xt[:, :], r=[xkey], w=[('h_res', tile)])
        for hb in range(2):
            ps, pk = next_ps(G)

            def fn(e, ps=ps, hb=hb):
                ins = None
                for i in range(4):
                    kk = hb * 4 + i
                    ins = e.matmul(ps[:, i * 128:(i + 1) * 128], xt[:, kk * 128:(kk + 1) * 128], G.ident_f[:, :],
                                   start=True, stop=True)
                return ins
            P.op('pe', fn, r=[xkey, 'ident_f'], w=[pk])
            yield
            P.op('act', lambda e, ps=ps, hb=hb: e.activation(
                G.hT[:, hb * 4:(hb + 1) * 4, tile * 128:(tile + 1) * 128],
                ps[:, :].rearrange("p (a t) -> p a t", t=128), AF.Copy), r=[pk], w=['hT'])
            if pss is not None:
                pss.append((ps, pk))
            yield


def load_ln_params(G, g_ap, b_ap):
    G.P.dma('sp', G.gbc[:, :], g_ap.partition_broadcast(128), w=['gbc'])
    G.P.dma('sp', G.bbc[:, :], b_ap.partition_broadcast(128), w=['bbc'])


def next_xt(G):
    i = G.xt_rr
    G.xt_rr = (i + 1) % len(G.xt)
    return G.xt[i], ('xt', i)


def phase_ln_in(G):
    P = G.P
    G.ps_sel = list(range(8))
    with contextlib.ExitStack() as st:
        sb = lambda n, s, d: G.sb(n, s, d, st)
        G.xt = [sb("i_xt%d" % i, [128, D], F32) for i in range(LN_W + 1)]
        G.gbc = sb("i_gbc", [128, D], F32)
        G.bbc = sb("i_bbc", [128, D], F32)
        load_consts_common(G)
        load_ln_params(G, G.W['ln_in_g'], G.W['ln_in_b'])
        def unit(t):
            xt, xk = next_xt(G)
            P.dma('sp', xt[:, :], G.x[t * 128:(t + 1) * 128, :], w=[xk])
            yield
            yield from ln_tile(G, xt, xk, t, slot=t % LN_W)
        rr((unit(t) for t in range(NT)), LN_W)


def phase_ret(G, l):
    P = G.P
    nc = G.nc
    w_in = G.W['w_in'][l]
    G.ps_sel = [0, 1]
    with contextlib.ExitStack() as st:
        sb = lambda n, s, d: G.sb(n, s, d, st)
        v_tok = sb("r_v", [128, NT, 512], BF16)
        g_tok = sb("r_g", [128, NT, 512], BF16)
        qT = sb("r_q", [128, 2, S], BF16)
        kT = sb("r_k", [128, 2, S], BF16)
        cs = [sb("r_cs%d" % i, [128, 2, 512], F32) for i in range(2)]
        xf = [sb("r_xf%d" % i, [128, 512], F32) for i in range(2)]
        t1 = [sb("r_t1%d" % i, [128, 512], F32) for i in range(2)]
        pm = sb("r_pm", [128, 128], F32)
        dtm = sb("r_dt", [128, 4, 128], F32)
        qdm = sb("r_qd", [128, 4, 128], F32)
        kdm = sb("r_kd", [128, 4], F32)
        scm = [sb("r_scm%d" % i, [128, 128], BF16) for i in range(2)]
        qd = [sb("r_qdd%d" % i, [128, 128], BF16) for i in range(2)]
        kd = [sb("r_kdd%d" % i, [128, 128], BF16) for i in range(2)]
        stf = [sb("r_stf%d" % i, [128, 256], F32) for i in range(2)]
        stb = [sb("r_stb%d" % i, [128, 256], BF16) for i in range(2)]
        xg = [sb("r_xg%d" % i, [128, 512], BF16) for i in range(2)]
        xo = [sb("r_xo%d" % i, [128, 4, 128], BF16) for i in range(2)]
        ss2 = [sb("r_ss%d" % i, [128, 4], F32) for i in range(2)]
        junk = sb("r_junk", [128, 256], F32)
        ps_o = [G.ps[2], G.ps[3]]
        ps_s = [G.ps[4], G.ps[5]]
        ps_m = [G.ps[6], G.ps[7]]
        P.dma('sp', pm[:, :], G.K['pm'], w=['pm'])
        P.dma('sp', dtm[:, :, :], G.K['ret_dt'], w=['dtm'])
        P.dma('sp', qdm[:, :, :], G.K['ret_qd'], w=['qdm'])
        P.dma('sp', kdm[:, :], G.K['ret_kd'], w=['kdm'])
        for hp in range(2):
            G.ps_sel = list(range(8))
            wt, wk = load_w(G, w_in, OFF['rv'] + hp * 512, 512, tag=('rv', l, hp))
            for t in range(NT):
                ps, pk = tm_group(G, wt, wk, t)
                P.op('act', lambda e, ps=ps, t=t: e.activation(v_tok[:, t, :], ps[:, :], AF.Copy), r=[pk], w=[('v', t)])
            wt, wk = load_w(G, w_in, OFF['rg'] + hp * 512, 512)
            for t in range(NT):
                ps, pk = tm_group(G, wt, wk, t)
                P.op('act', lambda e, ps=ps, t=t: e.activation(g_tok[:, t, :], ps[:, :], AF.Silu), r=[pk], w=[('g', t)])
            for (nm, dst) in (('rq', qT), ('rk', kT)):
                wt, wk = load_w(G, w_in, OFF[nm] + hp * 256, 256)
                for tg in range(4):
                    ci = tg % 2
                    P.dma('sp', cs[ci][:, 0, :], G.K['cosT'][:, tg * 512:(tg + 1) * 512], w=[('cs', ci, 0)])
                    P.dma('sp', cs[ci][:, 1, :], G.K['sinT'][:, tg * 512:(tg + 1) * 512], w=[('cs', ci, 1)])
                    for j in range(2):
                        bi = (tg * 2 + j) % 2
                        ps, pk = fm_group(G, wt, wk, j, tg)
                        P.op('act', lambda e, ps=ps, bi=bi: e.activation(xf[bi][:, :], ps[:, :], AF.Copy), r=[pk], w=[('xf', bi)])
                        ps2, pk2 = next_ps(G)
                        P.op('pe', lambda e, ps2=ps2, bi=bi: e.matmul(ps2[:, :], pm[:, :], xf[bi][:, :], start=True, stop=True),
                             r=[('xf', bi), 'pm'], w=[pk2])
                        P.op('dve', lambda e, bi=bi, ci=ci: e.tensor_tensor(t1[bi][:, :], xf[bi][:, :], cs[ci][:, 0, :], ALU.mult),
                             r=[('xf', bi), ('cs', ci, 0)], w=[('t1', bi)])
                        P.op('dve', lambda e, ps2=ps2, bi=bi, ci=ci: e.tensor_tensor(xf[bi][:, :], ps2[:, :], cs[ci][:, 1, :], ALU.mult),
                             r=[pk2, ('cs', ci, 1)], w=[('xf', bi)])
                        P.op('dve', lambda e, bi=bi, dst=dst, j=j, tg=tg: e.tensor_tensor(
                            dst[:, j, tg * 512:(tg + 1) * 512], t1[bi][:, :], xf[bi][:, :], ALU.add),
                            r=[('t1', bi), ('xf', bi)], w=[(nm, j, tg)])
            G.ps_sel = [0, 1]
            pending = None
            for n in range(NT):
                pso = ps_o[n % 2]
                pok = ('ps', 2 + n % 2)
                tgk = n // 4
                cols = slice(n * 128, (n + 1) * 128)
                for j in range(2):
                    h = hp * 2 + j
                    pss_ = ps_s[j]
                    psk = ('ps', 4 + j)
                    P.op('pe', lambda e, pss_=pss_, j=j, cols=cols: e.matmul(pss_[:, 0:128], kT[:, j, cols], qT[:, j, cols], start=True, stop=True),
                         r=[('rk', j, tgk), ('rq', j, tgk)], w=[psk])
                    P.op('dve', lambda e, pss_=pss_, j=j, h=h: e.tensor_tensor(scm[j][:, :], pss_[:, 0:128], dtm[:, h, :], ALU.mult),
                         r=[psk, 'dtm'], w=[('scm', j)])
                    if n > 0:
                        P.op('dve', lambda e, j=j, h=h, cols=cols: e.tensor_tensor(qd[j][:, :], qT[:, j, cols], qdm[:, h, :], ALU.mult),
                             r=[('rq', j, tgk), 'qdm'], w=[('qd', j)])
                if n < NT - 1:
                    for j in range(2):
                        h = hp * 2 + j
                        psm = ps_m[j]
                        pmk = ('ps', 6 + j)
                        P.op('pe', lambda e, psm=psm, j=j, cols=cols: e.matmul(psm[:, 0:128], kT[:, j, cols], G.ident_b[:, :], start=True, stop=True),
                             r=[('rk', j, tgk), 'ident_b'], w=[pmk])
                        P.op('act', lambda e, psm=psm, j=j, h=h: e.activation(kd[j][:, :], psm[:, 0:128], AF.Copy, scale=kdm[:, h:h + 1]),
                             r=[pmk, 'kdm'], w=[('kd', j)])
                for j in range(2):
                    def fo(e, pso=pso, j=j, n=n):
                        ins = e.matmul(pso[:, j * 256:(j + 1) * 256], scm[j][:, :], v_tok[:, n, j * 256:(j + 1) * 256],
                                       start=True, stop=(n == 0))
                        if n > 0:
                            ins = e.matmul(pso[:, j * 256:(j + 1) * 256], qd[j][:, :], stb[j][:, :], start=False, stop=True)
                        return ins
                    P.op('pe', fo, r=[('scm', j), ('v', n), ('qd', j), ('stb', j)], w=[pok])
                if n < NT - 1:
                    for j in range(2):
                        h = hp * 2 + j
                        psm = ps_m[j]
                        pmk = ('ps', 6 + j)
                        P.op('pe', lambda e, psm=psm, j=j, n=n: e.matmul(psm[:, 128:384], kd[j][:, :], v_tok[:, n, j * 256:(j + 1) * 256], start=True, stop=True),
                             r=[('kd', j), ('v', n)], w=[pmk])
                        if n == 0:
                            P.op('dve', lambda e, psm=psm, j=j: e.tensor_copy(stf[j][:, :], psm[:, 128:384]), r=[pmk], w=[('stf', j)])
                        else:
                            P.op('dve', lambda e, psm=psm, j=j, h=h: e.scalar_tensor_tensor(
                                stf[j][:, :], stf[j][:, :], G.cd[h], psm[:, 128:384], ALU.mult, ALU.add),
                                r=[pmk, ('stf', j)], w=[('stf', j)])
                        P.op('act', lambda e, j=j: e.activation(stb[j][:, :], stf[j][:, :], AF.Copy), r=[('stf', j)], w=[('stb', j)])
                xi = n % 2
                ss = ss2[xi]
                for j in range(2):
                    P.op('act', lambda e, pso=pso, j=j, ss=ss: e.activation(junk[:, :], pso[:, j * 256:(j + 1) * 256], AF.Square, accum_out=ss[:, j:j + 1]),
                         r=[pok], w=['junk', ('ss', xi, j)])
                P.op('act', lambda e, ss=ss: e.activation(ss[:, 2:4], ss[:, 0:2], AF.Sqrt, bias=RMS_EPS, scale=1.0 / 256.0),
                     r=[('ss', xi, 0), ('ss', xi, 1)], w=[('rstd', xi)])
                P.op('dve', lambda e, ss=ss: e.reciprocal(ss[:, 2:4], ss[:, 2:4]), r=[('rstd', xi)], w=[('rstd', xi)])
                for j in range(2):
                    P.op('dve', lambda e, pso=pso, j=j, n=n, xi=xi, ss=ss: e.scalar_tensor_tensor(
                        xg[xi][:, j * 256:(j + 1) * 256], pso[:, j * 256:(j + 1) * 256], ss[:, 2 + j:3 + j],
                        g_tok[:, n, j * 256:(j + 1) * 256], ALU.mult, ALU.mult),
                        r=[pok, ('rstd', xi), ('g', n)], w=[('xg', xi, j)])
                if pending is not None:
                    pending()

                def back_b(n=n, xi=xi, cols=cols, hp=hp):
                    ps, pk = next_ps(G)

                    def ft(e, ps=ps, xi=xi):
                        ins = None
                        for i in range(4):
                            ins = e.matmul(ps[:, i * 128:(i + 1) * 128], xg[xi][:, i * 128:(i + 1) * 128], G.ident_b[:, :], start=True, stop=True)
                        return ins
                    P.op('pe', ft, r=[('xg', xi, 0), ('xg', xi, 1), 'ident_b'], w=[pk])
                    P.op('act', lambda e, ps=ps, xi=xi: e.activation(xo[xi][:, :, :], ps[:, :].rearrange("p (a t) -> p a t", t=128), AF.Copy),
                         r=[pk], w=[('xo', xi)])
                    P.dma('sp', G.xT['a'][hp * 512:(hp + 1) * 512, cols].rearrange("(i p) t -> p i t", p=128), xo[xi][:, :, :],
                          r=[('xo', xi)], w=[('xaT', hp, n)])
                pending = back_b
            if pending is not None:
                pending()
                pending = None


def phase_hgrn(G, l):
    P = G.P
    w_in = G.W['w_in'][l]
    G.ps_sel = [0, 1]
    with contextlib.ExitStack() as st:
        sb = lambda n, s, d: G.sb(n, s, d, st)
        v_tok = sb("h_v", [128, NT, 512], BF16)
        qtT = sb("h_qt", [128, 4, S], BF16)
        ktT = sb("h_kt", [128, 4, S], BF16)
        ksT = sb("h_ks", [128, 4, S], BF16)
        sgT = sb("h_sg", [128, 4, S], BF16)
        bdT = sb("h_bd", [128, 4, 64], F32)
        T = [[sb("h_t%d_%d" % (a, i), [128, 512], F32) for i in range(5)] for a in range(2)]
        lbr = sb("h_lbr", [128, 2, 8], F32)
        lbt = sb("h_lb", [128, 8], F32)
        oml = sb("h_oml", [128, 8], F32)
        gn = sb("h_gn", [128, 1], F32)
        bm = sb("h_bm", [128, 128], F32)
        scanm = sb("h_scanm", [128, 512], F32)
        ones_f = sb("h_ones", [128, 128], BF16)
        am = [[sb("h_am%d_%d" % (a, j), [128, 128], BF16) for j in range(4)] for a in range(2)]
        kst = [[sb("h_kst%d_%d" % (a, j), [128, 128], BF16) for j in range(4)] for a in range(2)]
        stf = [sb("h_stf%d" % j, [128, 128], F32) for j in range(4)]
        stb = [[sb("h_stb%d_%d" % (j, a), [128, 128], BF16) for a in range(2)] for j in range(4)]
        osb = [sb("h_osb%d" % a, [128, 512], F32) for a in range(2)]
        sq = [sb("h_sq%d" % a, [128, 512], BF16) for a in range(2)]
        rt = [sb("h_rt%d" % a, [128, 512], F32) for a in range(2)]
        xo = [sb("h_xo%d" % i, [128, 4, 128], BF16) for i in range(2)]
        blkm = sb("h_blkm", [128, 128 // HL], F32)
        vblk = [[sb("h_vblk%d_%d" % (a, j), [128, 128 // HL, 128], BF16) for j in range(4)] for a in range(2)]
        psU = [G.ps[2], G.ps[3], G.ps[6], G.ps[7]]
        P.dma('sp', blkm[:, :], G.K['hg_blk'], w=['blkm'])
        P.dma('sp', bm[:, :], G.K['hg_bm'], w=['bm'])
        P.dma('sp', scanm[:, :], G.K['hg_scanm'], w=['scanm'])
        P.dma('sp', gn[:, :], G.W['hgrn_norm_g'][l].rearrange("(p o) -> p o", o=1), w=['gn'])
        P.op('pool', lambda e: e.memset(ones_f[:, :], 1.0), w=['ones_f'])
        if l == 0:
            P.op('pool', lambda e: e.memset(lbt[:, :], 0.0), w=['lbt'])
        else:
            for a in range(2):
                P.dma('sp', lbr[:, a, :], G.W['hgrn_lower_bounds'][a].rearrange("(h d) -> d h", d=128), w=[('lbr', a)],
                      allow_slow_non_contiguous=True)
            P.op('dve', lambda e: e.tensor_tensor(lbt[:, :], lbr[:, 1, :], lbr[:, 0, :], ALU.subtract), r=[('lbr', 0), ('lbr', 1)], w=['lbt'])
            P.op('act', lambda e: e.activation(lbt[:, :], lbt[:, :], AF.Sigmoid), r=['lbt'], w=['lbt'])
        P.op('dve', lambda e: e.tensor_scalar(oml[:, :], lbt[:, :], -1.0, 1.0, ALU.mult, ALU.add), r=['lbt'], w=['oml'])
        NB = 128 // HL
        for hg in range(2):
            G.ps_sel = [0, 1, 2, 3, 6, 7]
            wt, wk = load_w(G, w_in, OFF['hi'] + hg * 512, 512, tag=('hi', l, hg))
            for t in range(NT):
                ps, pk = tm_group(G, wt, wk, t)
                P.op('act', lambda e, ps=ps, t=t: e.activation(v_tok[:, t, :], ps[:, :], AF.Copy), r=[pk], w=[('v', t)])
            wq, wqk = load_w(G, w_in, OFF['hq'] + hg * 512, 512)
            wf, wfk = load_w(G, w_in, OFF['hf'] + hg * 512, 512)
            wg, wgk = load_w(G, w_in, OFF['hg'] + hg * 512, 512)

            def h2unit(j, tg, a):
                h = hg * 4 + j
                t1, t2, t3, t4, t5 = T[a]
                tk = lambda i, a=a: ('T', a, i)
                cols = slice(tg * 512, (tg + 1) * 512)
                psz, pkz = fm_group(G, wf, wfk, j, tg)
                yield
                P.op('act', lambda e: e.activation(t2[:, :], psz[:, :], AF.Exp, scale=-1.0), r=[pkz], w=[tk(2)])
                yield
                P.op('act', lambda e: e.activation(t1[:, :], t2[:, :], AF.Ln, bias=1.0, scale=1.0), r=[tk(2)], w=[tk(1)])
                yield
                P.op('act', lambda e: e.activation(t1[:, :], t1[:, :], AF.Exp, scale=-1.0), r=[tk(1)], w=[tk(1)])
                yield
                P.op('dve', lambda e: e.tensor_tensor(t2[:, :], t2[:, :], t1[:, :], ALU.mult), r=[tk(1), tk(2)], w=[tk(2)])
                psq, pkq = fm_group(G, wq, wqk, j, tg)
                yield
                P.op('dve', lambda e: e.tensor_scalar(t1[:, :], t1[:, :], oml[:, h:h + 1], lbt[:, h:h + 1], ALU.mult, ALU.add),
                     r=[tk(1), 'oml', 'lbt'], w=[tk(1)])
                yield
                P.op('act', lambda e: e.activation(t3[:, :], t1[:, :], AF.Ln), r=[tk(1)], w=[tk(3)])
                yield
                P.op('dve', lambda e: e.tensor_tensor_scan(t4[:, :], scanm[:, :], t3[:, :], 0.0, ALU.mult, ALU.add),
                     r=[tk(3), 'scanm'], w=[tk(4)])
                yield
                P.op('act', lambda e: e.activation(t1[:, :], t4[:, :], AF.Exp), r=[tk(4)], w=[tk(1)])
                P.op('act', lambda e: e.activation(t3[:, :], t4[:, :], AF.Exp, scale=-1.0), r=[tk(4)], w=[tk(3)])
                yield
                P.op('dve', lambda e: e.tensor_tensor(qtT[:, j, cols], psq[:, :], t1[:, :], ALU.mult),
                     r=[pkq, tk(1)], w=[('qt', j, tg)])
                yield
                P.op('dve', lambda e: e.scalar_tensor_tensor(t2[:, :], t2[:, :], oml[:, h:h + 1], t3[:, :], ALU.mult, ALU.mult),
                     r=[tk(2), tk(3), 'oml'], w=[tk(2)])
                yield
                P.op('act', lambda e: e.activation(ktT[:, j, cols], t2[:, :], AF.Copy), r=[tk(2)], w=[('kt', j, tg)])
                yield
                P.op('dve', lambda e: e.tensor_tensor(
                    ksT[:, j, cols].rearrange("p (b l) -> p b l", l=HL),
                    t2[:, :].rearrange("p (b l) -> p b l", l=HL),
                    t1[:, :].rearrange("p (b l) -> p b l", l=HL)[:, :, HL - 1:HL].broadcast_to([128, 512 // HL, HL]), ALU.mult),
                    r=[tk(1), tk(2)], w=[('ks', j, tg)])
                yield
                nb = 512 // HL
                P.op('dve', lambda e: e.tensor_copy(
                    bdT[:, j, tg * nb:(tg + 1) * nb],
                    t1[:, :].rearrange("p (b l) -> p b l", l=HL)[:, :, HL - 1:HL].rearrange("p b o -> p (b o)")),
                    r=[tk(1)], w=[('bd', j, tg)])
                yield
            for j in range(4):
                for tg in range(4):
                    psg, pkg = fm_group(G, wg, wgk, j, tg)
                    P.op('act', lambda e, psg=psg, j=j, tg=tg: e.activation(sgT[:, j, tg * 512:(tg + 1) * 512], psg[:, :], AF.Silu), r=[pkg], w=[('sg', j, tg)])
            units = [(j, tg) for j in range(4) for tg in range(4)]
            rr((h2unit(j, tg, n % 2) for n, (j, tg) in enumerate(units)), 2)
            if HG_LEVEL < 2:
                continue
            G.ps_sel = [0, 1]
            psok = [('ps', 4), ('ps', 5)]
            psUk = [('ps', 2), ('ps', 3), ('ps', 6), ('ps', 7)]

            def front(i):
                tg = i // 4
                cols = slice(i * 128, (i + 1) * 128)
                a = i % 2
                for j in range(4):
                    ps, pk = next_ps(G)
                    P.op('pe', lambda e, ps=ps, j=j: e.matmul(ps[:, 0:128], ktT[:, j, cols], qtT[:, j, cols], start=True, stop=True),
                         r=[('kt', j, tg), ('qt', j, tg)], w=[pk])
                    P.op('dve', lambda e, ps=ps, j=j: e.tensor_tensor(am[a][j][:, :], ps[:, 0:128], bm[:, :], ALU.mult), r=[pk, 'bm'], w=[('am', a, j)])
                    ps, pk = next_ps(G)
                    P.op('pe', lambda e, ps=ps, j=j: e.matmul(ps[:, 128:256], ksT[:, j, cols], G.ident_b[:, :], start=True, stop=True),
                         r=[('ks', j, tg), 'ident_b'], w=[pk])
                    P.op('act', lambda e, ps=ps, j=j: e.activation(kst[a][j][:, :], ps[:, 128:256], AF.Copy), r=[pk], w=[('kst', a, j)])
                    P.op('dve', lambda e, j=j: e.tensor_tensor(
                        vblk[a][j][:, :, :], v_tok[:, i, j * 128:(j + 1) * 128].rearrange("p (o v) -> p o v", o=1).broadcast_to([128, NB, 128]),
                        blkm[:, :].rearrange("p (b o) -> p b o", o=1).broadcast_to([128, NB, 128]), ALU.mult),
                        r=[('v', i), 'blkm'], w=[('vblk', a, j)])

            def umat(i):
                a = i % 2
                for j in range(4):
                    P.op('pe', lambda e, j=j: e.matmul(psU[j][:, :], kst[a][j][:, :], vblk[a][j][:, :, :].rearrange("p b v -> p (b v)"), start=True, stop=True),
                         r=[('kst', a, j), ('vblk', a, j)], w=[psUk[j]])

            def mid(i):
                tg = i // 4
                a = i % 2
                pso = G.ps[4 + a]
                pok = psok[a]
                if HG_MERGE:
                    for j in range(4):
                        P.op('pe', lambda e, j=j: e.matmul(pso[:, j * 128:(j + 1) * 128], v_tok[:, i, j * 128:(j + 1) * 128], am[a][j][:, :],
                                                           start=True, stop=False, skip_group_check=True),
                             r=[('v', i), ('am', a, j)], w=[pok])
                for b in range(NB):
                    gb = i * NB + b
                    for j in range(4):
                        def fo(e, j=j, b=b, gb=gb):
                            oc = pso[:, j * 128 + b * HL: j * 128 + (b + 1) * HL]
                            ins = None
                            if not HG_MERGE:
                                ins = e.matmul(oc, v_tok[:, i, j * 128:(j + 1) * 128], am[a][j][:, b * HL:(b + 1) * HL], start=True, stop=(gb == 0))
                            if gb > 0:
                                ins = e.matmul(oc, stb[j][gb % 2][:, :], qtT[:, j, i * 128 + b * HL: i * 128 + (b + 1) * HL], start=False, stop=True,
                                               skip_group_check=HG_MERGE)
                            return ins
                        if HG_MERGE and gb == 0:
                            continue
                        P.op('pe', fo, r=[('v', i), ('am', a, j), ('stb', j, gb % 2), ('qt', j, tg)], w=[pok])
                    if gb < S // HL - 1:
                        for j in range(4):
                            if gb == 0:
                                P.op('dve', lambda e, j=j, b=b: e.tensor_copy(stf[j][:, :], psU[j][:, b * 128:(b + 1) * 128]), r=[psUk[j]], w=[('stf', j)])
                            else:
                                P.op('dve', lambda e, j=j, gb=gb, b=b: e.scalar_tensor_tensor(
                                    stf[j][:, :], stf[j][:, :], bdT[:, j, gb:gb + 1], psU[j][:, b * 128:(b + 1) * 128], ALU.mult, ALU.add),
                                    r=[psUk[j], ('stf', j), ('bd', j, gb // (512 // HL))], w=[('stf', j)])
                            P.op('act', lambda e, j=j, gb=gb: e.activation(stb[j][(gb + 1) % 2][:, :], stf[j][:, :], AF.Copy),
                                 r=[('stf', j)], w=[('stb', j, (gb + 1) % 2)])

            def back(i, part):
                tg = i // 4
                cols = slice(i * 128, (i + 1) * 128)
                a = i % 2
                pso = G.ps[4 + a]
                pok = psok[a]
                osb_, sq_, rt_ = osb[a], sq[a], rt[a]
                if part == 0:
                    P.op('act', lambda e: e.activation(osb_[:, :], pso[:, :], AF.Copy), r=[pok], w=[('osb', a)])
                    P.op('act', lambda e: e.activation(sq_[:, :], pso[:, :], AF.Square), r=[pok], w=[('sq', a)])
                    return
                ps, pk = next_ps(G)
                P.op('pe', lambda e, ps=ps: e.matmul(ps[:, :], ones_f[:, :], sq_[:, :], start=True, stop=True), r=[('sq', a), 'ones_f'], w=[pk])
                P.op('act', lambda e, ps=ps: e.activation(rt_[:, :], ps[:, :], AF.Ln, bias=RMS_EPS, scale=1.0 / 128.0), r=[pk], w=[('rt', a)])
                P.op('act', lambda e: e.activation(rt_[:, :], rt_[:, :], AF.Exp, scale=-0.5), r=[('rt', a)], w=[('rt', a)])
                P.op('dve', lambda e: e.scalar_tensor_tensor(osb_[:, :], osb_[:, :], gn[:, 0:1], rt_[:, :], ALU.mult, ALU.mult), r=[('osb', a), ('rt', a), 'gn'], w=[('osb', a)])
                P.op('dve', lambda e: e.tensor_tensor(
                    xo[a][:, :, :], osb_[:, :].rearrange("p (a t) -> p a t", t=128), sgT[:, :, cols], ALU.mult),
                    r=[('osb', a)] + [('sg', j, tg) for j in range(4)], w=[('xo', a)])
                P.dma('sp', G.xT['b'][hg * 512:(hg + 1) * 512, cols].rearrange("(i p) t -> p i t", p=128), xo[a][:, :, :],
                      r=[('xo', a)], w=[('xbT', hg, i)])

            front(0)
            umat(0)
            for sstep in range(NT):
                if sstep + 1 < NT:
                    front(sstep + 1)
                mid(sstep)
                back(sstep, 0)
                if sstep >= 1:
                    back(sstep - 1, 1)
                if sstep + 1 < NT:
                    umat(sstep + 1)
            back(NT - 1, 1)


def phase_swa(G, l):
    P = G.P
    w_in = G.W['w_in'][l]
    G.ps_sel = list(range(8))
    with contextlib.ExitStack() as st:
        sb = lambda n, s, d: G.sb(n, s, d, st)
        qT = sb("s_q", [128, 8, S], BF16)
        kT = sb("s_k", [128, 4, 2, 128 + S], BF16)
        vt = sb("s_v", [128, NT + 1, 4, 2, 128], BF16)
        mask = sb("s_mask", [128, 2, 256], F32)
        sink = sb("s_sink", [128, 16], F32)
        with contextlib.ExitStack() as st1:
            wkd = G.sb("s_wkd", [128, KC, 4, 2, 128], BF16, st1)
            P.dma('sp', mask[:, :, :], G.K['swa_mask'], w=['mask'])
            P.dma('sp', sink[:, :], G.W['swa_sinks'][l].partition_broadcast(128), w=['sink'])
            P.op('pool', lambda e: e.memset(kT[:, :, :, 0:128], 0.0), w=['kpad'])
            P.op('pool', lambda e: e.memset(vt[:, :, :, :, :].rearrange("p a b c d -> p (a b c d)"), 0.0), w=['vt0'])
            P.op('pool', lambda e: e.memset(wkd[:, :, :, :, :].rearrange("p a b c d -> p (a b c d)"), 0.0), w=['wkd0'])
            for g in range(4):
                for var in range(2):
                    P.dma('pool', wkd[:, :, g, var, var * 64:(var + 1) * 64],
                          w_in[:, OFF['sk'] + g * 64: OFF['sk'] + (g + 1) * 64].rearrange("(k p) c -> p k c", p=128),
                          r=['wkd0'], w=[('wkd', g, var)])
            for blk in range(2):
                wt, wk = load_w(G, w_in, OFF['sq'] + blk * 512, 512, tag=('sq', l, blk))
                for cc in range(4):
                    for tg in range(4):
                        ps, pk = fm_group(G, wt, wk, cc, tg)
                        P.op('act', lambda e, ps=ps, c8=blk * 4 + cc, tg=tg: e.activation(qT[:, c8, tg * 512:(tg + 1) * 512], ps[:, :], AF.Copy, scale=0.125),
                             r=[pk], w=[('q', blk * 4 + cc, tg)])
            for g in range(4):
                for var in range(2):
                    for tg in range(4):
                        ps, pk = next_ps(G)

                        def fk(e, ps=ps, g=g, var=var, tg=tg):
                            ins = None
                            for k in range(KC):
                                ins = e.matmul(ps[:, :], wkd[:, k, g, var, :], G.hT[:, k, tg * 512:(tg + 1) * 512], start=(k == 0), stop=(k == KC - 1))
                            return ins
                        P.op('pe', fk, r=[('wkd', g, var), 'hT'], w=[pk])
                        P.op('act', lambda e, ps=ps, g=g, var=var, tg=tg: e.activation(kT[:, g, var, 128 + tg * 512:128 + (tg + 1) * 512], ps[:, :], AF.Copy),
                             r=[pk, 'kpad'], w=[('k', g, var, tg)])
            wt, wk = load_w(G, w_in, OFF['sv'], 256)
            for t in range(NT):
                ps, pk = tm_group(G, wt, wk, t, ncols=256)
                P.op('act', lambda e, ps=ps, t=t: e.activation(vt[:, t + 1, :, 0, 0:64], ps[:, 0:256].rearrange("p (g d) -> p g d", d=64), AF.Copy),
                     r=[pk, 'vt0'], w=[('vt', t + 1, 0)])
                P.op('act', lambda e, ps=ps, t=t: e.activation(vt[:, t + 1, :, 1, 64:128], ps[:, 0:256].rearrange("p (g d) -> p g d", d=64), AF.Copy),
                     r=[pk, 'vt0'], w=[('vt', t + 1, 1)])
            P.emit()
        NSL = SW_W
        sms = [sb("s_sm%d" % i, [128, 4, 256], F32) for i in range(NSL)]
        ps_ = [sb("s_p%d" % i, [128, 4, 256], BF16) for i in range(NSL)]
        mxs = [sb("s_mx%d" % i, [128, 16], F32) for i in range(NSL)]
        pTs = [sb("s_pT%d" % i, [128, 8, 128], BF16) for i in range(NSL)]
        xos = [sb("s_xo%d" % i, [128, 2, 128], BF16) for i in range(NSL)]

        def s2unit(i, g, slot):
            cols = slice(i * 128, (i + 1) * 128)
            mi = 0 if i == 0 else 1
            sm, p, mx, pT, xo = sms[slot], ps_[slot], mxs[slot], pTs[slot], xos[slot]
            bk = [slot * 2, slot * 2 + 1]
            kk = lambda n: (n, slot)
            for a in range(4):
                hq = 4 * g + a
                c8, var = hq // 2, hq % 2
                bank = bk[a // 2]
                P.op('pe', lambda e, a=a, c8=c8, var=var, bank=bank: e.matmul(
                    G.ps[bank][:, (a % 2) * 256:(a % 2 + 1) * 256], qT[:, c8, cols], kT[:, g, var, i * 128:i * 128 + 256], start=True, stop=True),
                    w=[('ps', bank)])
            yield
            for hh in range(2):
                bank = bk[hh]
                P.op('dve', lambda e, hh=hh, bank=bank: e.tensor_tensor(
                    sm[:, hh * 2:(hh + 1) * 2, :], G.ps[bank][:, :].rearrange("p (a k) -> p a k", k=256),
                    mask[:, mi:mi + 1, :].broadcast_to([128, 2, 256]), ALU.add),
                    r=[('ps', bank)], w=[kk(('sm', hh))])
            yield
            P.op('dve', lambda e: e.tensor_reduce(mx[:, 0:4], sm[:, :, :], AX.X, ALU.max), r=[kk(('sm', 0)), kk(('sm', 1))], w=[kk('mx_m')])
            yield
            P.op('dve', lambda e: e.tensor_tensor(mx[:, 0:4], mx[:, 0:4], sink[:, 4 * g:4 * g + 4], ALU.max), r=[kk('mx_m')], w=[kk('mx_m')])
            yield
            P.op('dve', lambda e: e.tensor_scalar(mx[:, 4:8], mx[:, 0:4], -1.0, None, ALU.mult), r=[kk('mx_m')], w=[kk('mx_n')])
            yield
            for a in range(4):
                P.op('act', lambda e, a=a: e.activation(p[:, a, :], sm[:, a, :], AF.Exp, bias=mx[:, 4 + a:5 + a], scale=1.0, accum_out=mx[:, 8 + a:9 + a]),
                     r=[kk(('sm', a // 2)), kk('mx_n')], w=[kk('p'), kk(('mx_s', a))])
            P.op('dve', lambda e: e.tensor_tensor(mx[:, 12:16], sink[:, 4 * g:4 * g + 4], mx[:, 4:8], ALU.add), r=[kk('mx_n')], w=[kk('mx_d')])
            yield
            P.op('act', lambda e: e.activation(mx[:, 12:16], mx[:, 12:16], AF.Exp), r=[kk('mx_d')], w=[kk('mx_d')])
            yield
            P.op('dve', lambda e: e.tensor_tensor(mx[:, 12:16], mx[:, 12:16], mx[:, 8:12], ALU.add), r=[kk('mx_d')] + [kk(('mx_s', a)) for a in range(4)], w=[kk('mx_d')])
            yield
            P.op('dve', lambda e: e.reciprocal(mx[:, 12:16], mx[:, 12:16]), r=[kk('mx_d')], w=[kk('mx_d')])
            yield
            P.op('dve', lambda e: e.tensor_tensor(p[:, :, :], p[:, :, :], mx[:, 12:16].rearrange("p (a o) -> p a o", o=1).broadcast_to([128, 4, 256]), ALU.mult),
                 r=[kk('p'), kk('mx_d')], w=[kk('p')])
            yield
            for hh in range(2):
                def ftr(e, hh=hh):
                    ins = None
                    for q in range(4):
                        idx = hh * 4 + q
                        a, kt = idx // 2, idx % 2
                        ins = e.matmul(G.ps[bk[hh]][:, q * 128:(q + 1) * 128], p[:, a, kt * 128:(kt + 1) * 128], G.ident_b[:, :], start=True, stop=True)
                    return ins
                P.op('pe', ftr, r=[kk('p'), 'ident_b', kk(('sm', hh))], w=[('ps', bk[hh])])
            yield
            P.op('act', lambda e: e.activation(pT[:, 0:4, :], G.ps[bk[0]][:, :].rearrange("p (a t) -> p a t", t=128), AF.Copy), r=[('ps', bk[0])], w=[kk(('pT', 0))])
            P.op('dve', lambda e: e.tensor_copy(pT[:, 4:8, :], G.ps[bk[1]][:, :].rearrange("p (a t) -> p a t", t=128)), r=[('ps', bk[1])], w=[kk(('pT', 1))])
            yield
            for pr in range(2):
                pso = G.ps[bk[pr]]

                def fpv(e, pso=pso, pr=pr):
                    ins = None
                    n = 0
                    for (a, var) in ((2 * pr, 0), (2 * pr + 1, 1)):
                        for kt in range(2):
                            ins = e.matmul(pso[:, 0:128], vt[:, i + kt, g, var, :], pT[:, a * 2 + kt, :], start=(n == 0), stop=(n == 3))
                            n += 1
                    return ins
                P.op('pe', fpv, r=[kk(('pT', pr))], w=[('ps', bk[pr])])
                yield
                P.op('act', lambda e, pso=pso, pr=pr: e.activation(xo[:, pr, :], pso[:, 0:128], AF.Copy), r=[('ps', bk[pr])], w=[kk(('xo', pr))])
                yield
            P.dma('sp', G.xT['c'][g * 256:(g + 1) * 256, cols].rearrange("(i p) t -> p i t", p=128), xo[:, :, :],
                  r=[kk(('xo', 0)), kk(('xo', 1))], w=[('xcT', g, i)])
            yield
        units = [(i, g) for i in range(NT) for g in range(4)]
        rr((s2unit(i, g, n % NSL) for n, (i, g) in enumerate(units)), NSL)


def phase_merge(G, l):
    P = G.P
    G.ps_sel = list(range(8))
    w_in = G.W['w_in'][l]
    with contextlib.ExitStack() as st:
        sb = lambda n, s, d: G.sb(n, s, d, st)
        xs = {b: sb("m_x" + b, [128, KC, S], BF16) for b in 'abc'}
        ws_save = G.ws
        G.ws = list(G.ws) + [sb("m_ws%d" % i, [128, KC, 512], BF16) for i in range(2)]
        G.ws_rr = 4 if getattr(G, 'prefetched', None) else 0
        sg = [sb("m_sg%d" % i, [128, 512], F32) for i in range(3)]
        macc = sb("m_acc", [128, 512], F32)
        mo = [sb("m_o%d" % i, [128, 512], BF16) for i in range(2)]
        for b in 'abc':
            for k in range(KC):
                P.dma('sp', xs[b][:, k, :], G.xT[b][k * 128:(k + 1) * 128, :], w=[('x', b)] if k == 0 else [('x', b, k)])
        xkeys = {b: [('x', b)] + [('x', b, k) for k in range(1, KC)] for b in 'abc'}
        wouts = {'a': G.W['ret_w_out'][l], 'b': G.W['hgrn_w_out'][l], 'c': G.W['swa_w_out'][l]}
        goff = {'a': OFF['ga'], 'b': OFF['gb'], 'c': OFF['gc']}
        n = 0
        for cb in range(2):
            wy = {b: load_w(G, wouts[b], cb * 512, 512, tag=('my', l, b, cb)) for b in 'abc'}
            wg = {b: load_w(G, w_in, goff[b] + cb * 512, 512, tag=('mg', l, b, cb)) for b in 'abc'}
            for cc in range(4):
                for tg in range(4):
                    oi = n % 2
                    n += 1
                    for bi, b in enumerate('abc'):
                        psy, pky = fm_group(G, wy[b][0], wy[b][1], cc, tg, rkeys=xkeys[b], src=xs[b])
                        psg, pkg = fm_group(G, wg[b][0], wg[b][1], cc, tg)
                        P.op('act', lambda e, psg=psg, bi=bi: e.activation(sg[bi][:, :], psg[:, :], AF.Sigmoid), r=[pkg], w=[('sg', bi)])
                        if bi == 0:
                            P.op('dve', lambda e, psy=psy, bi=bi: e.tensor_tensor(macc[:, :], sg[bi][:, :], psy[:, :], ALU.mult), r=[('sg', bi), pky], w=['macc'])
                        else:
                            P.op('dve', lambda e, psy=psy, bi=bi: e.tensor_tensor(sg[bi][:, :], sg[bi][:, :], psy[:, :], ALU.mult), r=[('sg', bi), pky], w=[('sg', bi)])
                            if bi == 1:
                                P.op('dve', lambda e, bi=bi: e.tensor_tensor(macc[:, :], macc[:, :], sg[bi][:, :], ALU.add), r=['macc', ('sg', bi)], w=['macc'])
                            else:
                                P.op('dve', lambda e, bi=bi, oi=oi: e.tensor_tensor(mo[oi][:, :], macc[:, :], sg[bi][:, :], ALU.add), r=['macc', ('sg', bi)], w=[('mo', oi)])
                    c8 = cb * 4 + cc
                    P.dma('sp', G.mgT[c8 * 128:(c8 + 1) * 128, tg * 512:(tg + 1) * 512], mo[oi][:, :], r=[('mo', oi)], w=[('mgT', c8, tg)])
        G.ws = ws_save
        G.ws_rr = 0


def router_tile(G, R, tile, pss, slot=0):
    P = G.P
    wr, hTfs, lgs, rrs = R
    hTf, lg, rr_ = hTfs[slot], lgs[slot], rrs[slot]
    k = lambda n: (n, slot)
    for hb, (ps, pk) in enumerate(pss):
        P.op('act', lambda e, ps=ps, hb=hb: e.activation(hTf[:, hb * 4:(hb + 1) * 4, :], ps[:, :].rearrange("p (a t) -> p a t", t=128), AF.Copy),
             r=[pk], w=[k(('hTf', hb))])
        yield
    ps, pk = next_ps(G)

    def fr(e, ps=ps):
        ins = None
        for kk in range(KC):
            ins = e.matmul(ps[:, 0:NEXP], hTf[:, kk, :], wr[:, kk, :], start=(kk == 0), stop=(kk == KC - 1))
        return ins
    P.op('pe', fr, r=[k(('hTf', 0)), k(('hTf', 1)), 'wr'], w=[pk])
    yield
    P.op('dve', lambda e, ps=ps: e.tensor_copy(lg[:, 0:8], ps[:, 0:NEXP]), r=[pk], w=[k('lg')])
    yield
    P.op('dve', lambda e: e.tensor_reduce(rr_[:, 0:1], lg[:, 0:8], AX.X, ALU.max), r=[k('lg')], w=[k('rr0')])
    yield
    P.op('dve', lambda e: e.tensor_scalar(lg[:, 8:16], lg[:, 0:8], rr_[:, 0:1], None, ALU.is_equal), r=[k('lg'), k('rr0')], w=[k('lg1')])
    yield
    P.op('dve', lambda e: e.scalar_tensor_tensor(lg[:, 8:16], lg[:, 8:16], -1e30, lg[:, 0:8], ALU.mult, ALU.add), r=[k('lg'), k('lg1')], w=[k('lg1')])
    yield
    P.op('dve', lambda e: e.tensor_reduce(rr_[:, 1:2], lg[:, 8:16], AX.X, ALU.max), r=[k('lg1')], w=[k('rr1')])
    yield
    P.op('dve', lambda e: e.tensor_scalar(lg[:, 8:16], lg[:, 0:8], rr_[:, 1:2], None, ALU.is_ge), r=[k('lg'), k('rr1'), k('lg1')], w=[k('lg1')])
    P.op('dve', lambda e: e.tensor_scalar(rr_[:, 2:3], rr_[:, 0:1], -1.0, None, ALU.mult), r=[k('rr0')], w=[k('rr2')])
    yield
    P.op('act', lambda e: e.activation(lg[:, 16:24], lg[:, 0:8], AF.Exp, bias=rr_[:, 2:3], scale=1.0), r=[k('lg'), k('rr2')], w=[k('lg2')])
    yield
    P.op('dve', lambda e: e.tensor_tensor(lg[:, 16:24], lg[:, 16:24], lg[:, 8:16], ALU.mult), r=[k('lg2'), k('lg1')], w=[k('lg2')])
    yield
    P.op('dve', lambda e: e.tensor_reduce(rr_[:, 3:4], lg[:, 16:24], AX.X, ALU.add), r=[k('lg2')], w=[k('rr3')])
    yield
    P.op('dve', lambda e: e.reciprocal(rr_[:, 3:4], rr_[:, 3:4]), r=[k('rr3')], w=[k('rr3')])
    yield
    P.op('dve', lambda e, tile=tile: e.tensor_scalar(G.gate_all[:, tile, :], lg[:, 16:24], rr_[:, 3:4], None, ALU.mult), r=[k('lg2'), k('rr3')], w=[('gate', tile)])
    yield


def phase_wo(G, l):
    P = G.P
    G.ps_sel = list(range(8))
    with contextlib.ExitStack() as st:
        sb = lambda n, s, d: G.sb(n, s, d, st)
        mg = sb("o_mg", [128, KC, S], BF16)
        G.xt = [sb("o_xt%d" % i, [128, D], F32) for i in range(LN_W + 1)]
        G.gbc = sb("o_gbc", [128, D], F32)
        G.bbc = sb("o_bbc", [128, D], F32)
        R = None
        if l % 2 == 1:
            wr = sb("o_wr", [128, KC, NEXP], F32)
            hTfs = [sb("o_hTf%d" % i, [128, KC, 128], F32) for i in range(LN_W)]
            lgs = [sb("o_lg%d" % i, [128, 24], F32) for i in range(LN_W)]
            rrs = [sb("o_rr%d" % i, [128, 4], F32) for i in range(LN_W)]
            R = (wr, hTfs, lgs, rrs)
            P.dma('sp', wr[:, :, :], G.W['moe_router'][l // 2].rearrange("(k p) e -> p k e", p=128), w=['wr'])
        for k in range(KC):
            P.dma('sp', mg[:, k, :], G.mgT[k * 128:(k + 1) * 128, :], w=[('mg', k)])
        mgk = [('mg', k) for k in range(KC)]
        load_ln_params(G, G.W['ln_mix_g'][l], G.W['ln_mix_b'][l])
        w0 = load_w(G, G.W['w_o'][l], 0, 512, tag=('wo', l))
        w1 = load_w(G, G.W['w_o'][l], 512, 512)

        def unit(t):
            xt, xk = next_xt(G)
            P.dma('sp', xt[:, :], G.h_res[t * 128:(t + 1) * 128, :], r=[('h_res', t)], w=[xk])
            yield
            for hf, (wt, wk) in enumerate((w0, w1)):
                ps, pk = tm_group(G, wt, wk, t, rkeys=mgk, src=mg)
                yield
                P.op('dve', lambda e, ps=ps, xt=xt, hf=hf: e.scalar_tensor_tensor(
                    xt[:, hf * 512:(hf + 1) * 512], xt[:, hf * 512:(hf + 1) * 512], DN_ALPHA, ps[:, :], ALU.mult, ALU.add), r=[pk, xk], w=[xk])
                yield
            pss = []
            yield from ln_tile(G, xt, xk, t, slot=t % LN_W, pss=pss)
            if R is not None:
                yield from router_tile(G, R, t, pss, slot=t % LN_W)
        rr((unit(t) for t in range(NT)), LN_W)


def phase_ffn(G, l):
    P = G.P
    G.ps_sel = list(range(8))
    moe = (l % 2 == 1)
    jx = l // 2
    last = (l == DEPTH - 1)
    with contextlib.ExitStack() as st:
        sb = lambda n, s, d: G.sb(n, s, d, st)
        yacc = sb("f_y", [128, NT, D], F32)
        hid = sb("f_hid", [128, 4, S], BF16)
        wds = [sb("f_wd%d" % i, [128, 4, D], BF16) for i in range(2)]
        sl = [sb("f_sl%d" % i, [128, 512], F32) for i in range(2)]
        G.xt = [sb("f_xt%d" % i, [128, D], F32) for i in range(LN_W + 1)]
        G.xt_rr = 0
        G.gbc = sb("f_gbc", [128, D], F32)
        G.bbc = sb("f_bbc", [128, D], F32)
        if moe:
            experts = [(G.W['moe_w_gate'][jx][e], G.W['moe_w_up'][jx][e], G.W['moe_w_down'][jx][e], e) for e in range(NEXP)]
            F = D_EXP
        else:
            experts = [(G.W['ffn_w_gate'][jx], G.W['ffn_w_up'][jx], G.W['ffn_w_down'][jx], None)]
            F = D_FF
        nblk = (F + 511) // 512
        load_ln_params(G, G.W['ln_ffn_g'][l], G.W['ln_ffn_b'][l])
        ln_active = []

        def unit(t):
            xt, xk = next_xt(G)
            P.dma('sp', xt[:, :], G.h_res[t * 128:(t + 1) * 128, :], r=[('h_res', t)], w=[xk])
            yield
            P.op('dve', lambda e, xt=xt, t=t: e.scalar_tensor_tensor(xt[:, :], xt[:, :], DN_ALPHA, yacc[:, t, :], ALU.mult, ALU.add),
                 r=[xk, ('y', t, 0), ('y', t, 1)], w=[xk])
            yield
            yield from ln_tile(G, xt, xk, t, slot=t % LN_W, out_final=(G.out if last else None))

        first = True
        n = 0
        wdi = 0
        for (wg_d, wu_d, wd_d, e_idx) in experts:
            for blk in range(nblk):
                ncols = min(512, F - blk * 512)
                nj = ncols // 128
                wg, wgk = load_w(G, wg_d, blk * 512, ncols, tag=('ffn', l, e_idx, blk))
                wu, wuk = load_w(G, wu_d, blk * 512, ncols)
                wd = wds[wdi % 2]
                wdk = ('wd', wdi % 2)
                wdi += 1
                P.dma('pool', wd[:, 0:nj, :], wd_d[blk * 512:blk * 512 + ncols, :].rearrange("(j p) c -> p j c", p=128), w=[wdk])
                for j in range(nj):
                    for tg in range(4):
                        si = n % 2
                        n += 1
                        psg, pkg = fm_group(G, wg, wgk, j, tg)
                        psu, pku = fm_group(G, wu, wuk, j, tg)
                        P.op('act', lambda e, psg=psg, si=si: e.activation(sl[si][:, :], psg[:, :], AF.Silu), r=[pkg], w=[('sl', si)])
                        P.op('dve', lambda e, psu=psu, si=si, j=j, tg=tg: e.tensor_tensor(hid[:, j, tg * 512:(tg + 1) * 512], sl[si][:, :], psu[:, :], ALU.mult),
                             r=[('sl', si), pku], w=[('hid', j, tg)])
                is_last = (e_idx is None or e_idx == NEXP - 1) and blk == nblk - 1
                for t in range(NT):
                    for hf in range(2):
                        ps, pk = next_ps(G)

                        def fd(e, ps=ps, t=t, hf=hf, nj=nj, wd=wd):
                            ins = None
                            for j in range(nj):
                                ins = e.matmul(ps[:, :], hid[:, j, t * 128:(t + 1) * 128], wd[:, j, hf * 512:(hf + 1) * 512], start=(j == 0), stop=(j == nj - 1))
                            return ins
                        P.op('pe', fd, r=[('hid', j, t // 4) for j in range(nj)] + [wdk], w=[pk])
                        ya = yacc[:, t, hf * 512:(hf + 1) * 512]
                        yk = ('y', t, hf)
                        if e_idx is None:
                            if first:
                                P.op('act', lambda e, ps=ps, ya=ya: e.activation(ya, ps[:, :], AF.Copy), r=[pk], w=[yk])
                            else:
                                P.op('dve', lambda e, ps=ps, ya=ya: e.tensor_tensor(ya, ya, ps[:, :], ALU.add), r=[pk, yk], w=[yk])
                        else:
                            gsc = G.gate_all[:, t, e_idx:e_idx + 1]
                            if first:
                                P.op('act', lambda e, ps=ps, ya=ya, gsc=gsc: e.activation(ya, ps[:, :], AF.Copy, scale=gsc), r=[pk, ('gate', t)], w=[yk])
                            else:
                                P.op('dve', lambda e, ps=ps, ya=ya, gsc=gsc: e.scalar_tensor_tensor(ya, ps[:, :], gsc, ya, ALU.mult, ALU.add),
                                     r=[pk, yk, ('gate', t)], w=[yk])
                    if is_last:
                        while len(ln_active) >= LN_W:
                            for g_ in list(ln_active):
                                try:
                                    next(g_)
                                except StopIteration:
                                    ln_active.remove(g_)
                        ln_active.append(unit(t))
                        for _ in range(4):
                            for g_ in list(ln_active):
                                try:
                                    next(g_)
                                except StopIteration:
                                    ln_active.remove(g_)
                first = False
        while ln_active:
            for g_ in list(ln_active):
                try:
                    next(g_)
                except StopIteration:
                    ln_active.remove(g_)


_CACHE = {}


def kernel(**inputs):
    if 'nc' not in _CACHE:
        _CACHE['nc'] = build()
    nc = _CACHE['nc']
    C = _consts()
    x = np.asarray(inputs['x'], np.float32)
    B = x.shape[0]
    base = {name: np.ascontiguousarray(np.asarray(inputs[name], np.float32)) for name, _ in W_SPECS}
    for name in CONST_NAMES:
        base["c_" + name] = np.ascontiguousarray(C[name])
    in_maps = []
    for b in range(B):
        m = dict(base)
        m['x'] = np.ascontiguousarray(x[b])
        in_maps.append(m)
    res = run_bass_kernel_spmd(nc, in_maps, core_ids=list(range(B)))
    return np.stack([np.asarray(r['out'], np.float32) for r in res.results], 0)
```
